# Optimizing a Trainium2 kernel written in Bass

```python
import math
import jax, jax.numpy as jnp
from jax import lax
import numpy as np

D_MODEL = 4096
BATCH = 1
SEQ = 8192
DEPTH = 1

MEM_LEN = 256
XA_HEADS = 4
XA_HEAD_DIM = 128
XA_WIDTH = XA_HEADS * XA_HEAD_DIM
SB_HEADS = 16
SB_HEAD_DIM = 128
SB_WIDTH = SB_HEADS * SB_HEAD_DIM
Q_BLOCK = 128
POOL_WINDOWS = (2, 4, 8, 16)
POOL_GROUPS = len(POOL_WINDOWS)
POOL_WIDTH = D_MODEL // 2
POOL_GROUP_DIM = POOL_WIDTH // POOL_GROUPS
IN_WIDTH = 3 * SB_WIDTH + POOL_WIDTH + 2 * D_MODEL
PEER_KEYS = 128
PEER_EXPERTS = PEER_KEYS * PEER_KEYS
PEER_HEADS = 8
PEER_QUERY_DIM = 256
PEER_HALF = PEER_QUERY_DIM // 2
PEER_TOPK = 16
PEER_CHUNK = 64
EPS = 1e-6

kernel_name = "hybrid_sba_pool_peer_block"


def rmsnorm(x, g):
    xf = x.astype(jnp.float32)
    y = xf * lax.rsqrt(jnp.mean(xf * xf, axis=-1, keepdims=True) + EPS)
    return (y * g.astype(jnp.float32)).astype(x.dtype)


def stick_breaking_attention(q, k, v):
    B, H, S, Dh = q.shape
    scale = Dh ** -0.5
    outs = []
    for blk in range(S // Q_BLOCK):
        t0 = blk * Q_BLOCK
        L = t0 + Q_BLOCK
        z = jnp.einsum('bhqd,bhkd->bhqk', q[:, :, t0:L], k[:, :, :L]).astype(jnp.float32) * scale
        t_idx = t0 + jnp.arange(Q_BLOCK)
        s_idx = jnp.arange(L)
        causal = s_idx[None, :] < t_idx[:, None]
        sp = jnp.where(causal, jax.nn.softplus(z), 0.0)
        between = lax.cumsum(sp, axis=3, reverse=True) - sp
        a = jnp.where(causal, jnp.exp(jax.nn.log_sigmoid(z) - between), 0.0)
        outs.append(jnp.einsum('bhqk,bhkd->bhqd', a.astype(v.dtype), v[:, :, :L]))
    return jnp.concatenate(outs, axis=2)


def multiscale_pool(u, w_groups, pool_scale):
    B, S, _ = u.shape
    ug = u.reshape(B, S, POOL_GROUPS, POOL_GROUP_DIM).astype(jnp.float32)
    cs = jnp.cumsum(ug, axis=1)
    pos = jnp.arange(S)
    outs = []
    for g, w in enumerate(POOL_WINDOWS):
        c = cs[:, :, g]
        lagged = jnp.pad(c, ((0, 0), (w, 0), (0, 0)))[:, :S]
        count = jnp.minimum(pos + 1, w).astype(jnp.float32)[None, :, None]
        outs.append((c - lagged) / count - ug[:, :, g])
    pooled = jnp.stack(outs, axis=2).astype(u.dtype)
    mixed = jnp.einsum('bsgc,gcd->bsgd', pooled, w_groups)
    return mixed.reshape(B, S, POOL_WIDTH) * pool_scale


def memory_cross_attention(h, mem_n, w_q, w_kv, w_o):
    B, S, _ = h.shape
    M = mem_n.shape[1]
    q = (h @ w_q).reshape(B, S, XA_HEADS, XA_HEAD_DIM)
    kv = (mem_n @ w_kv).reshape(B, M, 2, XA_HEADS, XA_HEAD_DIM)
    k, v = kv[:, :, 0], kv[:, :, 1]
    s = jnp.einsum('bqhd,bkhd->bhqk', q, k).astype(jnp.float32) * (XA_HEAD_DIM ** -0.5)
    p = jax.nn.softmax(s, axis=-1).astype(h.dtype)
    o = jnp.einsum('bhqk,bkhd->bqhd', p, v).reshape(B, S, XA_WIDTH)
    return o @ w_o


def peer_ffn(h, w_query, sub_keys, expert_down, expert_up):
    B, S, D = h.shape
    N = B * S
    tokens = h.reshape(N, D)
    q = (tokens @ w_query).reshape(N, PEER_HEADS, 2, PEER_HALF)
    scores = jnp.einsum('nhpc,hpkc->nhpk', q, sub_keys).astype(jnp.float32)
    s1, i1 = lax.top_k(scores[:, :, 0], PEER_TOPK)
    s2, i2 = lax.top_k(scores[:, :, 1], PEER_TOPK)
    cand = (s1[..., :, None] + s2[..., None, :]).reshape(N, PEER_HEADS, PEER_TOPK * PEER_TOPK)
    cand_id = (i1[..., :, None] * PEER_KEYS + i2[..., None, :]).reshape(N, PEER_HEADS, PEER_TOPK * PEER_TOPK)
    best, sel = lax.top_k(cand, PEER_TOPK)
    expert_id = jnp.take_along_axis(cand_id, sel, axis=-1)
    gate = jax.nn.softmax(best, axis=-1)
    n_chunks = N // PEER_CHUNK

    def chunk_fn(args):
        xc, idc, gc = args
        u = jnp.take(expert_down, idc, axis=0)
        act = jax.nn.gelu(jnp.einsum('chkd,cd->chk', u, xc).astype(jnp.float32), approximate=False)
        w = (gc * act).astype(xc.dtype)
        v = jnp.take(expert_up, idc, axis=0)
        return jnp.einsum('chk,chkd->cd', w, v)

    out = lax.map(chunk_fn, (tokens.reshape(n_chunks, PEER_CHUNK, D),
                             expert_id.reshape(n_chunks, PEER_CHUNK, PEER_HEADS, PEER_TOPK),
                             gate.reshape(n_chunks, PEER_CHUNK, PEER_HEADS, PEER_TOPK)))
    return out.reshape(B, S, D)


def setup_inputs(seed: int = 0) -> dict:
    key = jax.random.key(seed)
    ks = jax.random.split(key, 24)
    f32 = jnp.float32
    nrm = lambda k, shape, s: jax.random.normal(k, shape, f32) * s
    gain = lambda k, shape: 1.0 + 0.05 * jax.random.normal(k, shape, f32)
    L = DEPTH
    return {
        "x": nrm(ks[0], (BATCH, SEQ, D_MODEL), 1.0),
        "mem": nrm(ks[1], (BATCH, MEM_LEN, D_MODEL), 1.0),
        "norm_mix": gain(ks[2], (L, D_MODEL)),
        "norm_mem_q": gain(ks[3], (L, D_MODEL)),
        "norm_mem_kv": gain(ks[4], (L, D_MODEL)),
        "norm_ffn": gain(ks[5], (L, D_MODEL)),
        "norm_final": gain(ks[6], (D_MODEL,)),
        "w_in": nrm(ks[7], (L, D_MODEL, IN_WIDTH), D_MODEL ** -0.5),
        "pool_group_w": nrm(ks[8], (L, POOL_GROUPS, POOL_GROUP_DIM, POOL_GROUP_DIM), POOL_GROUP_DIM ** -0.5),
        "pool_scale": 0.5 + 0.1 * jax.random.normal(ks[9], (L, POOL_WIDTH), f32),
        "w_branch_sb": nrm(ks[10], (L, SB_WIDTH, D_MODEL), SB_WIDTH ** -0.5),
        "w_branch_pool": nrm(ks[11], (L, POOL_WIDTH, D_MODEL), POOL_WIDTH ** -0.5),
        "w_out": nrm(ks[12], (L, D_MODEL, D_MODEL), D_MODEL ** -0.5),
        "xa_w_q": nrm(ks[13], (L, D_MODEL, XA_WIDTH), D_MODEL ** -0.5),
        "xa_w_kv": nrm(ks[14], (L, D_MODEL, 2 * XA_WIDTH), D_MODEL ** -0.5),
        "xa_w_o": nrm(ks[15], (L, XA_WIDTH, D_MODEL), XA_WIDTH ** -0.5),
        "peer_w_query": nrm(ks[16], (L, D_MODEL, PEER_HEADS * PEER_QUERY_DIM), D_MODEL ** -0.5),
        "peer_sub_keys": nrm(ks[17], (L, PEER_HEADS, 2, PEER_KEYS, PEER_HALF), PEER_HALF ** -0.5),
        "peer_down": nrm(ks[18], (L, PEER_EXPERTS, D_MODEL), D_MODEL ** -0.5),
        "peer_up": nrm(ks[19], (L, PEER_EXPERTS, D_MODEL), PEER_HEADS ** -0.5),
    }


def reference(x, mem, norm_mix, norm_mem_q, norm_mem_kv, norm_ffn, norm_final, w_in, pool_group_w,
              pool_scale, w_branch_sb, w_branch_pool, w_out, xa_w_q, xa_w_kv, xa_w_o,
              peer_w_query, peer_sub_keys, peer_down, peer_up):
    B, S, _ = x.shape
    o_k = SB_WIDTH
    o_v = 2 * SB_WIDTH
    o_pool = 3 * SB_WIDTH
    o_gsb = o_pool + POOL_WIDTH
    o_gpool = o_gsb + D_MODEL
    for l in range(DEPTH):
        h = rmsnorm(x, norm_mix[l])
        proj = h @ w_in[l]
        to_heads = lambda t: t.reshape(B, S, SB_HEADS, SB_HEAD_DIM).transpose(0, 2, 1, 3)
        q = to_heads(proj[..., :o_k])
        k = to_heads(proj[..., o_k:o_v])
        v = to_heads(proj[..., o_v:o_pool])
        u_pool = proj[..., o_pool:o_gsb]
        g_sb = jax.nn.sigmoid(proj[..., o_gsb:o_gpool])
        g_pool = jax.nn.sigmoid(proj[..., o_gpool:])
        attn = stick_breaking_attention(q, k, v).transpose(0, 2, 1, 3).reshape(B, S, SB_WIDTH)
        y_sb = attn @ w_branch_sb[l]
        y_pool = multiscale_pool(u_pool, pool_group_w[l], pool_scale[l]) @ w_branch_pool[l]
        x = x + (g_sb * y_sb + g_pool * y_pool) @ w_out[l]
        mem_n = rmsnorm(mem, norm_mem_kv[l])
        x = x + memory_cross_attention(rmsnorm(x, norm_mem_q[l]), mem_n, xa_w_q[l], xa_w_kv[l], xa_w_o[l])
        x = x + peer_ffn(rmsnorm(x, norm_ffn[l]), peer_w_query[l], peer_sub_keys[l], peer_down[l], peer_up[l])
    return rmsnorm(x, norm_final)
```

```python
import contextlib
import numpy as np
import ml_dtypes
import concourse.bass as bass
import concourse.mybir as mybir
from concourse.bass_utils import run_bass_kernel_spmd

F32, BF16 = mybir.dt.float32, mybir.dt.bfloat16
AF = mybir.ActivationFunctionType
ALU = mybir.AluOpType
AX = mybir.AxisListType

NCORES = 8
EPS = 1e-6
NEG = -1.0e30


class Sem:
    def __init__(self, handle):
        self.h = handle
        self.count = 0


class Eng:
    def __init__(self, k, eng, name):
        self.k, self.e, self.name = k, eng, name
        self.sem = k.new_sem(name)
        self.seen = {}

    def wait(self, *toks):
        for t in toks:
            if t is None:
                continue
            if isinstance(t, (list, tuple)) and len(t) and not isinstance(t[0], Sem):
                self.wait(*t)
                continue
            s, v = t
            if self.seen.get(id(s), 0) >= v:
                continue
            self.e.wait_ge(s.h, v)
            self.seen[id(s)] = v

    def mark(self, ins):
        ins.then_inc(self.sem.h, 1)
        self.sem.count += 1
        return (self.sem, self.sem.count)


class K:
    def __init__(self, nc, stack, sem_stack=None):
        self.nc, self.stack = nc, stack
        self.sem_stack = sem_stack if sem_stack is not None else stack
        self._sems = []
        self.pe = Eng(self, nc.tensor, "pe")
        self.act = Eng(self, nc.scalar, "act")
        self.dve = Eng(self, nc.vector, "dve")
        self.pool = Eng(self, nc.gpsimd, "pool")
        self.sp = Eng(self, nc.sync, "sp")

    def new_sem(self, name):
        s = Sem(self.sem_stack.enter_context(self.nc.semaphore(name + "_%d" % len(self._sems))))
        self._sems.append(s)
        return s

    def sbuf(self, name, shape, dt):
        return self.stack.enter_context(self.nc.sbuf_tensor(name, shape, dt))

    def psum(self, name, shape, dt=F32):
        return self.stack.enter_context(self.nc.psum_tensor(name, shape, dt))

    def dma(self, q, out, in_, sem):
        ins = q.e.dma_start(out=out, in_=in_)
        ins.then_inc(sem.h, 16)
        sem.count += 16
        return (sem, sem.count)


def bc(ap, axis, shape):
    return ap.unsqueeze(axis).broadcast_to(list(shape))


def build_phase1(nc, k, S, D, T_in):
    HPC = 2
    KC = D // 128
    NT = S // 128
    xT, xin, wqkv, gains, cst, bias_tab, attn_scr = (T_in[n] for n in
                                                     ("xT", "xin", "wqkv", "gains", "cst", "bias_tab", "attn_scr"))
    NKB = (NT // 2, NT)
    with contextlib.ExitStack() as st:
        k.stack = st
        nc_ = nc
        pe, act, dve, pool, sp = k.pe, k.act, k.dve, k.pool, k.sp
        W = k.sbuf("W", [128, KC, 3 * HPC * 128], BF16)
        KT = k.sbuf("KT", [128, HPC, S], BF16)
        V = k.sbuf("V", [128, NT, HPC * 128], BF16)
        QT = [k.sbuf("QT%d" % i, [128, HPC, 512], BF16) for i in range(2)]
        xs = [k.sbuf("xs%d" % i, [128, KC, 128], F32) for i in range(2)]
        sq = k.sbuf("sq", [128, KC, 128], BF16)
        hb = k.sbuf("hb", [128, KC, 128], BF16)
        g_sb = k.sbuf("g1", [128, 6, KC], F32)
        c_sb = k.sbuf("c1", [128, 384], F32)
        ones_b = k.sbuf("ones1", [128, 128], BF16)
        nones_b = k.sbuf("nones", [128, 128], BF16)
        ntri_b = k.sbuf("ntri", [128, 128], BF16)
        ident_b = k.sbuf("ident1", [128, 128], BF16)
        rstd = k.sbuf("rstd", [128, 132], F32)
        ebuf = [k.sbuf("eb%d" % i, [128, 512], F32) for i in range(3)]
        spb = [k.sbuf("sp%d" % i, [128, 512], BF16) for i in range(3)]
        atb = [k.sbuf("at%d" % i, [128, 512], BF16) for i in range(3)]
        bias = [k.sbuf("bias%d" % i, [128, 512], BF16) for i in range(3)]
        spacc = k.sbuf("spacc", [128, HPC, 512], BF16)
        ostage = [k.sbuf("os%d" % i, [128, 512], BF16) for i in range(2)]
        pz = [k.psum("pz%d" % i, [128, 512]) for i in range(3)]
        po = [k.psum("po%d" % i, [128, 512]) for i in range(HPC)]
        pqk = k.psum("pqk", [128, 512])
        pv = k.psum("pv", [128, 512])

        s_c = k.new_sem("ldc")
        s_w = k.new_sem("ldw")
        s_x = [k.new_sem("ldx") for _ in range(2)]
        s_o = [k.new_sem("sto") for _ in range(2)]
        s_b = [k.new_sem("ldb") for _ in range(3)]

        k.dma(sp, g_sb[:], gains, s_c)
        t_c = k.dma(sp, c_sb[:], cst, s_c)
        dve.wait(t_c)
        nc.vector.memset(ones_b[:], 1.0)
        nc.vector.memset(nones_b[:], -1.0)
        nc.vector.tensor_copy(out=ntri_b[:], in_=c_sb[:, 128:256])
        t_cst = dve.mark(nc.vector.tensor_copy(out=ident_b[:], in_=c_sb[:, 256:384]))
        pe.wait(t_cst)
        act.wait(t_cst)
        pool.wait(t_cst)

        scale = 128.0 ** -0.5
        st8 = {"x_free": [None, None], "sq_free": None, "hb_free": None, "pqk_free": None, "pv_free": None,
               "rstd_free": None, "xctr": 0, "w_free": None, "kv_free": None, "last": None}
        qt_free = [None, None]

        def project(src_ap, mode, tt=None, slot=None, c0=None):
            d = st8
            sl = d["xctr"] % 2
            d["xctr"] += 1
            sp.wait(d["x_free"][sl])
            t_x = k.dma(sp, xs[sl][:], src_ap, s_x[sl])
            act.wait(t_x, d["sq_free"])
            t_sq = act.mark(nc.scalar.activation(out=sq[:], in_=xs[sl][:], func=AF.Square))
            pool.wait(t_x, d["hb_free"])
            t_hb = pool.mark(nc.gpsimd.tensor_tensor(out=hb[:], in0=xs[sl][:], in1=bc(g_sb[:, 0, :], 2, [128, KC, 128]),
                                                     op=ALU.mult))
            d["x_free"][sl] = [t_sq, t_hb]
            pe.wait(t_sq, d["pv_free"])
            for kc in range(KC):
                nc.tensor.matmul(pv[:, 256:384], lhsT=ones_b[:], rhs=sq[:, kc, :], start=(kc == 0), stop=(kc == KC - 1))
            for kc in range(KC):
                ins = nc.tensor.matmul(pv[:, 384:385], lhsT=sq[:, kc, :], rhs=ones_b[:, 0:1], start=(kc == 0),
                                       stop=(kc == KC - 1))
            t_ss = pe.mark(ins)
            d["sq_free"] = t_ss
            dve.wait(t_ss, d["rstd_free"])
            t_r00 = dve.mark(nc.vector.tensor_scalar(out=rstd[:, 0:129], in0=pv[:, 256:385], scalar1=1.0 / D, scalar2=EPS,
                                                     op0=ALU.mult, op1=ALU.add))
            act.wait(t_r00)
            t_r01 = act.mark(nc.scalar.activation(out=rstd[:, 0:129], in_=rstd[:, 0:129], func=AF.Ln))
            act.wait(t_r01)
            t_r0 = act.mark(nc.scalar.activation(out=rstd[:, 0:129], in_=rstd[:, 0:129], func=AF.Exp, scale=-0.5))
            pe.wait(t_hb, d["pqk_free"], d["w_ready"])
            blks = range(HPC, 2 * HPC) if mode == "kv" else range(0, HPC)
            for blk in blks:
                for kc in range(KC):
                    ins = nc.tensor.matmul(pqk[:, blk * 128:(blk + 1) * 128], lhsT=W[:, kc, blk * 128:(blk + 1) * 128],
                                           rhs=hb[:, kc, :], start=(kc == 0), stop=(kc == KC - 1))
            t_qk = pe.mark(ins)
            t_v = None
            if mode == "kv":
                for kc in range(KC):
                    ins = nc.tensor.matmul(pv[:, 0:HPC * 128], lhsT=hb[:, kc, :], rhs=W[:, kc, 2 * HPC * 128:3 * HPC * 128],
                                           start=(kc == 0), stop=(kc == KC - 1))
                t_v = pe.mark(ins)
            d["hb_free"] = t_v if t_v is not None else t_qk
            d["w_last"] = d["hb_free"]
            if mode == "kv":
                dve.wait(t_qk, t_r0, d["kv_free"])
                for hh in range(HPC):
                    ins = nc.vector.tensor_tensor(out=KT[:, hh, tt * 128:(tt + 1) * 128],
                                                  in0=pqk[:, (HPC + hh) * 128:(HPC + hh + 1) * 128], in1=rstd[:, 0:128],
                                                  op=ALU.mult)
                t_e1 = dve.mark(ins)
                act.wait(t_v, t_r0, d["kv_free"])
                t_e2 = act.mark(nc.scalar.activation(out=V[:, tt, :], in_=pv[:, 0:HPC * 128], func=AF.Copy,
                                                     scale=rstd[:, 128:129]))
                d["pv_free"] = [t_e2, t_r0]
                d["rstd_free"] = [t_e1, t_e2]
                d["last"] = [t_e1, t_e2]
                d["last_kv"] = [t_e1, t_e2]
            else:
                dve.wait(t_qk, t_r0, qt_free[slot])
                for hh in range(HPC):
                    ins = nc.vector.scalar_tensor_tensor(out=QT[slot][:, hh, c0:c0 + 128], in0=pqk[:, hh * 128:(hh + 1) * 128],
                                                         scalar=scale, in1=rstd[:, 0:128], op0=ALU.mult, op1=ALU.mult)
                t_e1 = dve.mark(ins)
                d["pv_free"] = [t_r0]
                d["rstd_free"] = [t_e1]
                d["last"] = [t_e1]
            d["pqk_free"] = t_e1

        it_ctr = [0]
        pz_free = [None] * 3
        sp_free = [None] * 3
        at_free = [None] * 3
        b_free = [None] * 3
        b_ctr = [0]
        po_free = [None] * HPC
        spacc_tok = [None] * HPC
        os_free = [None, None]
        o_ctr = [0]
        out_toks = []

        def attention(hp, slot):
            nkb = NKB[slot]
            qt = QT[slot]
            its = [(kb, hh) for kb in reversed(range(nkb)) for hh in range(HPC)]
            n = len(its)
            stt = [dict() for _ in range(n)]
            pe.wait(st8["last"], st8["last_kv"])
            pool.wait(st8["last"], st8["last_kv"])
            for hh in range(HPC):
                pool.wait(spacc_tok[hh])
            t_z = pool.mark(nc.gpsimd.memset(spacc[:], 0.0))
            for hh in range(HPC):
                spacc_tok[hh] = t_z
            last_av = [None] * HPC
            btile = {}

            def st0(i):
                kb, hh = its[i]
                b = (it_ctr[0] + i) % 3
                d = stt[i]
                d["b"] = b
                if hh == 0:
                    bs = b_ctr[0] % 3
                    b_ctr[0] += 1
                    sp.wait(b_free[bs])
                    btile[kb] = (bs, k.dma(sp, bias[bs][:], bias_tab[slot, kb], s_b[bs]))
                bs, t_b = btile[kb]
                pe.wait(pz_free[b], t_b)
                nc.tensor.matmul(pz[b][:, :], lhsT=KT[:, hh, kb * 128:(kb + 1) * 128], rhs=qt[:, hh, :], start=True, stop=False)
                d["z"] = pe.mark(nc.tensor.matmul(pz[b][:, :], lhsT=ident_b[:], rhs=bias[bs][:], start=False, stop=False))
                if hh == HPC - 1:
                    b_free[bs] = d["z"]
                act.wait(d["z"], sp_free[b])
                t_e = act.mark(nc.scalar.activation(out=ebuf[b][:], in_=pz[b][:, :], func=AF.Exp))
                act.wait(t_e)
                d["ln"] = act.mark(nc.scalar.activation(out=spb[b][:], in_=ebuf[b][:], func=AF.Ln, bias=1.0))

            def st1(i):
                kb, hh = its[i]
                d = stt[i]
                b = d["b"]
                first = (kb == nkb - 1)
                pe.wait(d["ln"], spacc_tok[hh])
                ins = nc.tensor.matmul(pz[b][:, :], lhsT=ntri_b[:], rhs=spb[b][:], start=False, stop=first)
                if not first:
                    ins = nc.tensor.matmul(pz[b][:, :], lhsT=nones_b[:], rhs=spacc[:, hh, :], start=False, stop=True)
                d["cs"] = pe.mark(ins)
                act.wait(d["cs"], at_free[b])
                d["ea"] = act.mark(nc.scalar.activation(out=atb[b][:], in_=pz[b][:, :], func=AF.Exp))
                pz_free[b] = d["ea"]
                pool.wait(d["cs"], d["ln"], spacc_tok[hh])
                spacc_tok[hh] = pool.mark(nc.gpsimd.tensor_tensor(out=spacc[:, hh, :], in0=spacc[:, hh, :], in1=spb[b][:],
                                                                  op=ALU.add))
                sp_free[b] = spacc_tok[hh]

            def st2(i):
                kb, hh = its[i]
                d = stt[i]
                b = d["b"]
                pe.wait(d["ea"], po_free[hh] if kb == nkb - 1 else None)
                d["av"] = pe.mark(nc.tensor.matmul(po[hh][:, :], lhsT=V[:, kb, hh * 128:(hh + 1) * 128], rhs=atb[b][:],
                                                   start=(kb == nkb - 1), stop=(kb == 0)))
                at_free[b] = d["av"]
                last_av[hh] = d["av"]

            for step in range(n + 2):
                if step < n:
                    st0(step)
                if 0 <= step - 1 < n:
                    st1(step - 1)
                if 0 <= step - 2 < n:
                    st2(step - 2)
            it_ctr[0] += n
            qt_free[slot] = [last_av[hh] for hh in range(HPC)]
            st8["kv_free"] = [last_av[hh] for hh in range(HPC)]
            for hh in range(HPC):
                o = o_ctr[0] % 2
                o_ctr[0] += 1
                act.wait(last_av[hh], os_free[o])
                t_cp = act.mark(nc.scalar.copy(out=ostage[o][:], in_=po[hh][:, :]))
                po_free[hh] = t_cp
                sp.wait(t_cp)
                h = hp * HPC + hh
                os_free[o] = k.dma(sp, attn_scr[h * 128:(h + 1) * 128, slot * 512:(slot + 1) * 512], ostage[o][:], s_o[o])
                out_toks.append(os_free[o])

        st8["w_ready"] = None
        st8["w_last"] = None
        for hp in range(8):
            pool.wait(st8["w_last"], st8["kv_free"])
            for i in range(3 * HPC):
                t_w = k.dma(pool, W[:, :, i * 128:(i + 1) * 128], wqkv[hp][:, :, i * 128:(i + 1) * 128], s_w)
            st8["w_ready"] = t_w
            for tt in range(NT):
                project(xT[tt], "kv", tt=tt)
            for j in range(8):
                slot, c0 = j // 4, (j % 4) * 128
                project(xin[slot][:, :, c0:c0 + 128].rearrange("kc q t -> q kc t"), "q", slot=slot, c0=c0)
            for slot in range(2):
                attention(hp, slot)
        fin = [(e.sem, e.sem.count) for e in (pe, act, dve, pool)] + [(s_, s_.count) for s_ in s_o]
        for e in (pe, act, dve, pool, sp):
            e.wait(fin)
    return nc


def host_phase1_inputs(x, w_in, norm_mix, S, D, HPC, ncores):
    KC = D // 128
    NT = S // 128
    SBW = ncores * HPC * 128
    xT = np.ascontiguousarray(x.reshape(NT, 128, KC, 128).transpose(0, 3, 2, 1))
    g = np.ascontiguousarray(norm_mix.reshape(KC, 128).T)
    s_idx = np.arange(128)
    cst = np.zeros((128, 384), np.float32)
    cst[:, 0:128] = (s_idx[:, None] < s_idx[None, :])
    cst[:, 128:256] = -1.0 * (s_idx[:, None] >= s_idx[None, :])
    cst[:, 256:384] = np.eye(128)
    maps = []
    for c in range(ncores):
        cols = []
        for part in range(3):
            for hh in range(HPC):
                h = c * HPC + hh
                cols.append(w_in[:, part * SBW + h * 128: part * SBW + (h + 1) * 128])
        w = np.concatenate(cols, axis=1)
        w = np.ascontiguousarray(w.reshape(KC, 128, -1).transpose(1, 0, 2))
        maps.append({"xT": xT, "wqkv": w, "gmix": g, "cst": cst})
    return maps


def build_phase2(nc, k, TOK, D, T_in):
    KC = D // 128
    NP = TOK // 512
    PC = (D // 2) // 128
    CPG = PC // 4
    NE = 128
    T = 512
    (xin, xhalo, invc, attn_in, memT, gains, cst, keysT, w_pool, w_grp, w_gsb, w_gpool, w_bsb, w_bpool, w_out, w_xq, w_xkv,
     w_xo, w_pq, w_down, w_up, outT) = (T_in[n] for n in (
        "xin", "xhalo", "invc", "attn_scr", "memT", "gains", "cst", "keysT", "w_pool", "w_grp", "w_gsb", "w_gpool", "w_bsb",
        "w_bpool", "w_out", "w_xq", "w_xkv", "w_xo", "w_pq", "w_down", "w_up", "outT"))
    xr1 = nc.dram_tensor("xr1", [KC, 128, T], F32).ap()
    xr2 = nc.dram_tensor("xr2", [KC, 128, T], F32).ap()
    xr3 = nc.dram_tensor("xr3", [KC, 128, T], F32).ap()

    with contextlib.ExitStack() as st:
        k.stack = st
        pe, act, dve, pool, sp = k.pe, k.act, k.dve, k.pool, k.sp
        KW = max(KC, 32)
        hT = k.sbuf("hT", [128, KC, T], BF16)
        MT = k.sbuf("MT", [128, 32, T], BF16)
        aux = k.sbuf("aux", [128, 16, T], BF16)
        wsl = [k.sbuf("ws%d" % i, [128, KW, 128], BF16) for i in range(3)]
        xc = [k.sbuf("xc%d" % i, [128, T], F32) for i in range(3)]
        sqc = [k.sbuf("sqc%d" % i, [128, T], BF16) for i in range(2)]
        ft = [k.sbuf("ft%d" % i, [128, T], F32) for i in range(4)]
        bt = [k.sbuf("bt%d" % i, [128, T], BF16) for i in range(3)]
        rs = k.sbuf("rs", [128, T], F32)
        rsh = k.sbuf("rsh", [128, 16], F32)
        rsm = k.sbuf("rsm", [128, 260], F32)
        g_sb = k.sbuf("g", [128, 6, KC], F32)
        c_sb = k.sbuf("c", [128, 384], F32)
        ones_b = k.sbuf("ones", [128, 128], BF16)
        ident_b = k.sbuf("identb", [128, 128], BF16)
        keys_b = k.sbuf("keysb", [128, 16, 128], BF16)
        xh = k.sbuf("xh", [128, KC, 16], F32)
        sqh = k.sbuf("sqh", [128, KC, 16], BF16)
        hh = k.sbuf("hh", [128, KC, 16], BF16)
        ubuf = k.sbuf("ubuf", [128, 528], F32)
        ra = k.sbuf("ra", [128, 528], F32)
        rb = k.sbuf("rb", [128, 528], F32)
        icv = k.sbuf("icv", [128, 4, 16], F32)
        t16 = k.sbuf("t16", [128, 16], F32)
        kmT = k.sbuf("kmT", [128, 4, 256], BF16)
        vm = k.sbuf("vm", [128, 2, 512], BF16)
        qx = k.sbuf("qx", [128, 4, T], BF16)
        ox = k.sbuf("ox", [128, 4, T], BF16)
        pT = k.sbuf("pT", [128, 2, T], BF16)
        top = k.sbuf("top", [128, 16, 16], F32)
        best = k.sbuf("best", [128, 8, 16], F32)
        zz = k.sbuf("zz", [128, 8, 4], F32)
        tau = k.sbuf("tau", [128, 4, 8], F32)
        negb = k.sbuf("negb", [128, 4, 8], F32)
        krep = k.sbuf("krep", [128, 2, 2, 256], BF16)
        ps = [k.psum("ps%d" % i, [128, 512]) for i in range(8)]
        A, B, SS, X = ps[0:2], ps[2:4], ps[4], ps[5:8]

        s_c = k.new_sem("ldc")
        s_w = [k.new_sem("ldw") for _ in range(3)]
        s_x = [k.new_sem("ldx") for _ in range(3)]
        s_m = k.new_sem("misc")

        k.dma(sp, g_sb[:], gains, s_c)
        t_kb = k.dma(pool, keys_b[:], keysT, s_w[0])
        t_c = k.dma(sp, c_sb[:], cst, s_c)
        dve.wait(t_c)
        nc.vector.memset(ones_b[:], 1.0)
        dve.wait(t_kb)
        t_cst = dve.mark(nc.vector.tensor_copy(out=ident_b[:], in_=c_sb[:, 256:384]))
        pe.wait(t_cst)
        act.wait(t_cst)

        wctr = [0]
        w_free = [None] * 3
        free = {}

        def fr(name):
            return free.get(name)

        def load_w(tile_ap, KCw):
            s = wctr[0] % 3
            wctr[0] += 1
            pool.wait(w_free[s])
            t = k.dma(pool, wsl[s][:, 0:KCw, :], tile_ap, s_w[s])
            return s, t

        def lin(tile_ap, KCw, outs, extra_wait=None):
            s, t = load_w(tile_ap, KCw)
            pe.wait(t, extra_wait)
            ins = None
            for (pap, rf) in outs:
                for kc in range(KCw):
                    ins = nc.tensor.matmul(pap, lhsT=wsl[s][:, kc, :], rhs=rf(kc), start=(kc == 0), stop=(kc == KCw - 1))
            tok = pe.mark(ins)
            w_free[s] = tok
            return tok

        xctr = [0]
        x_free = [None] * 3

        def load_x(src_ap, extra=None, n=T):
            s = xctr[0] % 3
            xctr[0] += 1
            sp.wait(x_free[s], extra)
            t = k.dma(sp, xc[s][:, 0:n], src_ap, s_x[s])
            return s, t

        sqctr = [0]
        sq_free = [None] * 2

        def stats_acc(src_ap, src_tok, kc, n=T):
            s = sqctr[0] % 2
            sqctr[0] += 1
            act.wait(src_tok, sq_free[s])
            t = act.mark(nc.scalar.activation(out=sqc[s][:, 0:n], in_=src_ap, func=AF.Square))
            pe.wait(t, fr("SS") if kc == 0 else None)
            tp = pe.mark(nc.tensor.matmul(SS[:, 0:n], lhsT=ones_b[:], rhs=sqc[s][:, 0:n], start=(kc == 0),
                                          stop=(kc == KC - 1)))
            sq_free[s] = tp
            return t, tp

        def make_rstd(dst_ap, src_ps_ap, tok, guard=None):
            dve.wait(tok, guard)
            t0 = dve.mark(nc.vector.tensor_scalar(out=dst_ap, in0=src_ps_ap, scalar1=1.0 / D, scalar2=EPS, op0=ALU.mult,
                                                  op1=ALU.add))
            act.wait(t0)
            t1 = act.mark(nc.scalar.activation(out=dst_ap, in_=dst_ap, func=AF.Ln))
            act.wait(t1)
            t2 = act.mark(nc.scalar.activation(out=dst_ap, in_=dst_ap, func=AF.Exp, scale=-0.5))
            return t2, t0

        abctr = [0]

        def nextAB():
            i = abctr[0] % 2
            abctr[0] += 1
            return i

        ab_free = {("A", 0): None, ("A", 1): None, ("B", 0): None, ("B", 1): None}

        hm = MT
        t_hm = None
        for kc in range(KC):
            s, t = load_x(memT[kc], n=256)
            t_sq, t_pe = stats_acc(xc[s][:, 0:256], t, kc, n=256)
            sqs = sqc[(sqctr[0] - 1) % 2]
            for mc in range(2):
                t_pe = pe.mark(nc.tensor.matmul(X[mc][:, 0:1], lhsT=sqs[:, mc * 128:(mc + 1) * 128],
                                                rhs=ones_b[:, 0:1], start=(kc == 0), stop=(kc == KC - 1)))
            sq_free[(sqctr[0] - 1) % 2] = t_pe
            dve.wait(t)
            t_hm = dve.mark(nc.vector.tensor_scalar(out=hm[:, kc, 0:256], in0=xc[s][:, 0:256], scalar1=g_sb[:, 2, kc:kc + 1],
                                                    scalar2=None, op0=ALU.mult))
            x_free[s] = [t_sq, t_hm]
        t_rsm, t_rsm0 = make_rstd(rsm[:, 0:256], SS[:, 0:256], t_pe)
        free["SS"] = t_rsm0
        for mc in range(2):
            t_rsm, t_x0 = make_rstd(rsm[:, 256 + mc:257 + mc], X[mc][:, 0:1], t_pe)
            free["X%d" % mc] = t_x0
        pe.wait(t_hm)
        for hd in range(4):
            a = nextAB()
            t_mm = lin(w_xkv[hd], KC, [(A[a][:, 0:256], lambda kc: hm[:, kc, 0:256])], extra_wait=ab_free[("A", a)])
            dve.wait(t_mm, t_rsm)
            ab_free[("A", a)] = dve.mark(nc.vector.tensor_tensor(out=kmT[:, hd, :], in0=A[a][:, 0:256], in1=rsm[:, 0:256],
                                                                 op=ALU.mult))
        for blk in range(4):
            s, t = load_w(w_xkv[4 + blk], KC)
            a = nextAB()
            pe.wait(t, ab_free[("A", a)])
            for mc in range(2):
                for kc in range(KC):
                    ins = nc.tensor.matmul(A[a][:, mc * 128:(mc + 1) * 128], lhsT=hm[:, kc, mc * 128:(mc + 1) * 128],
                                           rhs=wsl[s][:, kc, :], start=(kc == 0), stop=(kc == KC - 1))
            t_mm = pe.mark(ins)
            w_free[s] = t_mm
            act.wait(t_mm, t_rsm)
            for mc in range(2):
                t_e = act.mark(nc.scalar.activation(out=vm[:, mc, blk * 128:(blk + 1) * 128],
                                                    in_=A[a][:, mc * 128:(mc + 1) * 128], func=AF.Copy,
                                                    scale=rsm[:, 256 + mc:257 + mc]))
            ab_free[("A", a)] = t_e
        t_mem_done = [ab_free[("A", 0)], ab_free[("A", 1)]]
        free["MT"] = t_mm

        xa_scale = 128.0 ** -0.5
        st_tok = {}

        def residual_stage(p, n_blocks, w_tiles, KCw, rhs_fn, src_fn, dst, gain_idx, pre_wait=None, final=False):
            t_pe_last = None
            t_h = None
            for nb in range(n_blocks):
                a = nextAB()
                t_mm = lin(w_tiles[nb], KCw, [(A[a][:, :], rhs_fn)], extra_wait=[ab_free[("A", a)], pre_wait])
                s, t_x = load_x(src_fn(nb), extra=st_tok.get(("src", id(src_fn), nb)))
                dve.wait(t_mm, t_x)
                t_add = dve.mark(nc.vector.tensor_tensor(out=xc[s][:], in0=A[a][:, :], in1=xc[s][:], op=ALU.add))
                ab_free[("A", a)] = t_add
                sp.wait(t_add)
                t_st = k.dma(sp, dst[nb], xc[s][:], s_m)
                st_tok[(id(dst), nb)] = t_st
                t_sq, t_pe_last = stats_acc(xc[s][:], t_add, nb)
                if gain_idx is not None:
                    dve.wait(fr("hT"))
                    t_h = dve.mark(nc.vector.tensor_scalar(out=hT[:, nb, :], in0=xc[s][:], scalar1=g_sb[:, gain_idx, nb:nb + 1],
                                                           scalar2=None, op0=ALU.mult))
                    x_free[s] = [t_st, t_sq, t_h]
                else:
                    x_free[s] = [t_st, t_sq]
            t_rs, t_rs0 = make_rstd(rs[:], SS[:, :], t_pe_last, guard=fr("rs"))
            free["SS"] = t_rs0
            return t_rs, t_h

        for p in range(NP):
            tc0 = p * T
            dve.wait(fr("hT"))
            for kc in range(KC):
                s, t = load_x(xin[p, kc])
                t_sq, t_pe = stats_acc(xc[s][:], t, kc)
                dve.wait(t)
                t_h = dve.mark(nc.vector.tensor_scalar(out=hT[:, kc, :], in0=xc[s][:], scalar1=g_sb[:, 0, kc:kc + 1],
                                                       scalar2=None, op0=ALU.mult))
                x_free[s] = [t_sq, t_h]
            t_rs, t_rs0 = make_rstd(rs[:], SS[:, :], t_pe, guard=fr("rs"))
            free["SS"] = t_rs0
            sp.wait(fr("xh"))
            k.dma(sp, icv[:], invc[p], s_m)
            t_xh = k.dma(sp, xh[:], xhalo[p], s_m)
            act.wait(t_xh, fr("sqh"))
            t_sqh = act.mark(nc.scalar.activation(out=sqh[:], in_=xh[:], func=AF.Square))
            dve.wait(t_xh, fr("hh"))
            t_hh = dve.mark(nc.vector.tensor_tensor(out=hh[:], in0=xh[:], in1=bc(g_sb[:, 0, :], 2, [128, KC, 16]), op=ALU.mult))
            free["xh"] = [t_sqh, t_hh]
            pe.wait(t_sqh, fr("SS"))
            for kc in range(KC):
                ins = nc.tensor.matmul(SS[:, 0:16], lhsT=ones_b[:], rhs=sqh[:, kc, :], start=(kc == 0), stop=(kc == KC - 1))
            t_ssh = pe.mark(ins)
            free["sqh"] = t_ssh
            t_rsh, t_rsh0 = make_rstd(rsh[:], SS[:, 0:16], t_ssh, guard=fr("rsh"))
            free["SS"] = t_rsh0
            pe.wait(t_h, t_hh)

            pooledT = MT
            dve.wait(fr("MT"))
            t_pool = None
            for nb in range(PC):
                g = nb // CPG
                a = nextAB()
                t_mm = lin(w_pool[nb], KC, [(A[a][:, :], lambda kc: hT[:, kc, :]), (B[a][:, 0:16], lambda kc: hh[:, kc, :])],
                           extra_wait=[ab_free[("A", a)], ab_free[("B", a)]])
                dve.wait(t_mm, t_rs, t_rsh, t_pool)
                nc.vector.tensor_tensor(out=ubuf[:, 16:528], in0=A[a][:, :], in1=rs[:], op=ALU.mult)
                t_u = dve.mark(nc.vector.tensor_tensor(out=ubuf[:, 0:16], in0=B[a][:, 0:16], in1=rsh[:], op=ALU.mult))
                ab_free[("A", a)] = t_u
                ab_free[("B", a)] = t_u
                dve.wait(t_u)
                t_r = dve.mark(nc.vector.tensor_tensor(out=ra[:, 1:528], in0=ubuf[:, 1:528], in1=ubuf[:, 0:527], op=ALU.add))
                r = ra
                if g >= 1:
                    dve.wait(t_r)
                    t_r = dve.mark(nc.vector.tensor_tensor(out=rb[:, 3:528], in0=ra[:, 3:528], in1=ra[:, 1:526], op=ALU.add))
                    r = rb
                if g >= 2:
                    dve.wait(t_r)
                    t_r = dve.mark(nc.vector.tensor_tensor(out=ra[:, 7:528], in0=rb[:, 7:528], in1=rb[:, 3:524], op=ALU.add))
                    r = ra
                if g >= 3:
                    dve.wait(t_r)
                    t_r = dve.mark(nc.vector.tensor_tensor(out=rb[:, 15:528], in0=ra[:, 15:528], in1=ra[:, 7:520], op=ALU.add))
                    r = rb
                w = (2, 4, 8, 16)[g]
                dve.wait(t_r)
                nc.vector.scalar_tensor_tensor(out=pooledT[:, nb, 16:512], in0=r[:, 32:528], scalar=1.0 / w,
                                               in1=ubuf[:, 32:528], op0=ALU.mult, op1=ALU.subtract)
                t_a = dve.mark(nc.vector.tensor_tensor(out=t16[:], in0=r[:, 16:32], in1=icv[:, g, :], op=ALU.mult))
                dve.wait(t_a)
                t_pool = dve.mark(nc.vector.tensor_tensor(out=pooledT[:, nb, 0:16], in0=t16[:], in1=ubuf[:, 16:32],
                                                          op=ALU.subtract))
            mixedT = aux
            pe.wait(t_pool)
            act.wait(fr("aux"))
            for nb in range(PC):
                g = nb // CPG
                a = nextAB()
                t_mm = lin(w_grp[nb], CPG, [(A[a][:, :], lambda kc, g=g: pooledT[:, g * CPG + kc, :])],
                           extra_wait=ab_free[("A", a)])
                act.wait(t_mm)
                t_mx = act.mark(nc.scalar.activation(out=mixedT[:, nb, :], in_=A[a][:, :], func=AF.Copy,
                                                     scale=g_sb[:, 5, nb:nb + 1]))
                ab_free[("A", a)] = t_mx
            pe.wait(t_mx)
            dve.wait(t_mm)
            for nb in range(KC):
                a = nextAB()
                t_a = lin(w_bpool[nb], PC, [(A[a][:, :], lambda kc: mixedT[:, kc, :])], extra_wait=ab_free[("A", a)])
                t_b = lin(w_gpool[nb], KC, [(B[a][:, :], lambda kc: hT[:, kc, :])], extra_wait=ab_free[("B", a)])
                f = ft[nb % 2]
                dve.wait(t_b, fr(("ft", nb % 2)))
                t_g = dve.mark(nc.vector.tensor_tensor(out=f[:], in0=B[a][:, :], in1=rs[:], op=ALU.mult))
                ab_free[("B", a)] = t_g
                act.wait(t_g)
                t_s = act.mark(nc.scalar.activation(out=f[:], in_=f[:], func=AF.Sigmoid))
                dve.wait(t_s, t_a)
                t_m = dve.mark(nc.vector.tensor_tensor(out=MT[:, nb, :], in0=f[:], in1=A[a][:, :], op=ALU.mult))
                ab_free[("A", a)] = t_m
                free[("ft", nb % 2)] = t_m
            sp.wait(t_a)
            t_at = k.dma(sp, aux[:], attn_in.rearrange("(h q) t -> q h t", q=128)[:, :, tc0:tc0 + T], s_m)
            pe.wait(t_at)
            for nb in range(KC):
                a = nextAB()
                t_a = lin(w_bsb[nb], 16, [(A[a][:, :], lambda kc: aux[:, kc, :])], extra_wait=ab_free[("A", a)])
                t_b = lin(w_gsb[nb], KC, [(B[a][:, :], lambda kc: hT[:, kc, :])], extra_wait=ab_free[("B", a)])
                f = ft[nb % 2]
                dve.wait(t_b, fr(("ft", nb % 2)))
                t_g = dve.mark(nc.vector.tensor_tensor(out=f[:], in0=B[a][:, :], in1=rs[:], op=ALU.mult))
                ab_free[("B", a)] = t_g
                act.wait(t_g)
                t_s = act.mark(nc.scalar.activation(out=f[:], in_=f[:], func=AF.Sigmoid))
                dve.wait(t_s, t_a)
                t_m0 = dve.mark(nc.vector.tensor_tensor(out=f[:], in0=f[:], in1=A[a][:, :], op=ALU.mult))
                ab_free[("A", a)] = t_m0
                dve.wait(t_m0)
                t_m = dve.mark(nc.vector.tensor_tensor(out=MT[:, nb, :], in0=MT[:, nb, :], in1=f[:], op=ALU.add))
                free[("ft", nb % 2)] = t_m
            free["aux"] = t_a
            free["hT"] = t_b
            free["rs"] = t_g
            pe.wait(t_m)
            t_rs, t_h = residual_stage(p, KC, w_out, KC, lambda kc: MT[:, kc, :], lambda nb: xin[p, nb], xr1, 1)
            pe.wait(t_h)
            for hd in range(4):
                a = nextAB()
                t_mm = lin(w_xq[hd], KC, [(A[a][:, :], lambda kc: hT[:, kc, :])], extra_wait=ab_free[("A", a)])
                dve.wait(t_mm, t_rs, fr("qx"))
                t_q = dve.mark(nc.vector.scalar_tensor_tensor(out=qx[:, hd, :], in0=A[a][:, :], scalar=xa_scale, in1=rs[:],
                                                              op0=ALU.mult, op1=ALU.mult))
                ab_free[("A", a)] = t_q
            free["hT"] = t_mm
            free["rs"] = t_q
            pe.wait(t_q, t_mem_done)
            for hd in range(4):
                pe.wait(fr("X0"), fr("X1"))
                for mc in range(2):
                    ins = nc.tensor.matmul(X[mc][:, :], lhsT=kmT[:, hd, mc * 128:(mc + 1) * 128], rhs=qx[:, hd, :], start=True,
                                           stop=True)
                t_sc = pe.mark(ins)
                act.wait(t_sc, fr("pT"))
                for mc in range(2):
                    t_p = act.mark(nc.scalar.activation(out=pT[:, mc, :], in_=X[mc][:, :], func=AF.Exp))
                free["X0"] = t_p
                free["X1"] = t_p
                a = nextAB()
                pe.wait(t_p, ab_free[("A", a)], ab_free[("B", a)])
                for mc in range(2):
                    nc.tensor.matmul(B[a][:, :], lhsT=ones_b[:], rhs=pT[:, mc, :], start=(mc == 0), stop=(mc == 1))
                for mc in range(2):
                    ins = nc.tensor.matmul(A[a][:, :], lhsT=vm[:, mc, hd * 128:(hd + 1) * 128], rhs=pT[:, mc, :],
                                           start=(mc == 0), stop=(mc == 1))
                t_o = pe.mark(ins)
                free["pT"] = t_o
                f = ft[hd % 2]
                dve.wait(t_o, fr(("ft", hd % 2)), fr("ox"))
                t_rd = dve.mark(nc.vector.reciprocal(out=f[:], in_=B[a][:, :]))
                dve.wait(t_rd)
                t_ox = dve.mark(nc.vector.tensor_tensor(out=ox[:, hd, :], in0=A[a][:, :], in1=f[:], op=ALU.mult))
                ab_free[("A", a)] = t_ox
                ab_free[("B", a)] = t_ox
                free[("ft", hd % 2)] = t_ox
            free["qx"] = t_sc
            pe.wait(t_ox)
            src1 = lambda nb: xr1[nb]
            for nb in range(KC):
                st_tok[("src", id(src1), nb)] = st_tok[(id(xr1), nb)]
            t_rs, t_h = residual_stage(p, KC, w_xo, 4, lambda kc: ox[:, kc, :], src1, xr2, 3)
            free["ox"] = w_free[(wctr[0] - 1) % 3]
            qp = aux
            pe.wait(t_h)
            dve.wait(fr("aux"))
            for nb in range(16):
                a = nextAB()
                t_mm = lin(w_pq[nb], KC, [(A[a][:, :], lambda kc: hT[:, kc, :])], extra_wait=ab_free[("A", a)])
                dve.wait(t_mm, t_rs)
                t_q = dve.mark(nc.vector.tensor_tensor(out=qp[:, nb, :], in0=A[a][:, :], in1=rs[:], op=ALU.mult))
                ab_free[("A", a)] = t_q
            MTf = MT[:].rearrange("q a b -> q (a b)").bitcast(F32)
            sc = MTf[:, 0:2048]
            sc2 = MTf[:, 2048:4096]
            cand = MTf[:, 4096:6144]
            pe.wait(t_q)
            dve.wait(fr("MT"), t_m)
            act.wait(t_m)
            t_tk = None
            for tt in range(4):
                tsl = slice(tt * 128, (tt + 1) * 128)
                pe.wait(fr("X0"), fr("X1"), fr("X2"), fr("SS"))
                banks = [X[0], X[1], X[2], SS]
                for l in range(16):
                    ins = nc.tensor.matmul(banks[l // 4][:, (l % 4) * 128:(l % 4 + 1) * 128], lhsT=qp[:, l, tsl],
                                           rhs=keys_b[:, l, :], start=True, stop=True)
                t_s = pe.mark(ins)
                act.wait(t_s, t_tk)
                for bi in range(4):
                    t_cp = act.mark(nc.scalar.copy(out=sc[:, bi * 512:(bi + 1) * 512], in_=banks[bi][:, :]))
                for nm in ("X0", "X1", "X2", "SS"):
                    free[nm] = t_cp
                dve.wait(t_cp)
                for l in range(16):
                    row = sc[:, l * 128:(l + 1) * 128]
                    row2 = sc2[:, l * 128:(l + 1) * 128]
                    t1 = dve.mark(nc.vector.max(out=top[:, l, 0:8], in_=row))
                    dve.wait(t1)
                    t2 = dve.mark(nc.vector.match_replace(out=row2, in_to_replace=top[:, l, 0:8], in_values=row, imm_value=NEG))
                    dve.wait(t2)
                    t3 = dve.mark(nc.vector.max(out=top[:, l, 8:16], in_=row2))
                dve.wait(t3)
                top4 = top[:].rearrange("q (h two) a -> q h two a", two=2)
                cand4 = cand[:, 0:2048].rearrange("q (h a b) -> q h a b", h=8, a=16)
                t_cd = dve.mark(nc.vector.tensor_tensor(out=cand4, in0=bc(top4[:, :, 0, :], 3, [128, 8, 16, 16]),
                                                        in1=bc(top4[:, :, 1, :], 2, [128, 8, 16, 16]), op=ALU.add))
                dve.wait(t_cd)
                for h in range(8):
                    row = cand[:, h * 256:(h + 1) * 256]
                    row2 = sc2[:, h * 256:(h + 1) * 256]
                    t1 = dve.mark(nc.vector.max(out=best[:, h, 0:8], in_=row))
                    dve.wait(t1)
                    t2 = dve.mark(nc.vector.match_replace(out=row2, in_to_replace=best[:, h, 0:8], in_values=row, imm_value=NEG))
                    dve.wait(t2)
                    t3 = dve.mark(nc.vector.max(out=best[:, h, 8:16], in_=row2))
                dve.wait(t3)
                dd = sc2[:, 0:128].rearrange("q (h a) -> q h a", h=8)
                t_d = dve.mark(nc.vector.tensor_tensor(out=dd, in0=best[:], in1=bc(best[:, :, 0], 2, [128, 8, 16]),
                                                       op=ALU.subtract))
                act.wait(t_d)
                t_e = act.mark(nc.scalar.activation(out=dd, in_=dd, func=AF.Exp))
                dve.wait(t_e)
                t_z = dve.mark(nc.vector.reduce_sum(out=zz[:, :, 0], in_=dd, axis=AX.X))
                act.wait(t_z)
                t_lz = act.mark(nc.scalar.activation(out=zz[:, :, 1], in_=zz[:, :, 0], func=AF.Ln))
                dve.wait(t_lz)
                nc.vector.scalar_tensor_tensor(out=negb[:, tt, :], in0=best[:, :, 0], scalar=-1.0, in1=zz[:, :, 1],
                                               op0=ALU.mult, op1=ALU.subtract)
                t_tk = dve.mark(nc.vector.tensor_scalar(out=tau[:, tt, :], in0=best[:, :, 15], scalar1=-2e-5, scalar2=None,
                                                        op0=ALU.add))
            act.wait(t_tk)
            WT = MT
            gctr = 0
            free[("S", 0)] = fr("X0")
            free[("S", 1)] = fr("SS")
            free["Gs"] = [fr("X1"), fr("X2")]
            for qtr in range(4):
                for grp in range(16):
                    i0 = qtr * 32 + grp * 2
                    Gs = [X[1], X[2]]
                    for tt in range(4):
                        tsl = slice(tt * 128, (tt + 1) * 128)
                        for h in range(8):
                            it = gctr
                            gctr += 1
                            sb_ = it % 2
                            dve.wait(fr(("krep", sb_)))
                            t_k = dve.mark(nc.vector.tensor_copy(
                                out=krep[:, sb_, 0, :].rearrange("q (i j) -> q i j", i=2),
                                in_=bc(keys_b[:, 2 * h, i0:i0 + 2], 2, [128, 2, 128])))
                            t_k = dve.mark(nc.vector.tensor_copy(
                                out=krep[:, sb_, 1, :].rearrange("q (i j) -> q i j", i=2),
                                in_=bc(keys_b[:, 2 * h + 1, :], 1, [128, 2, 128])))
                            sbank = [X[0], SS][sb_]
                            pe.wait(t_k, fr(("S", sb_)))
                            nc.tensor.matmul(sbank[:, 0:256], lhsT=qp[:, 2 * h, tsl], rhs=krep[:, sb_, 0, :], start=True, stop=False)
                            t_S = pe.mark(nc.tensor.matmul(sbank[:, 0:256], lhsT=qp[:, 2 * h + 1, tsl], rhs=krep[:, sb_, 1, :],
                                                           start=False, stop=True))
                            free[("krep", sb_)] = t_S
                            e_t = ft[2 + sb_]
                            act.wait(t_S, fr(("et", sb_)))
                            t_E = act.mark(nc.scalar.activation(out=e_t[:, 0:256], in_=sbank[:, 0:256], func=AF.Exp,
                                                                bias=negb[:, tt, h:h + 1]))
                            g_t = bt[it % 3]
                            dve.wait(t_E, fr(("gt", it % 3)))
                            t_G = dve.mark(nc.vector.scalar_tensor_tensor(out=g_t[:, 0:256], in0=sbank[:, 0:256],
                                                                          scalar=tau[:, tt, h:h + 1], in1=e_t[:, 0:256],
                                                                          op0=ALU.is_ge, op1=ALU.mult))
                            free[("S", sb_)] = t_G
                            free[("et", sb_)] = t_G
                            pe.wait(t_G, fr("Gs") if (tt == 0 and h == 0) else None)
                            for ib in range(2):
                                ins = nc.tensor.matmul(Gs[ib][:, tsl], lhsT=g_t[:, ib * 128:(ib + 1) * 128], rhs=ident_b[:],
                                                       start=(h == 0), stop=(h == 7))
                            free[("gt", it % 3)] = pe.mark(ins)
                    t_Gs = free[("gt", (gctr - 1) % 3)]
                    for ib in range(2):
                        eb = i0 + ib
                        ebl = eb - qtr * 32
                        a = nextAB()
                        t_mm = lin(w_down[eb], KC, [(A[a][:, :], lambda kc: hT[:, kc, :])], extra_wait=ab_free[("A", a)])
                        f = ft[ib]
                        dve.wait(t_mm, t_rs, fr(("ft", ib)))
                        t_a = dve.mark(nc.vector.tensor_tensor(out=f[:], in0=A[a][:, :], in1=rs[:], op=ALU.mult))
                        ab_free[("A", a)] = t_a
                        act.wait(t_a)
                        t_ge = act.mark(nc.scalar.activation(out=f[:], in_=f[:], func=AF.Gelu))
                        dve.wait(t_ge, t_Gs, fr("WT"))
                        t_w = dve.mark(nc.vector.tensor_tensor(out=WT[:, ebl, :], in0=f[:], in1=Gs[ib][:, :], op=ALU.mult))
                        free[("ft", ib)] = t_w
                    free["Gs"] = t_w
                pe.wait(t_w)
                free["SS"] = free[("S", 1)]
                free["X0"] = free[("S", 0)]
                free["X1"] = t_w
                free["X2"] = t_w
                last = (qtr == 3)
                if qtr == 0:
                    srcq = lambda nb: xr2[nb]
                    for nb in range(KC):
                        st_tok[("src", id(srcq), nb)] = st_tok[(id(xr2), nb)]
                else:
                    srcq = lambda nb: xr3[nb]
                    for nb in range(KC):
                        st_tok[("src", id(srcq), nb)] = st_tok[(id(xr3), nb)]
                t_pe_last = None
                for nb in range(KC):
                    a = nextAB()
                    t_mm = lin(w_up[qtr, nb], 32, [(A[a][:, :], lambda kc: WT[:, kc, :])], extra_wait=ab_free[("A", a)])
                    s, t_x = load_x(srcq(nb), extra=st_tok.get(("src", id(srcq), nb)))
                    dve.wait(t_mm, t_x)
                    t_add = dve.mark(nc.vector.tensor_tensor(out=xc[s][:], in0=A[a][:, :], in1=xc[s][:], op=ALU.add))
                    ab_free[("A", a)] = t_add
                    sp.wait(t_add)
                    t_st = k.dma(sp, xr3[nb], xc[s][:], s_m)
                    st_tok[(id(xr3), nb)] = t_st
                    if last:
                        t_sq, t_pe_last = stats_acc(xc[s][:], t_add, nb)
                        x_free[s] = [t_st, t_sq]
                    else:
                        x_free[s] = [t_st]
                free["WT"] = t_mm
            free["hT"] = t_mm
            free["MT"] = t_mm
            free["aux"] = t_Gs
            t_rs, t_rs0 = make_rstd(rs[:], SS[:, :], t_pe_last, guard=[fr("rs"), t_a])
            free["SS"] = t_rs0
            for nb in range(KC):
                s, t_x = load_x(xr3[nb], extra=st_tok[(id(xr3), nb)])
                dve.wait(t_x, t_rs)
                t_o = dve.mark(nc.vector.scalar_tensor_tensor(out=xc[s][:], in0=xc[s][:], scalar=g_sb[:, 4, nb:nb + 1], in1=rs[:],
                                                              op0=ALU.mult, op1=ALU.mult))
                sp.wait(t_o)
                t_st = k.dma(sp, outT[p, nb], xc[s][:], s_m)
                x_free[s] = [t_st]
            free["rs"] = t_o
        sp.wait((s_m, s_m.count))
    return nc


def tile_w(W):
    K_, N_ = W.shape
    return np.ascontiguousarray(W.reshape(K_ // 128, 128, N_ // 128, 128).transpose(2, 1, 0, 3))


def vec_pk(v, KC):
    o = np.zeros((128, KC), np.float32)
    n = v.shape[0] // 128
    o[:, :n] = v.reshape(n, 128).T
    return o


def make_cst():
    s_idx = np.arange(128)
    cst = np.zeros((128, 384), np.float32)
    cst[:, 0:128] = (s_idx[:, None] < s_idx[None, :])
    cst[:, 128:256] = -1.0 * (s_idx[:, None] >= s_idx[None, :])
    cst[:, 256:384] = np.eye(128)
    return cst


def host_phase2_shared(inp, D):
    KC = D // 128
    SBW = 2048
    PW = D // 2
    PC = PW // 128
    w_in = inp["w_in"][0]
    o = 3 * SBW
    sh = {}
    sh["w_pool"] = tile_w(w_in[:, o:o + PW])
    sh["w_gsb"] = tile_w(w_in[:, o + PW:o + PW + D])
    sh["w_gpool"] = tile_w(w_in[:, o + PW + D:o + PW + 2 * D])
    sh["w_grp"] = np.concatenate([tile_w(inp["pool_group_w"][0, g]) for g in range(4)], axis=0)
    sh["w_bsb"] = tile_w(inp["w_branch_sb"][0])
    sh["w_bpool"] = tile_w(inp["w_branch_pool"][0])
    sh["w_out"] = tile_w(inp["w_out"][0])
    sh["w_xq"] = tile_w(inp["xa_w_q"][0])
    sh["w_xkv"] = tile_w(inp["xa_w_kv"][0])
    sh["w_xo"] = tile_w(inp["xa_w_o"][0])
    sh["w_pq"] = tile_w(inp["peer_w_query"][0])
    sh["w_down"] = tile_w(inp["peer_down"][0].T)
    sh["w_up"] = np.stack([tile_w(inp["peer_up"][0][q * 4096:(q + 1) * 4096]) for q in range(4)], axis=0)
    sh["keysT"] = np.ascontiguousarray(inp["peer_sub_keys"][0].transpose(3, 0, 1, 2).reshape(128, 16, 128))
    g = np.zeros((128, 6, KC), np.float32)
    g[:, 0] = vec_pk(inp["norm_mix"][0], KC)
    g[:, 1] = vec_pk(inp["norm_mem_q"][0], KC)
    g[:, 2] = vec_pk(inp["norm_mem_kv"][0], KC)
    g[:, 3] = vec_pk(inp["norm_ffn"][0], KC)
    g[:, 4] = vec_pk(inp["norm_final"], KC)
    g[:, 5] = vec_pk(inp["pool_scale"][0], KC)
    sh["gains"] = g
    sh["cst"] = make_cst()
    mem = inp["mem"][0]
    sh["memT"] = np.ascontiguousarray(mem.reshape(256, KC, 128).transpose(1, 2, 0))
    return sh


def host_phase2_core(x, attn_full, tok0, TOK, D):
    KC = D // 128
    NP = TOK // 512
    xs = x[tok0:tok0 + TOK]
    m = {}
    m["xin"] = np.ascontiguousarray(xs.reshape(NP, 512, KC, 128).transpose(0, 2, 3, 1))
    xh = np.zeros((NP, 128, KC, 16), np.float32)
    ic = np.zeros((NP, 128, 4, 16), np.float32)
    for p in range(NP):
        st = tok0 + p * 512
        if st >= 16:
            xh[p] = x[st - 16:st].reshape(16, KC, 128).transpose(2, 1, 0)
        pos = st + np.arange(16)
        for gi, w in enumerate((2, 4, 8, 16)):
            ic[p, :, gi, :] = 1.0 / np.minimum(pos + 1, w)
    m["xhalo"] = xh
    m["invc"] = ic
    m["attn_in"] = np.ascontiguousarray(attn_full[:, tok0:tok0 + TOK])
    return m


def unpack_out(outT, TOK, D):
    KC = D // 128
    NP = TOK // 512
    return np.ascontiguousarray(outT.reshape(NP, KC, 128, 512).transpose(0, 3, 1, 2)).reshape(TOK, D)


def kernel(**inp):
    S, D = 8192, 4096
    inp = {k_: np.asarray(v) for k_, v in inp.items()}
    nc = bass.Bass("TRN2", target_bir_lowering=False)
    build_fused(nc, S, D)
    maps, owners = host_fused_inputs(inp, S, D)
    r = run_bass_kernel_spmd(nc, maps, core_ids=list(range(NCORES)))
    out = np.zeros((S, D), np.float32)
    for c in range(NCORES):
        o = unpack_out(r.results[c]["outT"], 1024, D)
        for p, g in enumerate(owners[c]):
            out[g * 512:(g + 1) * 512] = o[p * 512:(p + 1) * 512]
    return out.reshape(1, S, D)


def declare_tensors(nc, S, TOK, D):
    KC = D // 128
    NT = S // 128
    NP = TOK // 512
    PC = (D // 2) // 128
    CPG = PC // 4
    T = 512

    def din(name, shape, dt=F32):
        return nc.dram_tensor(name, shape, dt, kind="ExternalInput").ap()
    t = {}
    t["xT"] = din("xT", [NT, 128, KC, 128])
    t["wqkv"] = din("wqkv", [8, 128, KC, 768])
    t["bias_tab"] = din("bias_tab", [2, NT, 128, 512], BF16)
    t["xin"] = din("xin", [NP, KC, 128, T])
    t["xhalo"] = din("xhalo", [NP, 128, KC, 16])
    t["invc"] = din("invc", [NP, 128, 4, 16])
    t["memT"] = din("memT", [KC, 128, 256])
    t["gains"] = din("gains", [128, 6, KC])
    t["cst"] = din("cst", [128, 384])
    t["keysT"] = din("keysT", [128, 16, 128])
    t["w_pool"] = din("w_pool", [PC, 128, KC, 128])
    t["w_grp"] = din("w_grp", [PC, 128, CPG, 128])
    t["w_gsb"] = din("w_gsb", [KC, 128, KC, 128])
    t["w_gpool"] = din("w_gpool", [KC, 128, KC, 128])
    t["w_bsb"] = din("w_bsb", [KC, 128, 16, 128])
    t["w_bpool"] = din("w_bpool", [KC, 128, PC, 128])
    t["w_out"] = din("w_out", [KC, 128, KC, 128])
    t["w_xq"] = din("w_xq", [4, 128, KC, 128])
    t["w_xkv"] = din("w_xkv", [8, 128, KC, 128])
    t["w_xo"] = din("w_xo", [KC, 128, 4, 128])
    t["w_pq"] = din("w_pq", [16, 128, KC, 128])
    t["w_down"] = din("w_down", [128, 128, KC, 128])
    t["w_up"] = din("w_up", [4, KC, 128, 32, 128])
    t["outT"] = nc.dram_tensor("outT", [NP, KC, 128, T], F32, kind="ExternalOutput").ap()
    t["attn_scr"] = nc.dram_tensor("attn_scr", [16 * 128, TOK], BF16).ap()
    return t


def build_fused(nc, S, D):
    TOK = 1024
    t = declare_tensors(nc, S, TOK, D)
    with contextlib.ExitStack() as sem_stack:
        k = K(nc, None, sem_stack)
        build_phase1(nc, k, S, D, t)
        build_phase2(nc, k, TOK, D, t)
    return nc


def host_fused_inputs(inp, S, D):
    KC = D // 128
    NT = S // 128
    NG = S // 512
    x = inp["x"][0]
    sh = host_phase2_shared(inp, D)
    sh["xT"] = np.ascontiguousarray(x.reshape(NT, 128, KC, 128).transpose(0, 3, 2, 1))
    w_in = inp["w_in"][0]
    SBW = 2048
    wq = np.zeros((8, 128, KC, 768), np.float32)
    for hp in range(8):
        cols = []
        for part in range(3):
            for hh in range(2):
                h = hp * 2 + hh
                cols.append(w_in[:, part * SBW + h * 128: part * SBW + (h + 1) * 128])
        w = np.concatenate(cols, axis=1)
        wq[hp] = w.reshape(KC, 128, -1).transpose(1, 0, 2)
    sh["wqkv"] = wq
    maps = []
    owners = []
    s_idx = np.arange(128)
    t_idx = np.arange(512)
    for c in range(NCORES):
        groups = (c, NG - 1 - c)
        owners.append(groups)
        m = dict(sh)
        xs = np.concatenate([x[g * 512:(g + 1) * 512] for g in groups], axis=0)
        m["xin"] = np.ascontiguousarray(xs.reshape(2, 512, KC, 128).transpose(0, 2, 3, 1))
        xh = np.zeros((2, 128, KC, 16), np.float32)
        ic = np.zeros((2, 128, 4, 16), np.float32)
        bt = np.zeros((2, NT, 128, 512), np.float32)
        for p, g in enumerate(groups):
            st = g * 512
            if st >= 16:
                xh[p] = x[st - 16:st].reshape(16, KC, 128).transpose(2, 1, 0)
            pos = st + np.arange(16)
            for gi, w in enumerate((2, 4, 8, 16)):
                ic[p, :, gi, :] = 1.0 / np.minimum(pos + 1, w)
            qpos = st + t_idx
            for kb in range(NT):
                kpos = kb * 128 + s_idx
                bt[p, kb] = np.where(kpos[:, None] < qpos[None, :], 0.0, -30000.0)
        m["xhalo"] = xh
        m["invc"] = ic
        m["bias_tab"] = bt.astype(ml_dtypes.bfloat16)
        maps.append(m)
    return maps, owners
```

```python
import contextlib
import numpy as np
import ml_dtypes
import concourse.bass as bass
import concourse.mybir as mybir
from concourse.bass_utils import run_bass_kernel_spmd

F32, BF16 = mybir.dt.float32, mybir.dt.bfloat16
AF = mybir.ActivationFunctionType
ALU = mybir.AluOpType
AX = mybir.AxisListType

NCORES = 8
EPS = 1e-6
NEG = -1.0e30


class Sem:
    def __init__(self, handle):
        self.h = handle
        self.count = 0


class Eng:
    def __init__(self, k, eng, name):
        self.k, self.e, self.name = k, eng, name
        self.sem = k.new_sem(name)
        self.seen = {}

    def wait(self, *toks):
        for t in toks:
            if t is None:
                continue
            if isinstance(t, (list, tuple)) and len(t) and not isinstance(t[0], Sem):
                self.wait(*t)
                continue
            s, v = t
            if self.seen.get(id(s), 0) >= v:
                continue
            self.e.wait_ge(s.h, v)
            self.seen[id(s)] = v

    def mark(self, ins):
        ins.then_inc(self.sem.h, 1)
        self.sem.count += 1
        return (self.sem, self.sem.count)


class K:
    def __init__(self, nc, stack, sem_stack=None):
        self.nc, self.stack = nc, stack
        self.sem_stack = sem_stack if sem_stack is not None else stack
        self._sems = []
        self.pe = Eng(self, nc.tensor, "pe")
        self.act = Eng(self, nc.scalar, "act")
        self.dve = Eng(self, nc.vector, "dve")
        self.pool = Eng(self, nc.gpsimd, "pool")
        self.sp = Eng(self, nc.sync, "sp")

    def new_sem(self, name):
        s = Sem(self.sem_stack.enter_context(self.nc.semaphore(name + "_%d" % len(self._sems))))
        self._sems.append(s)
        return s

    def sbuf(self, name, shape, dt):
        return self.stack.enter_context(self.nc.sbuf_tensor(name, shape, dt))

    def psum(self, name, shape, dt=F32):
        return self.stack.enter_context(self.nc.psum_tensor(name, shape, dt))

    def dma(self, q, out, in_, sem):
        ins = q.e.dma_start(out=out, in_=in_)
        ins.then_inc(sem.h, 16)
        sem.count += 16
        return (sem, sem.count)


def bc(ap, axis, shape):
    return ap.unsqueeze(axis).broadcast_to(list(shape))


def build_phase1(nc, k, S, D, T_in):
    HPC = 2
    KC = D // 128
    NT = S // 128
    xT, xin, wqkv, gains, cst, bias_tab, attn_scr = (T_in[n] for n in
                                                     ("xT", "xin", "wqkv", "gains", "cst", "bias_tab", "attn_scr"))
    NKB = (NT // 2, NT)
    with contextlib.ExitStack() as st:
        k.stack = st
        nc_ = nc
        pe, act, dve, pool, sp = k.pe, k.act, k.dve, k.pool, k.sp
        W = k.sbuf("W", [128, KC, 3 * HPC * 128], BF16)
        KT = k.sbuf("KT", [128, HPC, S], BF16)
        V = k.sbuf("V", [128, NT, HPC * 128], BF16)
        QT = [k.sbuf("QT%d" % i, [128, HPC, 512], BF16) for i in range(2)]
        xs = [k.sbuf("xs%d" % i, [128, KC, 128], F32) for i in range(2)]
        sq = k.sbuf("sq", [128, KC, 128], BF16)
        hbl = [k.sbuf("hb%d" % i, [128, KC, 128], BF16) for i in range(2)]
        g_sb = k.sbuf("g1", [128, 6, KC], F32)
        c_sb = k.sbuf("c1", [128, 384], F32)
        ones_b = k.sbuf("ones1", [128, 128], BF16)
        nones_b = k.sbuf("nones", [128, 128], BF16)
        ntri_b = k.sbuf("ntri", [128, 128], BF16)
        ident_b = k.sbuf("ident1", [128, 128], BF16)
        rstd = k.sbuf("rstd", [128, 132], F32)
        ebuf = [k.sbuf("eb%d" % i, [128, 512], F32) for i in range(3)]
        spb = [k.sbuf("sp%d" % i, [128, 512], BF16) for i in range(3)]
        atb = [k.sbuf("at%d" % i, [128, 512], BF16) for i in range(3)]
        bias = [k.sbuf("bias%d" % i, [128, 512], BF16) for i in range(3)]
        spacc = k.sbuf("spacc", [128, HPC, 512], BF16)
        ostage = [k.sbuf("os%d" % i, [128, 512], BF16) for i in range(2)]
        pz = [k.psum("pz%d" % i, [128, 512]) for i in range(3)]
        po = [k.psum("po%d" % i, [128, 512]) for i in range(HPC)]
        pqk = k.psum("pqk", [128, 512])
        pv = k.psum("pv", [128, 512])

        s_c = k.new_sem("ldc")
        s_w = k.new_sem("ldw")
        s_x = [k.new_sem("ldx") for _ in range(2)]
        s_o = [k.new_sem("sto") for _ in range(2)]
        s_b = [k.new_sem("ldb") for _ in range(3)]

        k.dma(sp, g_sb[:], gains, s_c)
        t_c = k.dma(sp, c_sb[:], cst, s_c)
        dve.wait(t_c)
        nc.vector.memset(ones_b[:], 1.0)
        nc.vector.memset(nones_b[:], -1.0)
        nc.vector.tensor_copy(out=ntri_b[:], in_=c_sb[:, 128:256])
        t_cst = dve.mark(nc.vector.tensor_copy(out=ident_b[:], in_=c_sb[:, 256:384]))
        pe.wait(t_cst)
        act.wait(t_cst)
        pool.wait(t_cst)

        scale = 128.0 ** -0.5
        st8 = {"x_free": [None, None], "sq_free": None, "hb_free": [None, None], "pqk_free": None, "pv_free": None,
               "rstd_free": None, "xctr": 0, "w_free": None, "kv_free": None, "last": None}
        qt_free = [None, None]

        def project(src_ap, mode, tt=None, slot=None, c0=None):
            d = st8
            sl = d["xctr"] % 2
            d["xctr"] += 1
            sp.wait(d["x_free"][sl])
            t_x = k.dma(sp, xs[sl][:], src_ap, s_x[sl])
            act.wait(t_x, d["sq_free"])
            t_sq = act.mark(nc.scalar.activation(out=sq[:], in_=xs[sl][:], func=AF.Square))
            hb = hbl[sl]
            pool.wait(t_x, d["hb_free"][sl])
            t_hb = pool.mark(nc.gpsimd.tensor_tensor(out=hb[:], in0=xs[sl][:], in1=bc(g_sb[:, 0, :], 2, [128, KC, 128]),
                                                     op=ALU.mult))
            d["x_free"][sl] = [t_sq, t_hb]
            pe.wait(t_sq, d["pv_free"])
            for kc in range(KC):
                nc.tensor.matmul(pv[:, 256:384], lhsT=ones_b[:], rhs=sq[:, kc, :], start=(kc == 0), stop=(kc == KC - 1))
            for kc in range(KC):
                ins = nc.tensor.matmul(pv[:, 384:385], lhsT=sq[:, kc, :], rhs=ones_b[:, 0:1], start=(kc == 0),
                                       stop=(kc == KC - 1))
            t_ss = pe.mark(ins)
            d["sq_free"] = t_ss
            dve.wait(t_ss, d["rstd_free"])
            t_r00 = dve.mark(nc.vector.tensor_scalar(out=rstd[:, 0:129], in0=pv[:, 256:385], scalar1=1.0 / D, scalar2=EPS,
                                                     op0=ALU.mult, op1=ALU.add))
            act.wait(t_r00)
            t_r01 = act.mark(nc.scalar.activation(out=rstd[:, 0:129], in_=rstd[:, 0:129], func=AF.Ln))
            act.wait(t_r01)
            t_r0 = act.mark(nc.scalar.activation(out=rstd[:, 0:129], in_=rstd[:, 0:129], func=AF.Exp, scale=-0.5))
            pe.wait(t_hb, d["pqk_free"], d["w_ready"])
            blks = range(HPC, 2 * HPC) if mode == "kv" else range(0, HPC)
            for blk in blks:
                for kc in range(KC):
                    ins = nc.tensor.matmul(pqk[:, blk * 128:(blk + 1) * 128], lhsT=W[:, kc, blk * 128:(blk + 1) * 128],
                                           rhs=hb[:, kc, :], start=(kc == 0), stop=(kc == KC - 1))
            t_qk = pe.mark(ins)
            t_v = None
            if mode == "kv":
                for kc in range(KC):
                    ins = nc.tensor.matmul(pv[:, 0:HPC * 128], lhsT=hb[:, kc, :], rhs=W[:, kc, 2 * HPC * 128:3 * HPC * 128],
                                           start=(kc == 0), stop=(kc == KC - 1))
                t_v = pe.mark(ins)
            d["hb_free"][sl] = t_v if t_v is not None else t_qk
            d["w_last"] = d["hb_free"][sl]
            if mode == "kv":
                dve.wait(t_qk, t_r0, d["kv_free"])
                for hh in range(HPC):
                    ins = nc.vector.tensor_tensor(out=KT[:, hh, tt * 128:(tt + 1) * 128],
                                                  in0=pqk[:, (HPC + hh) * 128:(HPC + hh + 1) * 128], in1=rstd[:, 0:128],
                                                  op=ALU.mult)
                t_e1 = dve.mark(ins)
                act.wait(t_v, t_r0, d["kv_free"])
                t_e2 = act.mark(nc.scalar.activation(out=V[:, tt, :], in_=pv[:, 0:HPC * 128], func=AF.Copy,
                                                     scale=rstd[:, 128:129]))
                d["pv_free"] = [t_e2, t_r0]
                d["rstd_free"] = [t_e1, t_e2]
                d["last"] = [t_e1, t_e2]
                d["last_kv"] = [t_e1, t_e2]
            else:
                dve.wait(t_qk, t_r0, qt_free[slot])
                for hh in range(HPC):
                    ins = nc.vector.scalar_tensor_tensor(out=QT[slot][:, hh, c0:c0 + 128], in0=pqk[:, hh * 128:(hh + 1) * 128],
                                                         scalar=scale, in1=rstd[:, 0:128], op0=ALU.mult, op1=ALU.mult)
                t_e1 = dve.mark(ins)
                d["pv_free"] = [t_r0]
                d["rstd_free"] = [t_e1]
                d["last"] = [t_e1]
            d["pqk_free"] = t_e1

        it_ctr = [0]
        pz_free = [None] * 3
        sp_free = [None] * 3
        at_free = [None] * 3
        b_free = [None] * 3
        b_ctr = [0]
        po_free = [None] * HPC
        spacc_tok = [None] * HPC
        os_free = [None, None]
        o_ctr = [0]
        out_toks = []

        def attention(hp, slot):
            nkb = NKB[slot]
            qt = QT[slot]
            its = [(kb, hh) for kb in reversed(range(nkb)) for hh in range(HPC)]
            n = len(its)
            stt = [dict() for _ in range(n)]
            pe.wait(st8["last"], st8["last_kv"])
            pool.wait(st8["last"], st8["last_kv"])
            for hh in range(HPC):
                pool.wait(spacc_tok[hh])
            t_z = pool.mark(nc.gpsimd.memset(spacc[:], 0.0))
            for hh in range(HPC):
                spacc_tok[hh] = t_z
            last_av = [None] * HPC
            btile = {}

            def st0(i):
                kb, hh = its[i]
                b = (it_ctr[0] + i) % 3
                d = stt[i]
                d["b"] = b
                if hh == 0:
                    bs = b_ctr[0] % 3
                    b_ctr[0] += 1
                    sp.wait(b_free[bs])
                    btile[kb] = (bs, k.dma(sp, bias[bs][:], bias_tab[slot, kb], s_b[bs]))
                bs, t_b = btile[kb]
                pe.wait(pz_free[b], t_b)
                nc.tensor.matmul(pz[b][:, :], lhsT=KT[:, hh, kb * 128:(kb + 1) * 128], rhs=qt[:, hh, :], start=True, stop=False)
                d["z"] = pe.mark(nc.tensor.matmul(pz[b][:, :], lhsT=ident_b[:], rhs=bias[bs][:], start=False, stop=False))
                if hh == HPC - 1:
                    b_free[bs] = d["z"]
                act.wait(d["z"], sp_free[b])
                t_e = act.mark(nc.scalar.activation(out=ebuf[b][:], in_=pz[b][:, :], func=AF.Exp))
                act.wait(t_e)
                d["ln"] = act.mark(nc.scalar.activation(out=spb[b][:], in_=ebuf[b][:], func=AF.Ln, bias=1.0))

            def st1(i):
                kb, hh = its[i]
                d = stt[i]
                b = d["b"]
                first = (kb == nkb - 1)
                pe.wait(d["ln"], spacc_tok[hh])
                ins = nc.tensor.matmul(pz[b][:, :], lhsT=ntri_b[:], rhs=spb[b][:], start=False, stop=first)
                if not first:
                    ins = nc.tensor.matmul(pz[b][:, :], lhsT=nones_b[:], rhs=spacc[:, hh, :], start=False, stop=True)
                d["cs"] = pe.mark(ins)
                act.wait(d["cs"], at_free[b])
                d["ea"] = act.mark(nc.scalar.activation(out=atb[b][:], in_=pz[b][:, :], func=AF.Exp))
                pz_free[b] = d["ea"]
                pool.wait(d["cs"], d["ln"], spacc_tok[hh])
                spacc_tok[hh] = pool.mark(nc.gpsimd.tensor_tensor(out=spacc[:, hh, :], in0=spacc[:, hh, :], in1=spb[b][:],
                                                                  op=ALU.add))
                sp_free[b] = spacc_tok[hh]

            def st2(i):
                kb, hh = its[i]
                d = stt[i]
                b = d["b"]
                pe.wait(d["ea"], po_free[hh] if kb == nkb - 1 else None)
                d["av"] = pe.mark(nc.tensor.matmul(po[hh][:, :], lhsT=V[:, kb, hh * 128:(hh + 1) * 128], rhs=atb[b][:],
                                                   start=(kb == nkb - 1), stop=(kb == 0)))
                at_free[b] = d["av"]
                last_av[hh] = d["av"]

            for step in range(n + 2):
                if step < n:
                    st0(step)
                if 0 <= step - 1 < n:
                    st1(step - 1)
                if 0 <= step - 2 < n:
                    st2(step - 2)
            it_ctr[0] += n
            qt_free[slot] = [last_av[hh] for hh in range(HPC)]
            st8["kv_free"] = [last_av[hh] for hh in range(HPC)]
            for hh in range(HPC):
                o = o_ctr[0] % 2
                o_ctr[0] += 1
                act.wait(last_av[hh], os_free[o])
                t_cp = act.mark(nc.scalar.copy(out=ostage[o][:], in_=po[hh][:, :]))
                po_free[hh] = t_cp
                sp.wait(t_cp)
                h = hp * HPC + hh
                os_free[o] = k.dma(sp, attn_scr[h * 128:(h + 1) * 128, slot * 512:(slot + 1) * 512], ostage[o][:], s_o[o])
                out_toks.append(os_free[o])

        st8["w_ready"] = None
        st8["w_last"] = None
        for hp in range(8):
            pool.wait(st8["w_last"], st8["kv_free"])
            for i in range(3 * HPC):
                t_w = k.dma(pool, W[:, :, i * 128:(i + 1) * 128], wqkv[hp][:, :, i * 128:(i + 1) * 128], s_w)
            st8["w_ready"] = t_w
            for tt in range(NT):
                project(xT[tt], "kv", tt=tt)
            for j in range(8):
                slot, c0 = j // 4, (j % 4) * 128
                project(xin[slot][:, :, c0:c0 + 128].rearrange("kc q t -> q kc t"), "q", slot=slot, c0=c0)
            for slot in range(2):
                attention(hp, slot)
        fin = [(e.sem, e.sem.count) for e in (pe, act, dve, pool)] + [(s_, s_.count) for s_ in s_o]
        for e in (pe, act, dve, pool, sp):
            e.wait(fin)
    return nc


def host_phase1_inputs(x, w_in, norm_mix, S, D, HPC, ncores):
    KC = D // 128
    NT = S // 128
    SBW = ncores * HPC * 128
    xT = np.ascontiguousarray(x.reshape(NT, 128, KC, 128).transpose(0, 3, 2, 1))
    g = np.ascontiguousarray(norm_mix.reshape(KC, 128).T)
    s_idx = np.arange(128)
    cst = np.zeros((128, 384), np.float32)
    cst[:, 0:128] = (s_idx[:, None] < s_idx[None, :])
    cst[:, 128:256] = -1.0 * (s_idx[:, None] >= s_idx[None, :])
    cst[:, 256:384] = np.eye(128)
    maps = []
    for c in range(ncores):
        cols = []
        for part in range(3):
            for hh in range(HPC):
                h = c * HPC + hh
                cols.append(w_in[:, part * SBW + h * 128: part * SBW + (h + 1) * 128])
        w = np.concatenate(cols, axis=1)
        w = np.ascontiguousarray(w.reshape(KC, 128, -1).transpose(1, 0, 2))
        maps.append({"xT": xT, "wqkv": w, "gmix": g, "cst": cst})
    return maps


def build_phase2(nc, k, TOK, D, T_in):
    KC = D // 128
    NP = TOK // 512
    PC = (D // 2) // 128
    CPG = PC // 4
    NE = 128
    T = 512
    (xin, xhalo, invc, attn_in, memT, gains, cst, keysT, w_pool, w_grp, w_gsb, w_gpool, w_bsb, w_bpool, w_out, w_xq, w_xkv,
     w_xo, w_pq, w_down, w_up, outT) = (T_in[n] for n in (
        "xin", "xhalo", "invc", "attn_scr", "memT", "gains", "cst", "keysT", "w_pool", "w_grp", "w_gsb", "w_gpool", "w_bsb",
        "w_bpool", "w_out", "w_xq", "w_xkv", "w_xo", "w_pq", "w_down", "w_up", "outT"))
    xr1 = nc.dram_tensor("xr1", [KC, 128, T], F32).ap()
    xr2 = nc.dram_tensor("xr2", [KC, 128, T], F32).ap()
    xr3 = nc.dram_tensor("xr3", [KC, 128, T], F32).ap()

    with contextlib.ExitStack() as st:
        k.stack = st
        pe, act, dve, pool, sp = k.pe, k.act, k.dve, k.pool, k.sp
        KW = max(KC, 32)
        hT = k.sbuf("hT", [128, KC, T], BF16)
        MT = k.sbuf("MT", [128, 32, T], BF16)
        aux = k.sbuf("aux", [128, 16, T], BF16)
        NWS = 6
        wsl = [k.sbuf("ws%d" % i, [128, KW, 128], BF16) for i in range(NWS)]
        xc = [k.sbuf("xc%d" % i, [128, T], F32) for i in range(3)]
        sqc = [k.sbuf("sqc%d" % i, [128, T], BF16) for i in range(2)]
        ft = [k.sbuf("ft%d" % i, [128, T], F32) for i in range(4)]
        bt = [k.sbuf("bt%d" % i, [128, T], BF16) for i in range(3)]
        rs = k.sbuf("rs", [128, T], F32)
        rsh = k.sbuf("rsh", [128, 16], F32)
        rsm = k.sbuf("rsm", [128, 260], F32)
        g_sb = k.sbuf("g", [128, 6, KC], F32)
        c_sb = k.sbuf("c", [128, 384], F32)
        ones_b = k.sbuf("ones", [128, 128], BF16)
        ident_b = k.sbuf("identb", [128, 128], BF16)
        keys_b = k.sbuf("keysb", [128, 16, 128], BF16)
        xh = k.sbuf("xh", [128, KC, 16], F32)
        sqh = k.sbuf("sqh", [128, KC, 16], BF16)
        hh = k.sbuf("hh", [128, KC, 16], BF16)
        ubuf = k.sbuf("ubuf", [128, 528], F32)
        ra = k.sbuf("ra", [128, 528], F32)
        rb = k.sbuf("rb", [128, 528], F32)
        icv = k.sbuf("icv", [128, 4, 16], F32)
        t16 = k.sbuf("t16", [128, 16], F32)
        kmT = k.sbuf("kmT", [128, 4, 256], BF16)
        vm = k.sbuf("vm", [128, 2, 512], BF16)
        qx = k.sbuf("qx", [128, 4, T], BF16)
        ox = k.sbuf("ox", [128, 4, T], BF16)
        pT = k.sbuf("pT", [128, 2, T], BF16)
        top = k.sbuf("top", [128, 16, 16], F32)
        best = k.sbuf("best", [128, 8, 16], F32)
        zz = k.sbuf("zz", [128, 8, 4], F32)
        tau = k.sbuf("tau", [128, 4, 8], F32)
        negb = k.sbuf("negb", [128, 4, 8], F32)
        ps = [k.psum("ps%d" % i, [128, 512]) for i in range(8)]
        A, B, SS, X = ps[0:2], ps[2:4], ps[4], ps[5:8]

        s_c = k.new_sem("ldc")
        s_w = [k.new_sem("ldw") for _ in range(6)]
        s_x = [k.new_sem("ldx") for _ in range(3)]
        s_m = k.new_sem("misc")

        k.dma(sp, g_sb[:], gains, s_c)
        t_kb = k.dma(pool, keys_b[:], keysT, s_w[0])
        t_c = k.dma(sp, c_sb[:], cst, s_c)
        dve.wait(t_c)
        nc.vector.memset(ones_b[:], 1.0)
        dve.wait(t_kb)
        t_cst = dve.mark(nc.vector.tensor_copy(out=ident_b[:], in_=c_sb[:, 256:384]))
        pe.wait(t_cst)
        act.wait(t_cst)

        wctr = [0]
        w_free = [None] * NWS
        free = {}

        def fr(name):
            return free.get(name)

        def load_w(tile_ap, KCw):
            s = wctr[0] % NWS
            wctr[0] += 1
            pool.wait(w_free[s])
            t = k.dma(pool, wsl[s][:, 0:KCw, :], tile_ap, s_w[s])
            return s, t

        def lin(tile_ap, KCw, outs, extra_wait=None):
            s, t = load_w(tile_ap, KCw)
            pe.wait(t, extra_wait)
            ins = None
            for (pap, rf) in outs:
                for kc in range(KCw):
                    ins = nc.tensor.matmul(pap, lhsT=wsl[s][:, kc, :], rhs=rf(kc), start=(kc == 0), stop=(kc == KCw - 1))
            tok = pe.mark(ins)
            w_free[s] = tok
            return tok

        xctr = [0]
        x_free = [None] * 3

        def load_x(src_ap, extra=None, n=T):
            s = xctr[0] % 3
            xctr[0] += 1
            sp.wait(x_free[s], extra)
            t = k.dma(sp, xc[s][:, 0:n], src_ap, s_x[s])
            return s, t

        sqctr = [0]
        sq_free = [None] * 2

        def stats_sq(src_ap, src_tok, n=T):
            s = sqctr[0] % 2
            sqctr[0] += 1
            act.wait(src_tok, sq_free[s])
            t = act.mark(nc.scalar.activation(out=sqc[s][:, 0:n], in_=src_ap, func=AF.Square))
            return (s, t, n)

        def stats_mm(pend, kc):
            s, t, n = pend
            pe.wait(t, fr("SS") if kc == 0 else None)
            tp = pe.mark(nc.tensor.matmul(SS[:, 0:n], lhsT=ones_b[:], rhs=sqc[s][:, 0:n], start=(kc == 0),
                                          stop=(kc == KC - 1)))
            sq_free[s] = tp
            return tp

        def stats_acc(src_ap, src_tok, kc, n=T):
            pend = stats_sq(src_ap, src_tok, n)
            return pend[1], stats_mm(pend, kc)

        def make_rstd(dst_ap, src_ps_ap, tok, guard=None):
            dve.wait(tok, guard)
            t0 = dve.mark(nc.vector.tensor_scalar(out=dst_ap, in0=src_ps_ap, scalar1=1.0 / D, scalar2=EPS, op0=ALU.mult,
                                                  op1=ALU.add))
            act.wait(t0)
            t1 = act.mark(nc.scalar.activation(out=dst_ap, in_=dst_ap, func=AF.Ln))
            act.wait(t1)
            t2 = act.mark(nc.scalar.activation(out=dst_ap, in_=dst_ap, func=AF.Exp, scale=-0.5))
            return t2, t0

        abctr = [0]

        def nextAB():
            i = abctr[0] % 2
            abctr[0] += 1
            return i

        ab_free = {("A", 0): None, ("A", 1): None, ("B", 0): None, ("B", 1): None}

        hm = MT
        t_hm = None
        for kc in range(KC):
            s, t = load_x(memT[kc], n=256)
            t_sq, t_pe = stats_acc(xc[s][:, 0:256], t, kc, n=256)
            sqs = sqc[(sqctr[0] - 1) % 2]
            for mc in range(2):
                t_pe = pe.mark(nc.tensor.matmul(X[mc][:, 0:1], lhsT=sqs[:, mc * 128:(mc + 1) * 128],
                                                rhs=ones_b[:, 0:1], start=(kc == 0), stop=(kc == KC - 1)))
            sq_free[(sqctr[0] - 1) % 2] = t_pe
            dve.wait(t)
            t_hm = dve.mark(nc.vector.tensor_scalar(out=hm[:, kc, 0:256], in0=xc[s][:, 0:256], scalar1=g_sb[:, 2, kc:kc + 1],
                                                    scalar2=None, op0=ALU.mult))
            x_free[s] = [t_sq, t_hm]
        t_rsm, t_rsm0 = make_rstd(rsm[:, 0:256], SS[:, 0:256], t_pe)
        free["SS"] = t_rsm0
        for mc in range(2):
            t_rsm, t_x0 = make_rstd(rsm[:, 256 + mc:257 + mc], X[mc][:, 0:1], t_pe)
            free["X%d" % mc] = t_x0
        pe.wait(t_hm)
        for hd in range(4):
            a = nextAB()
            t_mm = lin(w_xkv[hd], KC, [(A[a][:, 0:256], lambda kc: hm[:, kc, 0:256])], extra_wait=ab_free[("A", a)])
            dve.wait(t_mm, t_rsm)
            ab_free[("A", a)] = dve.mark(nc.vector.tensor_tensor(out=kmT[:, hd, :], in0=A[a][:, 0:256], in1=rsm[:, 0:256],
                                                                 op=ALU.mult))
        for blk in range(4):
            s, t = load_w(w_xkv[4 + blk], KC)
            a = nextAB()
            pe.wait(t, ab_free[("A", a)])
            for mc in range(2):
                for kc in range(KC):
                    ins = nc.tensor.matmul(A[a][:, mc * 128:(mc + 1) * 128], lhsT=hm[:, kc, mc * 128:(mc + 1) * 128],
                                           rhs=wsl[s][:, kc, :], start=(kc == 0), stop=(kc == KC - 1))
            t_mm = pe.mark(ins)
            w_free[s] = t_mm
            act.wait(t_mm, t_rsm)
            for mc in range(2):
                t_e = act.mark(nc.scalar.activation(out=vm[:, mc, blk * 128:(blk + 1) * 128],
                                                    in_=A[a][:, mc * 128:(mc + 1) * 128], func=AF.Copy,
                                                    scale=rsm[:, 256 + mc:257 + mc]))
            ab_free[("A", a)] = t_e
        t_mem_done = [ab_free[("A", 0)], ab_free[("A", 1)]]
        free["MT"] = t_mm

        xa_scale = 128.0 ** -0.5
        st_tok = {}

        def residual_stage(p, n_blocks, w_tiles, KCw, rhs_fn, src_fn, dst, gain_idx, pre_wait=None, final=False):
            t_pe_last = None
            t_h = None
            pend = None
            for nb in range(n_blocks):
                a = nextAB()
                t_mm = lin(w_tiles[nb], KCw, [(A[a][:, :], rhs_fn)], extra_wait=[ab_free[("A", a)], pre_wait])
                if pend is not None:
                    stats_mm(pend, nb - 1)
                s, t_x = load_x(src_fn(nb), extra=st_tok.get(("src", id(src_fn), nb)))
                dve.wait(t_mm, t_x)
                t_add = dve.mark(nc.vector.tensor_tensor(out=xc[s][:], in0=A[a][:, :], in1=xc[s][:], op=ALU.add))
                ab_free[("A", a)] = t_add
                sp.wait(t_add)
                t_st = k.dma(sp, dst[nb], xc[s][:], s_m)
                st_tok[(id(dst), nb)] = t_st
                pend = stats_sq(xc[s][:], t_add)
                t_sq = pend[1]
                if nb == n_blocks - 1:
                    t_pe_last = stats_mm(pend, nb)
                if gain_idx is not None:
                    dve.wait(fr("hT"))
                    t_h = dve.mark(nc.vector.tensor_scalar(out=hT[:, nb, :], in0=xc[s][:], scalar1=g_sb[:, gain_idx, nb:nb + 1],
                                                           scalar2=None, op0=ALU.mult))
                    x_free[s] = [t_st, t_sq, t_h]
                else:
                    x_free[s] = [t_st, t_sq]
            t_rs, t_rs0 = make_rstd(rs[:], SS[:, :], t_pe_last, guard=fr("rs"))
            free["SS"] = t_rs0
            return t_rs, t_h

        for p in range(NP):
            tc0 = p * T
            dve.wait(fr("hT"))
            for kc in range(KC):
                s, t = load_x(xin[p, kc])
                t_sq, t_pe = stats_acc(xc[s][:], t, kc)
                dve.wait(t)
                t_h = dve.mark(nc.vector.tensor_scalar(out=hT[:, kc, :], in0=xc[s][:], scalar1=g_sb[:, 0, kc:kc + 1],
                                                       scalar2=None, op0=ALU.mult))
                x_free[s] = [t_sq, t_h]
            t_rs, t_rs0 = make_rstd(rs[:], SS[:, :], t_pe, guard=fr("rs"))
            free["SS"] = t_rs0
            sp.wait(fr("xh"))
            k.dma(sp, icv[:], invc[p], s_m)
            t_xh = k.dma(sp, xh[:], xhalo[p], s_m)
            act.wait(t_xh, fr("sqh"))
            t_sqh = act.mark(nc.scalar.activation(out=sqh[:], in_=xh[:], func=AF.Square))
            dve.wait(t_xh, fr("hh"))
            t_hh = dve.mark(nc.vector.tensor_tensor(out=hh[:], in0=xh[:], in1=bc(g_sb[:, 0, :], 2, [128, KC, 16]), op=ALU.mult))
            free["xh"] = [t_sqh, t_hh]
            pe.wait(t_sqh, fr("SS"))
            for kc in range(KC):
                ins = nc.tensor.matmul(SS[:, 0:16], lhsT=ones_b[:], rhs=sqh[:, kc, :], start=(kc == 0), stop=(kc == KC - 1))
            t_ssh = pe.mark(ins)
            free["sqh"] = t_ssh
            t_rsh, t_rsh0 = make_rstd(rsh[:], SS[:, 0:16], t_ssh, guard=fr("rsh"))
            free["SS"] = t_rsh0
            pe.wait(t_h, t_hh)

            pooledT = MT
            dve.wait(fr("MT"))
            t_pool = None
            for nb in range(PC):
                g = nb // CPG
                a = nextAB()
                t_mm = lin(w_pool[nb], KC, [(A[a][:, :], lambda kc: hT[:, kc, :]), (B[a][:, 0:16], lambda kc: hh[:, kc, :])],
                           extra_wait=[ab_free[("A", a)], ab_free[("B", a)]])
                dve.wait(t_mm, t_rs, t_rsh, t_pool)
                nc.vector.tensor_tensor(out=ubuf[:, 16:528], in0=A[a][:, :], in1=rs[:], op=ALU.mult)
                t_u = dve.mark(nc.vector.tensor_tensor(out=ubuf[:, 0:16], in0=B[a][:, 0:16], in1=rsh[:], op=ALU.mult))
                ab_free[("A", a)] = t_u
                ab_free[("B", a)] = t_u
                dve.wait(t_u)
                t_r = dve.mark(nc.vector.tensor_tensor(out=ra[:, 1:528], in0=ubuf[:, 1:528], in1=ubuf[:, 0:527], op=ALU.add))
                r = ra
                if g >= 1:
                    dve.wait(t_r)
                    t_r = dve.mark(nc.vector.tensor_tensor(out=rb[:, 3:528], in0=ra[:, 3:528], in1=ra[:, 1:526], op=ALU.add))
                    r = rb
                if g >= 2:
                    dve.wait(t_r)
                    t_r = dve.mark(nc.vector.tensor_tensor(out=ra[:, 7:528], in0=rb[:, 7:528], in1=rb[:, 3:524], op=ALU.add))
                    r = ra
                if g >= 3:
                    dve.wait(t_r)
                    t_r = dve.mark(nc.vector.tensor_tensor(out=rb[:, 15:528], in0=ra[:, 15:528], in1=ra[:, 7:520], op=ALU.add))
                    r = rb
                w = (2, 4, 8, 16)[g]
                dve.wait(t_r)
                nc.vector.scalar_tensor_tensor(out=pooledT[:, nb, 16:512], in0=r[:, 32:528], scalar=1.0 / w,
                                               in1=ubuf[:, 32:528], op0=ALU.mult, op1=ALU.subtract)
                t_a = dve.mark(nc.vector.tensor_tensor(out=t16[:], in0=r[:, 16:32], in1=icv[:, g, :], op=ALU.mult))
                dve.wait(t_a)
                t_pool = dve.mark(nc.vector.tensor_tensor(out=pooledT[:, nb, 0:16], in0=t16[:], in1=ubuf[:, 16:32],
                                                          op=ALU.subtract))
            mixedT = aux
            pe.wait(t_pool)
            act.wait(fr("aux"))
            for nb in range(PC):
                g = nb // CPG
                a = nextAB()
                t_mm = lin(w_grp[nb], CPG, [(A[a][:, :], lambda kc, g=g: pooledT[:, g * CPG + kc, :])],
                           extra_wait=ab_free[("A", a)])
                act.wait(t_mm)
                t_mx = act.mark(nc.scalar.activation(out=mixedT[:, nb, :], in_=A[a][:, :], func=AF.Copy,
                                                     scale=g_sb[:, 5, nb:nb + 1]))
                ab_free[("A", a)] = t_mx
            pe.wait(t_mx)
            dve.wait(t_mm)
            for nb in range(KC):
                a = nextAB()
                t_a = lin(w_bpool[nb], PC, [(A[a][:, :], lambda kc: mixedT[:, kc, :])], extra_wait=ab_free[("A", a)])
                t_b = lin(w_gpool[nb], KC, [(B[a][:, :], lambda kc: hT[:, kc, :])], extra_wait=ab_free[("B", a)])
                f = ft[nb % 2]
                dve.wait(t_b, fr(("ft", nb % 2)))
                t_g = dve.mark(nc.vector.tensor_tensor(out=f[:], in0=B[a][:, :], in1=rs[:], op=ALU.mult))
                ab_free[("B", a)] = t_g
                act.wait(t_g)
                t_s = act.mark(nc.scalar.activation(out=f[:], in_=f[:], func=AF.Sigmoid))
                dve.wait(t_s, t_a)
                t_m = dve.mark(nc.vector.tensor_tensor(out=MT[:, nb, :], in0=f[:], in1=A[a][:, :], op=ALU.mult))
                ab_free[("A", a)] = t_m
                free[("ft", nb % 2)] = t_m
            sp.wait(t_a)
            t_at = k.dma(sp, aux[:], attn_in.rearrange("(h q) t -> q h t", q=128)[:, :, tc0:tc0 + T], s_m)
            pe.wait(t_at)
            for nb in range(KC):
                a = nextAB()
                t_a = lin(w_bsb[nb], 16, [(A[a][:, :], lambda kc: aux[:, kc, :])], extra_wait=ab_free[("A", a)])
                t_b = lin(w_gsb[nb], KC, [(B[a][:, :], lambda kc: hT[:, kc, :])], extra_wait=ab_free[("B", a)])
                f = ft[nb % 2]
                dve.wait(t_b, fr(("ft", nb % 2)))
                t_g = dve.mark(nc.vector.tensor_tensor(out=f[:], in0=B[a][:, :], in1=rs[:], op=ALU.mult))
                ab_free[("B", a)] = t_g
                act.wait(t_g)
                t_s = act.mark(nc.scalar.activation(out=f[:], in_=f[:], func=AF.Sigmoid))
                dve.wait(t_s, t_a)
                t_m0 = dve.mark(nc.vector.tensor_tensor(out=f[:], in0=f[:], in1=A[a][:, :], op=ALU.mult))
                ab_free[("A", a)] = t_m0
                dve.wait(t_m0)
                t_m = dve.mark(nc.vector.tensor_tensor(out=MT[:, nb, :], in0=MT[:, nb, :], in1=f[:], op=ALU.add))
                free[("ft", nb % 2)] = t_m
            free["aux"] = t_a
            free["hT"] = t_b
            free["rs"] = t_g
            pe.wait(t_m)
            t_rs, t_h = residual_stage(p, KC, w_out, KC, lambda kc: MT[:, kc, :], lambda nb: xin[p, nb], xr1, 1)
            pe.wait(t_h)
            for hd in range(4):
                a = nextAB()
                t_mm = lin(w_xq[hd], KC, [(A[a][:, :], lambda kc: hT[:, kc, :])], extra_wait=ab_free[("A", a)])
                dve.wait(t_mm, t_rs, fr("qx"))
                t_q = dve.mark(nc.vector.scalar_tensor_tensor(out=qx[:, hd, :], in0=A[a][:, :], scalar=xa_scale, in1=rs[:],
                                                              op0=ALU.mult, op1=ALU.mult))
                ab_free[("A", a)] = t_q
            free["hT"] = t_mm
            free["rs"] = t_q
            pe.wait(t_q, t_mem_done)
            for hd in range(4):
                pe.wait(fr("X0"), fr("X1"))
                for mc in range(2):
                    ins = nc.tensor.matmul(X[mc][:, :], lhsT=kmT[:, hd, mc * 128:(mc + 1) * 128], rhs=qx[:, hd, :], start=True,
                                           stop=True)
                t_sc = pe.mark(ins)
                act.wait(t_sc, fr("pT"))
                for mc in range(2):
                    t_p = act.mark(nc.scalar.activation(out=pT[:, mc, :], in_=X[mc][:, :], func=AF.Exp))
                free["X0"] = t_p
                free["X1"] = t_p
                a = nextAB()
                pe.wait(t_p, ab_free[("A", a)], ab_free[("B", a)])
                for mc in range(2):
                    nc.tensor.matmul(B[a][:, :], lhsT=ones_b[:], rhs=pT[:, mc, :], start=(mc == 0), stop=(mc == 1))
                for mc in range(2):
                    ins = nc.tensor.matmul(A[a][:, :], lhsT=vm[:, mc, hd * 128:(hd + 1) * 128], rhs=pT[:, mc, :],
                                           start=(mc == 0), stop=(mc == 1))
                t_o = pe.mark(ins)
                free["pT"] = t_o
                f = ft[hd % 2]
                dve.wait(t_o, fr(("ft", hd % 2)), fr("ox"))
                t_rd = dve.mark(nc.vector.reciprocal(out=f[:], in_=B[a][:, :]))
                dve.wait(t_rd)
                t_ox = dve.mark(nc.vector.tensor_tensor(out=ox[:, hd, :], in0=A[a][:, :], in1=f[:], op=ALU.mult))
                ab_free[("A", a)] = t_ox
                ab_free[("B", a)] = t_ox
                free[("ft", hd % 2)] = t_ox
            free["qx"] = t_sc
            pe.wait(t_ox)
            src1 = lambda nb: xr1[nb]
            for nb in range(KC):
                st_tok[("src", id(src1), nb)] = st_tok[(id(xr1), nb)]
            t_rs, t_h = residual_stage(p, KC, w_xo, 4, lambda kc: ox[:, kc, :], src1, xr2, 3)
            free["ox"] = w_free[(wctr[0] - 1) % NWS]
            qp = aux
            pe.wait(t_h)
            dve.wait(fr("aux"))
            for nb in range(16):
                a = nextAB()
                t_mm = lin(w_pq[nb], KC, [(A[a][:, :], lambda kc: hT[:, kc, :])], extra_wait=ab_free[("A", a)])
                dve.wait(t_mm, t_rs)
                t_q = dve.mark(nc.vector.tensor_tensor(out=qp[:, nb, :], in0=A[a][:, :], in1=rs[:], op=ALU.mult))
                ab_free[("A", a)] = t_q
            MTf = MT[:].rearrange("q a b -> q (a b)").bitcast(F32)
            sc = MTf[:, 0:2048]
            sc2 = MTf[:, 2048:4096]
            cand = MTf[:, 4096:6144]
            pe.wait(t_q)
            dve.wait(fr("MT"), t_m)
            act.wait(t_m)
            t_tk = None
            for tt in range(4):
                tsl = slice(tt * 128, (tt + 1) * 128)
                pe.wait(fr("X0"), fr("X1"), fr("X2"), fr("SS"))
                banks = [X[0], X[1], X[2], SS]
                for l in range(16):
                    ins = nc.tensor.matmul(banks[l // 4][:, (l % 4) * 128:(l % 4 + 1) * 128], lhsT=qp[:, l, tsl],
                                           rhs=keys_b[:, l, :], start=True, stop=True)
                t_s = pe.mark(ins)
                act.wait(t_s, t_tk)
                for bi in range(4):
                    t_cp = act.mark(nc.scalar.copy(out=sc[:, bi * 512:(bi + 1) * 512], in_=banks[bi][:, :]))
                for nm in ("X0", "X1", "X2", "SS"):
                    free[nm] = t_cp
                dve.wait(t_cp)
                for l in range(16):
                    row = sc[:, l * 128:(l + 1) * 128]
                    row2 = sc2[:, l * 128:(l + 1) * 128]
                    t1 = dve.mark(nc.vector.max(out=top[:, l, 0:8], in_=row))
                    dve.wait(t1)
                    t2 = dve.mark(nc.vector.match_replace(out=row2, in_to_replace=top[:, l, 0:8], in_values=row, imm_value=NEG))
                    dve.wait(t2)
                    t3 = dve.mark(nc.vector.max(out=top[:, l, 8:16], in_=row2))
                dve.wait(t3)
                top4 = top[:].rearrange("q (h two) a -> q h two a", two=2)
                cand4 = cand[:, 0:2048].rearrange("q (h a b) -> q h a b", h=8, a=16)
                t_cd = dve.mark(nc.vector.tensor_tensor(out=cand4, in0=bc(top4[:, :, 0, :], 3, [128, 8, 16, 16]),
                                                        in1=bc(top4[:, :, 1, :], 2, [128, 8, 16, 16]), op=ALU.add))
                dve.wait(t_cd)
                for h in range(8):
                    row = cand[:, h * 256:(h + 1) * 256]
                    row2 = sc2[:, h * 256:(h + 1) * 256]
                    t1 = dve.mark(nc.vector.max(out=best[:, h, 0:8], in_=row))
                    dve.wait(t1)
                    t2 = dve.mark(nc.vector.match_replace(out=row2, in_to_replace=best[:, h, 0:8], in_values=row, imm_value=NEG))
                    dve.wait(t2)
                    t3 = dve.mark(nc.vector.max(out=best[:, h, 8:16], in_=row2))
                dve.wait(t3)
                dd = sc2[:, 0:128].rearrange("q (h a) -> q h a", h=8)
                t_d = dve.mark(nc.vector.tensor_tensor(out=dd, in0=best[:], in1=bc(best[:, :, 0], 2, [128, 8, 16]),
                                                       op=ALU.subtract))
                act.wait(t_d)
                t_e = act.mark(nc.scalar.activation(out=dd, in_=dd, func=AF.Exp))
                dve.wait(t_e)
                t_z = dve.mark(nc.vector.reduce_sum(out=zz[:, :, 0], in_=dd, axis=AX.X))
                act.wait(t_z)
                t_lz = act.mark(nc.scalar.activation(out=zz[:, :, 1], in_=zz[:, :, 0], func=AF.Ln))
                dve.wait(t_lz)
                nc.vector.scalar_tensor_tensor(out=negb[:, tt, :], in0=best[:, :, 0], scalar=-1.0, in1=zz[:, :, 1],
                                               op0=ALU.mult, op1=ALU.subtract)
                t_tk = dve.mark(nc.vector.tensor_scalar(out=tau[:, tt, :], in0=best[:, :, 15], scalar1=-2e-5, scalar2=None,
                                                        op0=ALU.add))
            act.wait(t_tk)
            WT = MT
            gctr = 0
            free[("S", 0)] = fr("X0")
            free[("S", 1)] = fr("SS")
            free["Gs"] = [fr("X1"), fr("X2")]
            for qtr in range(4):
                for grp in range(16):
                    i0 = qtr * 32 + grp * 2
                    Gs = [X[1], X[2]]
                    gits = [(tt, h) for tt in range(4) for h in range(8)]
                    grec = [dict() for _ in gits]

                    def gA(ii):
                        tt, h = gits[ii]
                        tsl = slice(tt * 128, (tt + 1) * 128)
                        it = gbase + ii
                        sb_ = it % 2
                        sbank = [X[0], SS][sb_]
                        pe.wait(fr(("S", sb_)))
                        nc.tensor.matmul(sbank[:, 0:256], lhsT=qp[:, 2 * h, tsl],
                                         rhs=bc(keys_b[:, 2 * h, i0:i0 + 2], 2, [128, 2, 128]), start=True, stop=False)
                        t_S = pe.mark(nc.tensor.matmul(sbank[:, 0:256], lhsT=qp[:, 2 * h + 1, tsl],
                                                       rhs=bc(keys_b[:, 2 * h + 1, :], 1, [128, 2, 128]), start=False, stop=True))
                        e_t = ft[2 + sb_]
                        act.wait(t_S, fr(("et", sb_)))
                        t_E = act.mark(nc.scalar.activation(out=e_t[:, 0:256], in_=sbank[:, 0:256], func=AF.Exp,
                                                            bias=negb[:, tt, h:h + 1]))
                        g_t = bt[it % 3]
                        dve.wait(t_E, fr(("gt", it % 3)))
                        t_G = dve.mark(nc.vector.scalar_tensor_tensor(out=g_t[:, 0:256], in0=sbank[:, 0:256],
                                                                      scalar=tau[:, tt, h:h + 1], in1=e_t[:, 0:256],
                                                                      op0=ALU.is_ge, op1=ALU.mult))
                        free[("S", sb_)] = t_G
                        free[("et", sb_)] = t_G
                        grec[ii]["G"] = t_G

                    def gB(ii):
                        tt, h = gits[ii]
                        tsl = slice(tt * 128, (tt + 1) * 128)
                        it = gbase + ii
                        g_t = bt[it % 3]
                        pe.wait(grec[ii]["G"], fr("Gs") if ii == 0 else None)
                        for ib in range(2):
                            ins = nc.tensor.matmul(Gs[ib][:, tsl], lhsT=g_t[:, ib * 128:(ib + 1) * 128], rhs=ident_b[:],
                                                   start=(h == 0), stop=(h == 7))
                        free[("gt", it % 3)] = pe.mark(ins)

                    gbase = gctr
                    for ii in range(len(gits) + 1):
                        if ii < len(gits):
                            gA(ii)
                        if ii >= 1:
                            gB(ii - 1)
                    gctr += len(gits)
                    t_Gs = free[("gt", (gctr - 1) % 3)]
                    for ib in range(2):
                        eb = i0 + ib
                        ebl = eb - qtr * 32
                        a = nextAB()
                        t_mm = lin(w_down[eb], KC, [(A[a][:, :], lambda kc: hT[:, kc, :])], extra_wait=ab_free[("A", a)])
                        f = ft[ib]
                        dve.wait(t_mm, t_rs, fr(("ft", ib)))
                        t_a = dve.mark(nc.vector.tensor_tensor(out=f[:], in0=A[a][:, :], in1=rs[:], op=ALU.mult))
                        ab_free[("A", a)] = t_a
                        act.wait(t_a)
                        t_ge = act.mark(nc.scalar.activation(out=f[:], in_=f[:], func=AF.Gelu))
                        dve.wait(t_ge, t_Gs, fr("WT"))
                        t_w = dve.mark(nc.vector.tensor_tensor(out=WT[:, ebl, :], in0=f[:], in1=Gs[ib][:, :], op=ALU.mult))
                        free[("ft", ib)] = t_w
                    free["Gs"] = t_w
                pe.wait(t_w)
                free["SS"] = free[("S", 1)]
                free["X0"] = free[("S", 0)]
                free["X1"] = t_w
                free["X2"] = t_w
                last = (qtr == 3)
                if qtr == 0:
                    srcq = lambda nb: xr2[nb]
                    for nb in range(KC):
                        st_tok[("src", id(srcq), nb)] = st_tok[(id(xr2), nb)]
                else:
                    srcq = lambda nb: xr3[nb]
                    for nb in range(KC):
                        st_tok[("src", id(srcq), nb)] = st_tok[(id(xr3), nb)]
                t_pe_last = None
                pend = None
                for nb in range(KC):
                    a = nextAB()
                    t_mm = lin(w_up[qtr, nb], 32, [(A[a][:, :], lambda kc: WT[:, kc, :])], extra_wait=ab_free[("A", a)])
                    if pend is not None:
                        stats_mm(pend, nb - 1)
                        pend = None
                    s, t_x = load_x(srcq(nb), extra=st_tok.get(("src", id(srcq), nb)))
                    dve.wait(t_mm, t_x)
                    t_add = dve.mark(nc.vector.tensor_tensor(out=xc[s][:], in0=A[a][:, :], in1=xc[s][:], op=ALU.add))
                    ab_free[("A", a)] = t_add
                    sp.wait(t_add)
                    t_st = k.dma(sp, xr3[nb], xc[s][:], s_m)
                    st_tok[(id(xr3), nb)] = t_st
                    if last:
                        pend = stats_sq(xc[s][:], t_add)
                        x_free[s] = [t_st, pend[1]]
                        if nb == KC - 1:
                            t_pe_last = stats_mm(pend, nb)
                    else:
                        x_free[s] = [t_st]
                free["WT"] = t_mm
            free["hT"] = t_mm
            free["MT"] = t_mm
            free["aux"] = t_Gs
            t_rs, t_rs0 = make_rstd(rs[:], SS[:, :], t_pe_last, guard=[fr("rs"), t_a])
            free["SS"] = t_rs0
            for nb in range(KC):
                s, t_x = load_x(xr3[nb], extra=st_tok[(id(xr3), nb)])
                dve.wait(t_x, t_rs)
                t_o = dve.mark(nc.vector.scalar_tensor_tensor(out=xc[s][:], in0=xc[s][:], scalar=g_sb[:, 4, nb:nb + 1], in1=rs[:],
                                                              op0=ALU.mult, op1=ALU.mult))
                sp.wait(t_o)
                t_st = k.dma(sp, outT[p, nb], xc[s][:], s_m)
                x_free[s] = [t_st]
            free["rs"] = t_o
        sp.wait((s_m, s_m.count))
    return nc


def tile_w(W):
    K_, N_ = W.shape
    return np.ascontiguousarray(W.reshape(K_ // 128, 128, N_ // 128, 128).transpose(2, 1, 0, 3))


def vec_pk(v, KC):
    o = np.zeros((128, KC), np.float32)
    n = v.shape[0] // 128
    o[:, :n] = v.reshape(n, 128).T
    return o


def make_cst():
    s_idx = np.arange(128)
    cst = np.zeros((128, 384), np.float32)
    cst[:, 0:128] = (s_idx[:, None] < s_idx[None, :])
    cst[:, 128:256] = -1.0 * (s_idx[:, None] >= s_idx[None, :])
    cst[:, 256:384] = np.eye(128)
    return cst


def host_phase2_shared(inp, D):
    KC = D // 128
    SBW = 2048
    PW = D // 2
    PC = PW // 128
    w_in = inp["w_in"][0]
    o = 3 * SBW
    sh = {}
    sh["w_pool"] = tile_w(w_in[:, o:o + PW])
    sh["w_gsb"] = tile_w(w_in[:, o + PW:o + PW + D])
    sh["w_gpool"] = tile_w(w_in[:, o + PW + D:o + PW + 2 * D])
    sh["w_grp"] = np.concatenate([tile_w(inp["pool_group_w"][0, g]) for g in range(4)], axis=0)
    sh["w_bsb"] = tile_w(inp["w_branch_sb"][0])
    sh["w_bpool"] = tile_w(inp["w_branch_pool"][0])
    sh["w_out"] = tile_w(inp["w_out"][0])
    sh["w_xq"] = tile_w(inp["xa_w_q"][0])
    sh["w_xkv"] = tile_w(inp["xa_w_kv"][0])
    sh["w_xo"] = tile_w(inp["xa_w_o"][0])
    sh["w_pq"] = tile_w(inp["peer_w_query"][0])
    sh["w_down"] = tile_w(inp["peer_down"][0].T)
    sh["w_up"] = np.stack([tile_w(inp["peer_up"][0][q * 4096:(q + 1) * 4096]) for q in range(4)], axis=0)
    sh["keysT"] = np.ascontiguousarray(inp["peer_sub_keys"][0].transpose(3, 0, 1, 2).reshape(128, 16, 128))
    g = np.zeros((128, 6, KC), np.float32)
    g[:, 0] = vec_pk(inp["norm_mix"][0], KC)
    g[:, 1] = vec_pk(inp["norm_mem_q"][0], KC)
    g[:, 2] = vec_pk(inp["norm_mem_kv"][0], KC)
    g[:, 3] = vec_pk(inp["norm_ffn"][0], KC)
    g[:, 4] = vec_pk(inp["norm_final"], KC)
    g[:, 5] = vec_pk(inp["pool_scale"][0], KC)
    sh["gains"] = g
    sh["cst"] = make_cst()
    mem = inp["mem"][0]
    sh["memT"] = np.ascontiguousarray(mem.reshape(256, KC, 128).transpose(1, 2, 0))
    return sh


def host_phase2_core(x, attn_full, tok0, TOK, D):
    KC = D // 128
    NP = TOK // 512
    xs = x[tok0:tok0 + TOK]
    m = {}
    m["xin"] = np.ascontiguousarray(xs.reshape(NP, 512, KC, 128).transpose(0, 2, 3, 1))
    xh = np.zeros((NP, 128, KC, 16), np.float32)
    ic = np.zeros((NP, 128, 4, 16), np.float32)
    for p in range(NP):
        st = tok0 + p * 512
        if st >= 16:
            xh[p] = x[st - 16:st].reshape(16, KC, 128).transpose(2, 1, 0)
        pos = st + np.arange(16)
        for gi, w in enumerate((2, 4, 8, 16)):
            ic[p, :, gi, :] = 1.0 / np.minimum(pos + 1, w)
    m["xhalo"] = xh
    m["invc"] = ic
    m["attn_in"] = np.ascontiguousarray(attn_full[:, tok0:tok0 + TOK])
    return m


def unpack_out(outT, TOK, D):
    KC = D // 128
    NP = TOK // 512
    return np.ascontiguousarray(outT.reshape(NP, KC, 128, 512).transpose(0, 3, 1, 2)).reshape(TOK, D)


def kernel(**inp):
    S, D = 8192, 4096
    inp = {k_: np.asarray(v) for k_, v in inp.items()}
    nc = bass.Bass("TRN2", target_bir_lowering=False)
    build_fused(nc, S, D)
    maps, owners = host_fused_inputs(inp, S, D)
    r = run_bass_kernel_spmd(nc, maps, core_ids=list(range(NCORES)))
    out = np.zeros((S, D), np.float32)
    for c in range(NCORES):
        o = unpack_out(r.results[c]["outT"], 1024, D)
        for p, g in enumerate(owners[c]):
            out[g * 512:(g + 1) * 512] = o[p * 512:(p + 1) * 512]
    return out.reshape(1, S, D)


def declare_tensors(nc, S, TOK, D):
    KC = D // 128
    NT = S // 128
    NP = TOK // 512
    PC = (D // 2) // 128
    CPG = PC // 4
    T = 512

    def din(name, shape, dt=F32):
        return nc.dram_tensor(name, shape, dt, kind="ExternalInput").ap()
    t = {}
    t["xT"] = din("xT", [NT, 128, KC, 128])
    t["wqkv"] = din("wqkv", [8, 128, KC, 768])
    t["bias_tab"] = din("bias_tab", [2, NT, 128, 512], BF16)
    t["xin"] = din("xin", [NP, KC, 128, T])
    t["xhalo"] = din("xhalo", [NP, 128, KC, 16])
    t["invc"] = din("invc", [NP, 128, 4, 16])
    t["memT"] = din("memT", [KC, 128, 256])
    t["gains"] = din("gains", [128, 6, KC])
    t["cst"] = din("cst", [128, 384])
    t["keysT"] = din("keysT", [128, 16, 128])
    t["w_pool"] = din("w_pool", [PC, 128, KC, 128])
    t["w_grp"] = din("w_grp", [PC, 128, CPG, 128])
    t["w_gsb"] = din("w_gsb", [KC, 128, KC, 128])
    t["w_gpool"] = din("w_gpool", [KC, 128, KC, 128])
    t["w_bsb"] = din("w_bsb", [KC, 128, 16, 128])
    t["w_bpool"] = din("w_bpool", [KC, 128, PC, 128])
    t["w_out"] = din("w_out", [KC, 128, KC, 128])
    t["w_xq"] = din("w_xq", [4, 128, KC, 128])
    t["w_xkv"] = din("w_xkv", [8, 128, KC, 128])
    t["w_xo"] = din("w_xo", [KC, 128, 4, 128])
    t["w_pq"] = din("w_pq", [16, 128, KC, 128])
    t["w_down"] = din("w_down", [128, 128, KC, 128])
    t["w_up"] = din("w_up", [4, KC, 128, 32, 128])
    t["outT"] = nc.dram_tensor("outT", [NP, KC, 128, T], F32, kind="ExternalOutput").ap()
    t["attn_scr"] = nc.dram_tensor("attn_scr", [16 * 128, TOK], BF16).ap()
    return t


def build_fused(nc, S, D):
    TOK = 1024
    t = declare_tensors(nc, S, TOK, D)
    with contextlib.ExitStack() as sem_stack:
        k = K(nc, None, sem_stack)
        build_phase1(nc, k, S, D, t)
        build_phase2(nc, k, TOK, D, t)
    return nc


def host_fused_inputs(inp, S, D):
    KC = D // 128
    NT = S // 128
    NG = S // 512
    x = inp["x"][0]
    sh = host_phase2_shared(inp, D)
    sh["xT"] = np.ascontiguousarray(x.reshape(NT, 128, KC, 128).transpose(0, 3, 2, 1))
    w_in = inp["w_in"][0]
    SBW = 2048
    wq = np.zeros((8, 128, KC, 768), np.float32)
    for hp in range(8):
        cols = []
        for part in range(3):
            for hh in range(2):
                h = hp * 2 + hh
                cols.append(w_in[:, part * SBW + h * 128: part * SBW + (h + 1) * 128])
        w = np.concatenate(cols, axis=1)
        wq[hp] = w.reshape(KC, 128, -1).transpose(1, 0, 2)
    sh["wqkv"] = wq
    maps = []
    owners = []
    s_idx = np.arange(128)
    t_idx = np.arange(512)
    for c in range(NCORES):
        groups = (c, NG - 1 - c)
        owners.append(groups)
        m = dict(sh)
        xs = np.concatenate([x[g * 512:(g + 1) * 512] for g in groups], axis=0)
        m["xin"] = np.ascontiguousarray(xs.reshape(2, 512, KC, 128).transpose(0, 2, 3, 1))
        xh = np.zeros((2, 128, KC, 16), np.float32)
        ic = np.zeros((2, 128, 4, 16), np.float32)
        bt = np.zeros((2, NT, 128, 512), np.float32)
        for p, g in enumerate(groups):
            st = g * 512
            if st >= 16:
                xh[p] = x[st - 16:st].reshape(16, KC, 128).transpose(2, 1, 0)
            pos = st + np.arange(16)
            for gi, w in enumerate((2, 4, 8, 16)):
                ic[p, :, gi, :] = 1.0 / np.minimum(pos + 1, w)
            qpos = st + t_idx
            for kb in range(NT):
                kpos = kb * 128 + s_idx
                bt[p, kb] = np.where(kpos[:, None] < qpos[None, :], 0.0, -30000.0)
        m["xhalo"] = xh
        m["invc"] = ic
        m["bias_tab"] = bt.astype(ml_dtypes.bfloat16)
        maps.append(m)
    return maps, owners
```

```python
import contextlib
import numpy as np
import ml_dtypes
import concourse.bass as bass
import concourse.mybir as mybir
from concourse.bass_utils import run_bass_kernel_spmd

F32, BF16 = mybir.dt.float32, mybir.dt.bfloat16
AF = mybir.ActivationFunctionType
ALU = mybir.AluOpType
AX = mybir.AxisListType

NCORES = 8
EPS = 1e-6
NEG = -1.0e30


class Sem:
    def __init__(self, handle):
        self.h = handle
        self.count = 0


class Eng:
    def __init__(self, k, eng, name):
        self.k, self.e, self.name = k, eng, name
        self.sem = k.new_sem(name)
        self.seen = {}

    def wait(self, *toks):
        for t in toks:
            if t is None:
                continue
            if isinstance(t, (list, tuple)) and len(t) and not isinstance(t[0], Sem):
                self.wait(*t)
                continue
            s, v = t
            if self.seen.get(id(s), 0) >= v:
                continue
            self.e.wait_ge(s.h, v)
            self.seen[id(s)] = v

    def mark(self, ins):
        ins.then_inc(self.sem.h, 1)
        self.sem.count += 1
        return (self.sem, self.sem.count)


class K:
    def __init__(self, nc, stack, sem_stack=None):
        self.nc, self.stack = nc, stack
        self.sem_stack = sem_stack if sem_stack is not None else stack
        self._sems = []
        self.pe = Eng(self, nc.tensor, "pe")
        self.act = Eng(self, nc.scalar, "act")
        self.dve = Eng(self, nc.vector, "dve")
        self.pool = Eng(self, nc.gpsimd, "pool")
        self.sp = Eng(self, nc.sync, "sp")

    def new_sem(self, name):
        s = Sem(self.sem_stack.enter_context(self.nc.semaphore(name + "_%d" % len(self._sems))))
        self._sems.append(s)
        return s

    def sbuf(self, name, shape, dt):
        return self.stack.enter_context(self.nc.sbuf_tensor(name, shape, dt))

    def psum(self, name, shape, dt=F32):
        return self.stack.enter_context(self.nc.psum_tensor(name, shape, dt))

    def dma(self, q, out, in_, sem):
        ins = q.e.dma_start(out=out, in_=in_)
        ins.then_inc(sem.h, 16)
        sem.count += 16
        return (sem, sem.count)


def bc(ap, axis, shape):
    return ap.unsqueeze(axis).broadcast_to(list(shape))


def build_phase1(nc, k, S, D, T_in):
    HPC = 2
    KC = D // 128
    NT = S // 128
    xT, xin, wqkv, gains, cst, bias_tab, attn_scr = (T_in[n] for n in
                                                     ("xT", "xin", "wqkv", "gains", "cst", "bias_tab", "attn_scr"))
    NKB = (NT // 2, NT)
    with contextlib.ExitStack() as st:
        k.stack = st
        nc_ = nc
        pe, act, dve, pool, sp = k.pe, k.act, k.dve, k.pool, k.sp
        W = k.sbuf("W", [128, KC, 3 * HPC * 128], BF16)
        KT = k.sbuf("KT", [128, HPC, S], BF16)
        V = k.sbuf("V", [128, NT, HPC * 128], BF16)
        QT = [k.sbuf("QT%d" % i, [128, HPC, 512], BF16) for i in range(2)]
        xs = [k.sbuf("xs%d" % i, [128, KC, 128], F32) for i in range(2)]
        sq = k.sbuf("sq", [128, KC, 128], BF16)
        hbl = [k.sbuf("hb%d" % i, [128, KC, 128], BF16) for i in range(2)]
        g_sb = k.sbuf("g1", [128, 6, KC], F32)
        c_sb = k.sbuf("c1", [128, 384], F32)
        ones_b = k.sbuf("ones1", [128, 128], BF16)
        nones_b = k.sbuf("nones", [128, 128], BF16)
        ntri_b = k.sbuf("ntri", [128, 128], BF16)
        ident_b = k.sbuf("ident1", [128, 128], BF16)
        rstdl = [k.sbuf("rstd%d" % i, [128, 132], F32) for i in range(2)]
        ebuf = [k.sbuf("eb%d" % i, [128, 512], F32) for i in range(3)]
        spb = [k.sbuf("sp%d" % i, [128, 512], BF16) for i in range(3)]
        atb = [k.sbuf("at%d" % i, [128, 512], BF16) for i in range(3)]
        bias = [k.sbuf("bias%d" % i, [128, 512], BF16) for i in range(3)]
        spacc = k.sbuf("spacc", [128, HPC, 512], BF16)
        ostage = [k.sbuf("os%d" % i, [128, 512], BF16) for i in range(2)]
        pz = [k.psum("pz%d" % i, [128, 512]) for i in range(3)]
        po = [k.psum("po%d" % i, [128, 512]) for i in range(HPC)]
        pqk = k.psum("pqk", [128, 512])
        pv = k.psum("pv", [128, 512])

        s_c = k.new_sem("ldc")
        s_w = k.new_sem("ldw")
        s_x = [k.new_sem("ldx") for _ in range(2)]
        s_o = [k.new_sem("sto") for _ in range(2)]
        s_b = [k.new_sem("ldb") for _ in range(3)]

        k.dma(sp, g_sb[:], gains, s_c)
        t_c = k.dma(sp, c_sb[:], cst, s_c)
        dve.wait(t_c)
        nc.vector.memset(ones_b[:], 1.0)
        nc.vector.memset(nones_b[:], -1.0)
        nc.vector.tensor_copy(out=ntri_b[:], in_=c_sb[:, 128:256])
        t_cst = dve.mark(nc.vector.tensor_copy(out=ident_b[:], in_=c_sb[:, 256:384]))
        pe.wait(t_cst)
        act.wait(t_cst)
        pool.wait(t_cst)

        scale = 128.0 ** -0.5
        st8 = {"x_free": [None, None], "sq_free": None, "hb_free": [None, None], "pqk_free": None, "pv_free": None,
               "rstd_free": [None, None], "xctr": 0, "scr_ready": None, "w_free": None, "kv_free": None, "last": None}
        qt_free = [None, None]

        hb_scr = nc.dram_tensor("hb_scr", [NT + 8, 128, KC, 128], BF16).ap()
        rs_scr = nc.dram_tensor("rs_scr", [NT + 8, 128, 132], F32).ap()
        s_hsl = [k.new_sem("sths") for _ in range(2)]
        s_r = [k.new_sem("ldr") for _ in range(2)]
        st8["rstd_free"] = [None, None]

        def project(src_ap, mode, idx, first, tt=None, slot=None, c0=None):
            d = st8
            sl = d["xctr"] % 2
            d["xctr"] += 1
            hb = hbl[sl]
            rstd = rstdl[sl]
            if first:
                sp.wait(d["x_free"][sl])
                t_x = k.dma(sp, xs[sl][:], src_ap, s_x[sl])
                act.wait(t_x, d["sq_free"])
                t_sq = act.mark(nc.scalar.activation(out=sq[:], in_=xs[sl][:], func=AF.Square))
                pool.wait(t_x, d["hb_free"][sl])
                t_hb = pool.mark(nc.gpsimd.tensor_tensor(out=hb[:], in0=xs[sl][:], in1=bc(g_sb[:, 0, :], 2, [128, KC, 128]),
                                                         op=ALU.mult))
                d["x_free"][sl] = [t_sq, t_hb]
                sp.wait(t_hb)
                t_hs = k.dma(sp, hb_scr[idx], hb[:], s_hsl[sl])
                pe.wait(t_sq, d["pv_free"])
                for kc in range(KC):
                    nc.tensor.matmul(pv[:, 256:384], lhsT=ones_b[:], rhs=sq[:, kc, :], start=(kc == 0), stop=(kc == KC - 1))
                for kc in range(KC):
                    ins = nc.tensor.matmul(pv[:, 384:385], lhsT=sq[:, kc, :], rhs=ones_b[:, 0:1], start=(kc == 0),
                                           stop=(kc == KC - 1))
                t_ss = pe.mark(ins)
                d["sq_free"] = t_ss
                dve.wait(t_ss, d["rstd_free"][sl])
                t_r00 = dve.mark(nc.vector.tensor_scalar(out=rstd[:, 0:129], in0=pv[:, 256:385], scalar1=1.0 / D, scalar2=EPS,
                                                         op0=ALU.mult, op1=ALU.add))
                act.wait(t_r00)
                t_r01 = act.mark(nc.scalar.activation(out=rstd[:, 0:129], in_=rstd[:, 0:129], func=AF.Ln))
                act.wait(t_r01)
                t_r0 = act.mark(nc.scalar.activation(out=rstd[:, 0:129], in_=rstd[:, 0:129], func=AF.Exp, scale=-0.5))
                sp.wait(t_r0)
                t_rs = k.dma(sp, rs_scr[idx][:, 0:129], rstd[:, 0:129], s_hsl[sl])
                pv_rel = [t_r00]
                extra_free = [t_hs, t_rs]
            else:
                sp.wait(d["hb_free"][sl], d["rstd_free"][sl], d["scr_ready"])
                t_hb = k.dma(sp, hb[:], hb_scr[idx], s_x[sl])
                t_r0 = k.dma(sp, rstd[:, 0:129], rs_scr[idx][:, 0:129], s_r[sl])
                pv_rel = []
                extra_free = []
            pe.wait(t_hb, d["pqk_free"], d["w_ready"])
            blks = range(HPC, 2 * HPC) if mode == "kv" else range(0, HPC)
            for blk in blks:
                for kc in range(KC):
                    ins = nc.tensor.matmul(pqk[:, blk * 128:(blk + 1) * 128], lhsT=W[:, kc, blk * 128:(blk + 1) * 128],
                                           rhs=hb[:, kc, :], start=(kc == 0), stop=(kc == KC - 1))
            t_qk = pe.mark(ins)
            t_v = None
            if mode == "kv":
                pe.wait(d["pv_free"])
                for kc in range(KC):
                    ins = nc.tensor.matmul(pv[:, 0:HPC * 128], lhsT=hb[:, kc, :], rhs=W[:, kc, 2 * HPC * 128:3 * HPC * 128],
                                           start=(kc == 0), stop=(kc == KC - 1))
                t_v = pe.mark(ins)
            d["hb_free"][sl] = [t_v if t_v is not None else t_qk] + extra_free
            d["w_last"] = t_v if t_v is not None else t_qk
            if mode == "kv":
                dve.wait(t_qk, t_r0, d["kv_free"])
                for hh in range(HPC):
                    ins = nc.vector.tensor_tensor(out=KT[:, hh, tt * 128:(tt + 1) * 128],
                                                  in0=pqk[:, (HPC + hh) * 128:(HPC + hh + 1) * 128], in1=rstd[:, 0:128],
                                                  op=ALU.mult)
                t_e1 = dve.mark(ins)
                act.wait(t_v, t_r0, d["kv_free"])
                t_e2 = act.mark(nc.scalar.activation(out=V[:, tt, :], in_=pv[:, 0:HPC * 128], func=AF.Copy,
                                                     scale=rstd[:, 128:129]))
                d["pv_free"] = [t_e2] + pv_rel
                d["rstd_free"][sl] = [t_e1, t_e2] + extra_free
                d["last"] = [t_e1, t_e2]
                d["last_kv"] = [t_e1, t_e2]
            else:
                dve.wait(t_qk, t_r0, qt_free[slot])
                for hh in range(HPC):
                    ins = nc.vector.scalar_tensor_tensor(out=QT[slot][:, hh, c0:c0 + 128], in0=pqk[:, hh * 128:(hh + 1) * 128],
                                                         scalar=scale, in1=rstd[:, 0:128], op0=ALU.mult, op1=ALU.mult)
                t_e1 = dve.mark(ins)
                if pv_rel:
                    d["pv_free"] = pv_rel
                d["rstd_free"][sl] = [t_e1] + extra_free
                d["last"] = [t_e1]
            d["pqk_free"] = t_e1

        it_ctr = [0]
        pz_free = [None] * 3
        sp_free = [None] * 3
        at_free = [None] * 3
        b_free = [None] * 3
        b_ctr = [0]
        po_free = [None] * HPC
        spacc_tok = [None] * HPC
        os_free = [None, None]
        o_ctr = [0]
        out_toks = []

        def attention(hp, slot):
            nkb = NKB[slot]
            qt = QT[slot]
            its = [(kb, hh) for kb in reversed(range(nkb)) for hh in range(HPC)]
            n = len(its)
            stt = [dict() for _ in range(n)]
            pe.wait(st8["last"], st8["last_kv"])
            pool.wait(st8["last"], st8["last_kv"])
            for hh in range(HPC):
                pool.wait(spacc_tok[hh])
            t_z = pool.mark(nc.gpsimd.memset(spacc[:], 0.0))
            for hh in range(HPC):
                spacc_tok[hh] = t_z
            last_av = [None] * HPC
            btile = {}

            def st0(i):
                kb, hh = its[i]
                b = (it_ctr[0] + i) % 3
                d = stt[i]
                d["b"] = b
                if hh == 0:
                    bs = b_ctr[0] % 3
                    b_ctr[0] += 1
                    sp.wait(b_free[bs])
                    btile[kb] = (bs, k.dma(sp, bias[bs][:], bias_tab[slot, kb], s_b[bs]))
                bs, t_b = btile[kb]
                pe.wait(pz_free[b], t_b)
                nc.tensor.matmul(pz[b][:, :], lhsT=KT[:, hh, kb * 128:(kb + 1) * 128], rhs=qt[:, hh, :], start=True, stop=False)
                d["z"] = pe.mark(nc.tensor.matmul(pz[b][:, :], lhsT=ident_b[:], rhs=bias[bs][:], start=False, stop=False))
                if hh == HPC - 1:
                    b_free[bs] = d["z"]
                act.wait(d["z"], sp_free[b])
                t_e = act.mark(nc.scalar.activation(out=ebuf[b][:], in_=pz[b][:, :], func=AF.Exp))
                act.wait(t_e)
                d["ln"] = act.mark(nc.scalar.activation(out=spb[b][:], in_=ebuf[b][:], func=AF.Ln, bias=1.0))

            def st1(i):
                kb, hh = its[i]
                d = stt[i]
                b = d["b"]
                first = (kb == nkb - 1)
                pe.wait(d["ln"], spacc_tok[hh])
                ins = nc.tensor.matmul(pz[b][:, :], lhsT=ntri_b[:], rhs=spb[b][:], start=False, stop=first)
                if not first:
                    ins = nc.tensor.matmul(pz[b][:, :], lhsT=nones_b[:], rhs=spacc[:, hh, :], start=False, stop=True)
                d["cs"] = pe.mark(ins)
                act.wait(d["cs"], at_free[b])
                d["ea"] = act.mark(nc.scalar.activation(out=atb[b][:], in_=pz[b][:, :], func=AF.Exp))
                pz_free[b] = d["ea"]
                pool.wait(d["cs"], d["ln"], spacc_tok[hh])
                spacc_tok[hh] = pool.mark(nc.gpsimd.tensor_tensor(out=spacc[:, hh, :], in0=spacc[:, hh, :], in1=spb[b][:],
                                                                  op=ALU.add))
                sp_free[b] = spacc_tok[hh]

            def st2(i):
                kb, hh = its[i]
                d = stt[i]
                b = d["b"]
                pe.wait(d["ea"], po_free[hh] if kb == nkb - 1 else None)
                d["av"] = pe.mark(nc.tensor.matmul(po[hh][:, :], lhsT=V[:, kb, hh * 128:(hh + 1) * 128], rhs=atb[b][:],
                                                   start=(kb == nkb - 1), stop=(kb == 0)))
                at_free[b] = d["av"]
                last_av[hh] = d["av"]

            for step in range(n + 2):
                if step < n:
                    st0(step)
                if 0 <= step - 1 < n:
                    st1(step - 1)
                if 0 <= step - 2 < n:
                    st2(step - 2)
            it_ctr[0] += n
            qt_free[slot] = [last_av[hh] for hh in range(HPC)]
            st8["kv_free"] = [last_av[hh] for hh in range(HPC)]
            for hh in range(HPC):
                o = o_ctr[0] % 2
                o_ctr[0] += 1
                act.wait(last_av[hh], os_free[o])
                t_cp = act.mark(nc.scalar.copy(out=ostage[o][:], in_=po[hh][:, :]))
                po_free[hh] = t_cp
                sp.wait(t_cp)
                h = hp * HPC + hh
                os_free[o] = k.dma(sp, attn_scr[h * 128:(h + 1) * 128, slot * 512:(slot + 1) * 512], ostage[o][:], s_o[o])
                out_toks.append(os_free[o])

        st8["w_ready"] = None
        st8["w_last"] = None
        for hp in range(8):
            pool.wait(st8["w_last"], st8["kv_free"])
            for i in range(3 * HPC):
                t_w = k.dma(pool, W[:, :, i * 128:(i + 1) * 128], wqkv[hp][:, :, i * 128:(i + 1) * 128], s_w)
            st8["w_ready"] = t_w
            for tt in range(NT):
                project(xT[tt], "kv", tt, hp == 0, tt=tt)
            for j in range(8):
                slot, c0 = j // 4, (j % 4) * 128
                project(xin[slot][:, :, c0:c0 + 128].rearrange("kc q t -> q kc t"), "q", NT + j, hp == 0, slot=slot, c0=c0)
            if hp == 0:
                st8["scr_ready"] = [(s_, s_.count) for s_ in s_hsl]
            for slot in range(2):
                attention(hp, slot)
        fin = [(e.sem, e.sem.count) for e in (pe, act, dve, pool)] + [(s_, s_.count) for s_ in s_o]
        for e in (pe, act, dve, pool, sp):
            e.wait(fin)
    return nc


def host_phase1_inputs(x, w_in, norm_mix, S, D, HPC, ncores):
    KC = D // 128
    NT = S // 128
    SBW = ncores * HPC * 128
    xT = np.ascontiguousarray(x.reshape(NT, 128, KC, 128).transpose(0, 3, 2, 1))
    g = np.ascontiguousarray(norm_mix.reshape(KC, 128).T)
    s_idx = np.arange(128)
    cst = np.zeros((128, 384), np.float32)
    cst[:, 0:128] = (s_idx[:, None] < s_idx[None, :])
    cst[:, 128:256] = -1.0 * (s_idx[:, None] >= s_idx[None, :])
    cst[:, 256:384] = np.eye(128)
    maps = []
    for c in range(ncores):
        cols = []
        for part in range(3):
            for hh in range(HPC):
                h = c * HPC + hh
                cols.append(w_in[:, part * SBW + h * 128: part * SBW + (h + 1) * 128])
        w = np.concatenate(cols, axis=1)
        w = np.ascontiguousarray(w.reshape(KC, 128, -1).transpose(1, 0, 2))
        maps.append({"xT": xT, "wqkv": w, "gmix": g, "cst": cst})
    return maps


def build_phase2(nc, k, TOK, D, T_in):
    KC = D // 128
    NP = TOK // 512
    PC = (D // 2) // 128
    CPG = PC // 4
    NE = 128
    T = 512
    (xin, xhalo, invc, attn_in, memT, gains, cst, keysT, w_pool, w_grp, w_gsb, w_gpool, w_bsb, w_bpool, w_out, w_xq, w_xkv,
     w_xo, w_pq, w_down, w_up, outT) = (T_in[n] for n in (
        "xin", "xhalo", "invc", "attn_scr", "memT", "gains", "cst", "keysT", "w_pool", "w_grp", "w_gsb", "w_gpool", "w_bsb",
        "w_bpool", "w_out", "w_xq", "w_xkv", "w_xo", "w_pq", "w_down", "w_up", "outT"))
    xr1 = nc.dram_tensor("xr1", [KC, 128, T], F32).ap()
    xr2 = nc.dram_tensor("xr2", [KC, 128, T], F32).ap()
    xr3 = nc.dram_tensor("xr3", [KC, 128, T], F32).ap()

    with contextlib.ExitStack() as st:
        k.stack = st
        pe, act, dve, pool, sp = k.pe, k.act, k.dve, k.pool, k.sp
        KW = max(KC, 32)
        hT = k.sbuf("hT", [128, KC, T], BF16)
        MT = k.sbuf("MT", [128, 32, T], BF16)
        aux = k.sbuf("aux", [128, 16, T], BF16)
        NWS = 6
        wsl = [k.sbuf("ws%d" % i, [128, KW, 128], BF16) for i in range(NWS)]
        xc = [k.sbuf("xc%d" % i, [128, T], F32) for i in range(3)]
        sqc = [k.sbuf("sqc%d" % i, [128, T], BF16) for i in range(2)]
        ft = [k.sbuf("ft%d" % i, [128, T], F32) for i in range(6)]
        bt = [k.sbuf("bt%d" % i, [128, T], BF16) for i in range(6)]
        rs = k.sbuf("rs", [128, T], F32)
        rsh = k.sbuf("rsh", [128, 16], F32)
        rsm = k.sbuf("rsm", [128, 260], F32)
        g_sb = k.sbuf("g", [128, 6, KC], F32)
        c_sb = k.sbuf("c", [128, 384], F32)
        ones_b = k.sbuf("ones", [128, 128], BF16)
        ident_b = k.sbuf("identb", [128, 128], BF16)
        keys_b = k.sbuf("keysb", [128, 16, 128], BF16)
        xh = k.sbuf("xh", [128, KC, 16], F32)
        sqh = k.sbuf("sqh", [128, KC, 16], BF16)
        hh = k.sbuf("hh", [128, KC, 16], BF16)
        ubuf = k.sbuf("ubuf", [128, 528], F32)
        ra = k.sbuf("ra", [128, 528], F32)
        rb = k.sbuf("rb", [128, 528], F32)
        icv = k.sbuf("icv", [128, 4, 16], F32)
        t16 = k.sbuf("t16", [128, 16], F32)
        kmT = k.sbuf("kmT", [128, 4, 256], BF16)
        vm = k.sbuf("vm", [128, 2, 512], BF16)
        qx = k.sbuf("qx", [128, 4, T], BF16)
        ox = k.sbuf("ox", [128, 4, T], BF16)
        pT = k.sbuf("pT", [128, 2, T], BF16)
        top = k.sbuf("top", [128, 16, 16], F32)
        best = k.sbuf("best", [128, 8, 16], F32)
        zz = k.sbuf("zz", [128, 8, 4], F32)
        tau = k.sbuf("tau", [128, 4, 8], F32)
        negb = k.sbuf("negb", [128, 4, 8], F32)
        ps = [k.psum("ps%d" % i, [128, 512]) for i in range(8)]
        A, B, SS, X = ps[0:2], ps[2:4], ps[4], ps[5:8]

        s_c = k.new_sem("ldc")
        s_w = [k.new_sem("ldw") for _ in range(6)]
        s_x = [k.new_sem("ldx") for _ in range(3)]
        s_m = k.new_sem("misc")

        k.dma(sp, g_sb[:], gains, s_c)
        t_kb = k.dma(pool, keys_b[:], keysT, s_w[0])
        t_c = k.dma(sp, c_sb[:], cst, s_c)
        dve.wait(t_c)
        nc.vector.memset(ones_b[:], 1.0)
        dve.wait(t_kb)
        t_cst = dve.mark(nc.vector.tensor_copy(out=ident_b[:], in_=c_sb[:, 256:384]))
        pe.wait(t_cst)
        act.wait(t_cst)

        wctr = [0]
        w_free = [None] * NWS
        free = {}

        def fr(name):
            return free.get(name)

        def load_w(tile_ap, KCw):
            s = wctr[0] % NWS
            wctr[0] += 1
            pool.wait(w_free[s])
            t = k.dma(pool, wsl[s][:, 0:KCw, :], tile_ap, s_w[s])
            return s, t

        def lin(tile_ap, KCw, outs, extra_wait=None):
            s, t = load_w(tile_ap, KCw)
            pe.wait(t, extra_wait)
            ins = None
            for (pap, rf) in outs:
                for kc in range(KCw):
                    ins = nc.tensor.matmul(pap, lhsT=wsl[s][:, kc, :], rhs=rf(kc), start=(kc == 0), stop=(kc == KCw - 1))
            tok = pe.mark(ins)
            w_free[s] = tok
            return tok

        xctr = [0]
        x_free = [None] * 3

        def load_x(src_ap, extra=None, n=T):
            s = xctr[0] % 3
            xctr[0] += 1
            sp.wait(x_free[s], extra)
            t = k.dma(sp, xc[s][:, 0:n], src_ap, s_x[s])
            return s, t

        sqctr = [0]
        sq_free = [None] * 2

        def stats_sq(src_ap, src_tok, n=T):
            s = sqctr[0] % 2
            sqctr[0] += 1
            act.wait(src_tok, sq_free[s])
            t = act.mark(nc.scalar.activation(out=sqc[s][:, 0:n], in_=src_ap, func=AF.Square))
            return (s, t, n)

        def stats_mm(pend, kc):
            s, t, n = pend
            pe.wait(t, fr("SS") if kc == 0 else None)
            tp = pe.mark(nc.tensor.matmul(SS[:, 0:n], lhsT=ones_b[:], rhs=sqc[s][:, 0:n], start=(kc == 0),
                                          stop=(kc == KC - 1)))
            sq_free[s] = tp
            return tp

        def stats_acc(src_ap, src_tok, kc, n=T):
            pend = stats_sq(src_ap, src_tok, n)
            return pend[1], stats_mm(pend, kc)

        def make_rstd(dst_ap, src_ps_ap, tok, guard=None):
            dve.wait(tok, guard)
            t0 = dve.mark(nc.vector.tensor_scalar(out=dst_ap, in0=src_ps_ap, scalar1=1.0 / D, scalar2=EPS, op0=ALU.mult,
                                                  op1=ALU.add))
            act.wait(t0)
            t1 = act.mark(nc.scalar.activation(out=dst_ap, in_=dst_ap, func=AF.Ln))
            act.wait(t1)
            t2 = act.mark(nc.scalar.activation(out=dst_ap, in_=dst_ap, func=AF.Exp, scale=-0.5))
            return t2, t0

        abctr = [0]

        def nextAB():
            i = abctr[0] % 2
            abctr[0] += 1
            return i

        ab_free = {("A", 0): None, ("A", 1): None, ("B", 0): None, ("B", 1): None}

        hm = MT
        t_hm = None
        for kc in range(KC):
            s, t = load_x(memT[kc], n=256)
            t_sq, t_pe = stats_acc(xc[s][:, 0:256], t, kc, n=256)
            sqs = sqc[(sqctr[0] - 1) % 2]
            for mc in range(2):
                t_pe = pe.mark(nc.tensor.matmul(X[mc][:, 0:1], lhsT=sqs[:, mc * 128:(mc + 1) * 128],
                                                rhs=ones_b[:, 0:1], start=(kc == 0), stop=(kc == KC - 1)))
            sq_free[(sqctr[0] - 1) % 2] = t_pe
            dve.wait(t)
            t_hm = dve.mark(nc.vector.tensor_scalar(out=hm[:, kc, 0:256], in0=xc[s][:, 0:256], scalar1=g_sb[:, 2, kc:kc + 1],
                                                    scalar2=None, op0=ALU.mult))
            x_free[s] = [t_sq, t_hm]
        t_rsm, t_rsm0 = make_rstd(rsm[:, 0:256], SS[:, 0:256], t_pe)
        free["SS"] = t_rsm0
        for mc in range(2):
            t_rsm, t_x0 = make_rstd(rsm[:, 256 + mc:257 + mc], X[mc][:, 0:1], t_pe)
            free["X%d" % mc] = t_x0
        pe.wait(t_hm)
        for hd in range(4):
            a = nextAB()
            t_mm = lin(w_xkv[hd], KC, [(A[a][:, 0:256], lambda kc: hm[:, kc, 0:256])], extra_wait=ab_free[("A", a)])
            dve.wait(t_mm, t_rsm)
            ab_free[("A", a)] = dve.mark(nc.vector.tensor_tensor(out=kmT[:, hd, :], in0=A[a][:, 0:256], in1=rsm[:, 0:256],
                                                                 op=ALU.mult))
        for blk in range(4):
            s, t = load_w(w_xkv[4 + blk], KC)
            a = nextAB()
            pe.wait(t, ab_free[("A", a)])
            for mc in range(2):
                for kc in range(KC):
                    ins = nc.tensor.matmul(A[a][:, mc * 128:(mc + 1) * 128], lhsT=hm[:, kc, mc * 128:(mc + 1) * 128],
                                           rhs=wsl[s][:, kc, :], start=(kc == 0), stop=(kc == KC - 1))
            t_mm = pe.mark(ins)
            w_free[s] = t_mm
            act.wait(t_mm, t_rsm)
            for mc in range(2):
                t_e = act.mark(nc.scalar.activation(out=vm[:, mc, blk * 128:(blk + 1) * 128],
                                                    in_=A[a][:, mc * 128:(mc + 1) * 128], func=AF.Copy,
                                                    scale=rsm[:, 256 + mc:257 + mc]))
            ab_free[("A", a)] = t_e
        t_mem_done = [ab_free[("A", 0)], ab_free[("A", 1)]]
        free["MT"] = t_mm

        xa_scale = 128.0 ** -0.5
        st_tok = {}

        def residual_stage(p, n_blocks, w_tiles, KCw, rhs_fn, src_fn, dst, gain_idx, pre_wait=None, final=False):
            t_pe_last = None
            t_h = None
            pend = None
            for nb in range(n_blocks):
                a = nextAB()
                t_mm = lin(w_tiles[nb], KCw, [(A[a][:, :], rhs_fn)], extra_wait=[ab_free[("A", a)], pre_wait])
                if pend is not None:
                    stats_mm(pend, nb - 1)
                s, t_x = load_x(src_fn(nb), extra=st_tok.get(("src", id(src_fn), nb)))
                dve.wait(t_mm, t_x)
                t_add = dve.mark(nc.vector.tensor_tensor(out=xc[s][:], in0=A[a][:, :], in1=xc[s][:], op=ALU.add))
                ab_free[("A", a)] = t_add
                sp.wait(t_add)
                t_st = k.dma(sp, dst[nb], xc[s][:], s_m)
                st_tok[(id(dst), nb)] = t_st
                pend = stats_sq(xc[s][:], t_add)
                t_sq = pend[1]
                if nb == n_blocks - 1:
                    t_pe_last = stats_mm(pend, nb)
                if gain_idx is not None:
                    dve.wait(fr("hT"))
                    t_h = dve.mark(nc.vector.tensor_scalar(out=hT[:, nb, :], in0=xc[s][:], scalar1=g_sb[:, gain_idx, nb:nb + 1],
                                                           scalar2=None, op0=ALU.mult))
                    x_free[s] = [t_st, t_sq, t_h]
                else:
                    x_free[s] = [t_st, t_sq]
            t_rs, t_rs0 = make_rstd(rs[:], SS[:, :], t_pe_last, guard=fr("rs"))
            free["SS"] = t_rs0
            return t_rs, t_h

        for p in range(NP):
            tc0 = p * T
            dve.wait(fr("hT"))
            for kc in range(KC):
                s, t = load_x(xin[p, kc])
                t_sq, t_pe = stats_acc(xc[s][:], t, kc)
                dve.wait(t)
                t_h = dve.mark(nc.vector.tensor_scalar(out=hT[:, kc, :], in0=xc[s][:], scalar1=g_sb[:, 0, kc:kc + 1],
                                                       scalar2=None, op0=ALU.mult))
                x_free[s] = [t_sq, t_h]
            t_rs, t_rs0 = make_rstd(rs[:], SS[:, :], t_pe, guard=fr("rs"))
            free["SS"] = t_rs0
            sp.wait(fr("xh"))
            k.dma(sp, icv[:], invc[p], s_m)
            t_xh = k.dma(sp, xh[:], xhalo[p], s_m)
            act.wait(t_xh, fr("sqh"))
            t_sqh = act.mark(nc.scalar.activation(out=sqh[:], in_=xh[:], func=AF.Square))
            dve.wait(t_xh, fr("hh"))
            t_hh = dve.mark(nc.vector.tensor_tensor(out=hh[:], in0=xh[:], in1=bc(g_sb[:, 0, :], 2, [128, KC, 16]), op=ALU.mult))
            free["xh"] = [t_sqh, t_hh]
            pe.wait(t_sqh, fr("SS"))
            for kc in range(KC):
                ins = nc.tensor.matmul(SS[:, 0:16], lhsT=ones_b[:], rhs=sqh[:, kc, :], start=(kc == 0), stop=(kc == KC - 1))
            t_ssh = pe.mark(ins)
            free["sqh"] = t_ssh
            t_rsh, t_rsh0 = make_rstd(rsh[:], SS[:, 0:16], t_ssh, guard=fr("rsh"))
            free["SS"] = t_rsh0
            pe.wait(t_h, t_hh)

            pooledT = MT
            dve.wait(fr("MT"))
            t_pool = None
            for nb in range(PC):
                g = nb // CPG
                a = nextAB()
                t_mm = lin(w_pool[nb], KC, [(A[a][:, :], lambda kc: hT[:, kc, :]), (B[a][:, 0:16], lambda kc: hh[:, kc, :])],
                           extra_wait=[ab_free[("A", a)], ab_free[("B", a)]])
                dve.wait(t_mm, t_rs, t_rsh, t_pool)
                nc.vector.tensor_tensor(out=ubuf[:, 16:528], in0=A[a][:, :], in1=rs[:], op=ALU.mult)
                t_u = dve.mark(nc.vector.tensor_tensor(out=ubuf[:, 0:16], in0=B[a][:, 0:16], in1=rsh[:], op=ALU.mult))
                ab_free[("A", a)] = t_u
                ab_free[("B", a)] = t_u
                dve.wait(t_u)
                t_r = dve.mark(nc.vector.tensor_tensor(out=ra[:, 1:528], in0=ubuf[:, 1:528], in1=ubuf[:, 0:527], op=ALU.add))
                r = ra
                if g >= 1:
                    dve.wait(t_r)
                    t_r = dve.mark(nc.vector.tensor_tensor(out=rb[:, 3:528], in0=ra[:, 3:528], in1=ra[:, 1:526], op=ALU.add))
                    r = rb
                if g >= 2:
                    dve.wait(t_r)
                    t_r = dve.mark(nc.vector.tensor_tensor(out=ra[:, 7:528], in0=rb[:, 7:528], in1=rb[:, 3:524], op=ALU.add))
                    r = ra
                if g >= 3:
                    dve.wait(t_r)
                    t_r = dve.mark(nc.vector.tensor_tensor(out=rb[:, 15:528], in0=ra[:, 15:528], in1=ra[:, 7:520], op=ALU.add))
                    r = rb
                w = (2, 4, 8, 16)[g]
                dve.wait(t_r)
                nc.vector.scalar_tensor_tensor(out=pooledT[:, nb, 16:512], in0=r[:, 32:528], scalar=1.0 / w,
                                               in1=ubuf[:, 32:528], op0=ALU.mult, op1=ALU.subtract)
                t_a = dve.mark(nc.vector.tensor_tensor(out=t16[:], in0=r[:, 16:32], in1=icv[:, g, :], op=ALU.mult))
                dve.wait(t_a)
                t_pool = dve.mark(nc.vector.tensor_tensor(out=pooledT[:, nb, 0:16], in0=t16[:], in1=ubuf[:, 16:32],
                                                          op=ALU.subtract))
            mixedT = aux
            pe.wait(t_pool)
            act.wait(fr("aux"))
            for nb in range(PC):
                g = nb // CPG
                a = nextAB()
                t_mm = lin(w_grp[nb], CPG, [(A[a][:, :], lambda kc, g=g: pooledT[:, g * CPG + kc, :])],
                           extra_wait=ab_free[("A", a)])
                act.wait(t_mm)
                t_mx = act.mark(nc.scalar.activation(out=mixedT[:, nb, :], in_=A[a][:, :], func=AF.Copy,
                                                     scale=g_sb[:, 5, nb:nb + 1]))
                ab_free[("A", a)] = t_mx
            pe.wait(t_mx)
            dve.wait(t_mm)
            for nb in range(KC):
                a = nextAB()
                t_a = lin(w_bpool[nb], PC, [(A[a][:, :], lambda kc: mixedT[:, kc, :])], extra_wait=ab_free[("A", a)])
                t_b = lin(w_gpool[nb], KC, [(B[a][:, :], lambda kc: hT[:, kc, :])], extra_wait=ab_free[("B", a)])
                f = ft[nb % 2]
                dve.wait(t_b, fr(("ft", nb % 2)))
                t_g = dve.mark(nc.vector.tensor_tensor(out=f[:], in0=B[a][:, :], in1=rs[:], op=ALU.mult))
                ab_free[("B", a)] = t_g
                act.wait(t_g)
                t_s = act.mark(nc.scalar.activation(out=f[:], in_=f[:], func=AF.Sigmoid))
                dve.wait(t_s, t_a)
                t_m = dve.mark(nc.vector.tensor_tensor(out=MT[:, nb, :], in0=f[:], in1=A[a][:, :], op=ALU.mult))
                ab_free[("A", a)] = t_m
                free[("ft", nb % 2)] = t_m
            sp.wait(t_a)
            t_at = k.dma(sp, aux[:], attn_in.rearrange("(h q) t -> q h t", q=128)[:, :, tc0:tc0 + T], s_m)
            pe.wait(t_at)
            for nb in range(KC):
                a = nextAB()
                t_a = lin(w_bsb[nb], 16, [(A[a][:, :], lambda kc: aux[:, kc, :])], extra_wait=ab_free[("A", a)])
                t_b = lin(w_gsb[nb], KC, [(B[a][:, :], lambda kc: hT[:, kc, :])], extra_wait=ab_free[("B", a)])
                f = ft[nb % 2]
                dve.wait(t_b, fr(("ft", nb % 2)))
                t_g = dve.mark(nc.vector.tensor_tensor(out=f[:], in0=B[a][:, :], in1=rs[:], op=ALU.mult))
                ab_free[("B", a)] = t_g
                act.wait(t_g)
                t_s = act.mark(nc.scalar.activation(out=f[:], in_=f[:], func=AF.Sigmoid))
                dve.wait(t_s, t_a)
                t_m0 = dve.mark(nc.vector.tensor_tensor(out=f[:], in0=f[:], in1=A[a][:, :], op=ALU.mult))
                ab_free[("A", a)] = t_m0
                dve.wait(t_m0)
                t_m = dve.mark(nc.vector.tensor_tensor(out=MT[:, nb, :], in0=MT[:, nb, :], in1=f[:], op=ALU.add))
                free[("ft", nb % 2)] = t_m
            free["aux"] = t_a
            free["hT"] = t_b
            free["rs"] = t_g
            pe.wait(t_m)
            t_rs, t_h = residual_stage(p, KC, w_out, KC, lambda kc: MT[:, kc, :], lambda nb: xin[p, nb], xr1, 1)
            pe.wait(t_h)
            for hd in range(4):
                a = nextAB()
                t_mm = lin(w_xq[hd], KC, [(A[a][:, :], lambda kc: hT[:, kc, :])], extra_wait=ab_free[("A", a)])
                dve.wait(t_mm, t_rs, fr("qx"))
                t_q = dve.mark(nc.vector.scalar_tensor_tensor(out=qx[:, hd, :], in0=A[a][:, :], scalar=xa_scale, in1=rs[:],
                                                              op0=ALU.mult, op1=ALU.mult))
                ab_free[("A", a)] = t_q
            free["hT"] = t_mm
            free["rs"] = t_q
            pe.wait(t_q, t_mem_done)
            for hd in range(4):
                pe.wait(fr("X0"), fr("X1"))
                for mc in range(2):
                    ins = nc.tensor.matmul(X[mc][:, :], lhsT=kmT[:, hd, mc * 128:(mc + 1) * 128], rhs=qx[:, hd, :], start=True,
                                           stop=True)
                t_sc = pe.mark(ins)
                act.wait(t_sc, fr("pT"))
                for mc in range(2):
                    t_p = act.mark(nc.scalar.activation(out=pT[:, mc, :], in_=X[mc][:, :], func=AF.Exp))
                free["X0"] = t_p
                free["X1"] = t_p
                a = nextAB()
                pe.wait(t_p, ab_free[("A", a)], ab_free[("B", a)])
                for mc in range(2):
                    nc.tensor.matmul(B[a][:, :], lhsT=ones_b[:], rhs=pT[:, mc, :], start=(mc == 0), stop=(mc == 1))
                for mc in range(2):
                    ins = nc.tensor.matmul(A[a][:, :], lhsT=vm[:, mc, hd * 128:(hd + 1) * 128], rhs=pT[:, mc, :],
                                           start=(mc == 0), stop=(mc == 1))
                t_o = pe.mark(ins)
                free["pT"] = t_o
                f = ft[hd % 2]
                dve.wait(t_o, fr(("ft", hd % 2)), fr("ox"))
                t_rd = dve.mark(nc.vector.reciprocal(out=f[:], in_=B[a][:, :]))
                dve.wait(t_rd)
                t_ox = dve.mark(nc.vector.tensor_tensor(out=ox[:, hd, :], in0=A[a][:, :], in1=f[:], op=ALU.mult))
                ab_free[("A", a)] = t_ox
                ab_free[("B", a)] = t_ox
                free[("ft", hd % 2)] = t_ox
            free["qx"] = t_sc
            pe.wait(t_ox)
            src1 = lambda nb: xr1[nb]
            for nb in range(KC):
                st_tok[("src", id(src1), nb)] = st_tok[(id(xr1), nb)]
            t_rs, t_h = residual_stage(p, KC, w_xo, 4, lambda kc: ox[:, kc, :], src1, xr2, 3)
            free["ox"] = w_free[(wctr[0] - 1) % NWS]
            qp = aux
            pe.wait(t_h)
            dve.wait(fr("aux"))
            for nb in range(16):
                a = nextAB()
                t_mm = lin(w_pq[nb], KC, [(A[a][:, :], lambda kc: hT[:, kc, :])], extra_wait=ab_free[("A", a)])
                dve.wait(t_mm, t_rs)
                t_q = dve.mark(nc.vector.tensor_tensor(out=qp[:, nb, :], in0=A[a][:, :], in1=rs[:], op=ALU.mult))
                ab_free[("A", a)] = t_q
            MTf = MT[:].rearrange("q a b -> q (a b)").bitcast(F32)
            sc = MTf[:, 0:2048]
            sc2 = MTf[:, 2048:4096]
            cand = MTf[:, 4096:6144]
            pe.wait(t_q)
            dve.wait(fr("MT"), t_m)
            act.wait(t_m)
            t_tk = None
            for tt in range(4):
                tsl = slice(tt * 128, (tt + 1) * 128)
                pe.wait(fr("X0"), fr("X1"), fr("X2"), fr("SS"))
                banks = [X[0], X[1], X[2], SS]
                for l in range(16):
                    ins = nc.tensor.matmul(banks[l // 4][:, (l % 4) * 128:(l % 4 + 1) * 128], lhsT=qp[:, l, tsl],
                                           rhs=keys_b[:, l, :], start=True, stop=True)
                t_s = pe.mark(ins)
                act.wait(t_s, t_tk)
                for bi in range(4):
                    t_cp = act.mark(nc.scalar.copy(out=sc[:, bi * 512:(bi + 1) * 512], in_=banks[bi][:, :]))
                for nm in ("X0", "X1", "X2", "SS"):
                    free[nm] = t_cp
                dve.wait(t_cp)
                for l in range(16):
                    row = sc[:, l * 128:(l + 1) * 128]
                    row2 = sc2[:, l * 128:(l + 1) * 128]
                    t1 = dve.mark(nc.vector.max(out=top[:, l, 0:8], in_=row))
                    dve.wait(t1)
                    t2 = dve.mark(nc.vector.match_replace(out=row2, in_to_replace=top[:, l, 0:8], in_values=row, imm_value=NEG))
                    dve.wait(t2)
                    t3 = dve.mark(nc.vector.max(out=top[:, l, 8:16], in_=row2))
                dve.wait(t3)
                top4 = top[:].rearrange("q (h two) a -> q h two a", two=2)
                cand4 = cand[:, 0:2048].rearrange("q (h a b) -> q h a b", h=8, a=16)
                t_cd = dve.mark(nc.vector.tensor_tensor(out=cand4, in0=bc(top4[:, :, 0, :], 3, [128, 8, 16, 16]),
                                                        in1=bc(top4[:, :, 1, :], 2, [128, 8, 16, 16]), op=ALU.add))
                dve.wait(t_cd)
                for h in range(8):
                    row = cand[:, h * 256:(h + 1) * 256]
                    row2 = sc2[:, h * 256:(h + 1) * 256]
                    t1 = dve.mark(nc.vector.max(out=best[:, h, 0:8], in_=row))
                    dve.wait(t1)
                    t2 = dve.mark(nc.vector.match_replace(out=row2, in_to_replace=best[:, h, 0:8], in_values=row, imm_value=NEG))
                    dve.wait(t2)
                    t3 = dve.mark(nc.vector.max(out=best[:, h, 8:16], in_=row2))
                dve.wait(t3)
                dd = sc2[:, 0:128].rearrange("q (h a) -> q h a", h=8)
                t_d = dve.mark(nc.vector.tensor_tensor(out=dd, in0=best[:], in1=bc(best[:, :, 0], 2, [128, 8, 16]),
                                                       op=ALU.subtract))
                act.wait(t_d)
                t_e = act.mark(nc.scalar.activation(out=dd, in_=dd, func=AF.Exp))
                dve.wait(t_e)
                t_z = dve.mark(nc.vector.reduce_sum(out=zz[:, :, 0], in_=dd, axis=AX.X))
                act.wait(t_z)
                t_lz = act.mark(nc.scalar.activation(out=zz[:, :, 1], in_=zz[:, :, 0], func=AF.Ln))
                dve.wait(t_lz)
                nc.vector.scalar_tensor_tensor(out=negb[:, tt, :], in0=best[:, :, 0], scalar=-1.0, in1=zz[:, :, 1],
                                               op0=ALU.mult, op1=ALU.subtract)
                t_tk = dve.mark(nc.vector.tensor_scalar(out=tau[:, tt, :], in0=best[:, :, 15], scalar1=-2e-5, scalar2=None,
                                                        op0=ALU.add))
            act.wait(t_tk)
            WT = MT
            gctr = 0
            free[("S", 0)] = fr("X0")
            free[("S", 1)] = fr("SS")
            free[("S", 2)] = ab_free[("B", 0)]
            free[("S", 3)] = ab_free[("B", 1)]
            for i_ in range(4):
                free[("et", i_)] = [fr(("ft", 0)), fr(("ft", 1))]
            free["Gs"] = [fr("X1"), fr("X2")]
            for qtr in range(4):
                for grp in range(16):
                    i0 = qtr * 32 + grp * 2
                    Gs = [X[1], X[2]]
                    gits = [(tt, h) for tt in range(4) for h in range(8)]
                    grec = [dict() for _ in gits]

                    def gA(ii):
                        tt, h = gits[ii]
                        tsl = slice(tt * 128, (tt + 1) * 128)
                        it = gbase + ii
                        sb_ = it % 4
                        sbank = [X[0], SS, B[0], B[1]][sb_]
                        pe.wait(fr(("S", sb_)))
                        nc.tensor.matmul(sbank[:, 0:256], lhsT=qp[:, 2 * h, tsl],
                                         rhs=bc(keys_b[:, 2 * h, i0:i0 + 2], 2, [128, 2, 128]), start=True, stop=False)
                        t_S = pe.mark(nc.tensor.matmul(sbank[:, 0:256], lhsT=qp[:, 2 * h + 1, tsl],
                                                       rhs=bc(keys_b[:, 2 * h + 1, :], 1, [128, 2, 128]), start=False, stop=True))
                        e_t = ft[2 + sb_]
                        act.wait(t_S, fr(("et", sb_)))
                        t_E = act.mark(nc.scalar.activation(out=e_t[:, 0:256], in_=sbank[:, 0:256], func=AF.Exp,
                                                            bias=negb[:, tt, h:h + 1]))
                        g_t = bt[it % 6]
                        dve.wait(t_E, fr(("gt", it % 6)))
                        t_G = dve.mark(nc.vector.scalar_tensor_tensor(out=g_t[:, 0:256], in0=sbank[:, 0:256],
                                                                      scalar=tau[:, tt, h:h + 1], in1=e_t[:, 0:256],
                                                                      op0=ALU.is_ge, op1=ALU.mult))
                        free[("S", sb_)] = t_G
                        free[("et", sb_)] = t_G
                        grec[ii]["G"] = t_G

                    def gB(ii):
                        tt, h = gits[ii]
                        tsl = slice(tt * 128, (tt + 1) * 128)
                        it = gbase + ii
                        g_t = bt[it % 6]
                        pe.wait(grec[ii]["G"], fr("Gs") if ii == 0 else None)
                        for ib in range(2):
                            ins = nc.tensor.matmul(Gs[ib][:, tsl], lhsT=g_t[:, ib * 128:(ib + 1) * 128], rhs=ident_b[:],
                                                   start=(h == 0), stop=(h == 7))
                        free[("gt", it % 6)] = pe.mark(ins)

                    gbase = gctr
                    SK = 3
                    for ii in range(len(gits) + SK):
                        if ii < len(gits):
                            gA(ii)
                        if ii >= SK:
                            gB(ii - SK)
                    gctr += len(gits)
                    t_Gs = free[("gt", (gctr - 1) % 6)]
                    for ib in range(2):
                        eb = i0 + ib
                        ebl = eb - qtr * 32
                        a = nextAB()
                        t_mm = lin(w_down[eb], KC, [(A[a][:, :], lambda kc: hT[:, kc, :])], extra_wait=ab_free[("A", a)])
                        f = ft[ib]
                        dve.wait(t_mm, t_rs, fr(("ft", ib)))
                        t_a = dve.mark(nc.vector.tensor_tensor(out=f[:], in0=A[a][:, :], in1=rs[:], op=ALU.mult))
                        ab_free[("A", a)] = t_a
                        act.wait(t_a)
                        t_ge = act.mark(nc.scalar.activation(out=f[:], in_=f[:], func=AF.Gelu))
                        dve.wait(t_ge, t_Gs, fr("WT"))
                        t_w = dve.mark(nc.vector.tensor_tensor(out=WT[:, ebl, :], in0=f[:], in1=Gs[ib][:, :], op=ALU.mult))
                        free[("ft", ib)] = t_w
                    free["Gs"] = t_w
                pe.wait(t_w)
                free["SS"] = free[("S", 1)]
                free["X0"] = free[("S", 0)]
                free["X1"] = t_w
                free["X2"] = t_w
                last = (qtr == 3)
                if qtr == 0:
                    srcq = lambda nb: xr2[nb]
                    for nb in range(KC):
                        st_tok[("src", id(srcq), nb)] = st_tok[(id(xr2), nb)]
                else:
                    srcq = lambda nb: xr3[nb]
                    for nb in range(KC):
                        st_tok[("src", id(srcq), nb)] = st_tok[(id(xr3), nb)]
                t_pe_last = None
                pend = None
                for nb in range(KC):
                    a = nextAB()
                    t_mm = lin(w_up[qtr, nb], 32, [(A[a][:, :], lambda kc: WT[:, kc, :])], extra_wait=ab_free[("A", a)])
                    if pend is not None:
                        stats_mm(pend, nb - 1)
                        pend = None
                    s, t_x = load_x(srcq(nb), extra=st_tok.get(("src", id(srcq), nb)))
                    dve.wait(t_mm, t_x)
                    t_add = dve.mark(nc.vector.tensor_tensor(out=xc[s][:], in0=A[a][:, :], in1=xc[s][:], op=ALU.add))
                    ab_free[("A", a)] = t_add
                    sp.wait(t_add)
                    t_st = k.dma(sp, xr3[nb], xc[s][:], s_m)
                    st_tok[(id(xr3), nb)] = t_st
                    if last:
                        pend = stats_sq(xc[s][:], t_add)
                        x_free[s] = [t_st, pend[1]]
                        if nb == KC - 1:
                            t_pe_last = stats_mm(pend, nb)
                    else:
                        x_free[s] = [t_st]
                free["WT"] = t_mm
            free["hT"] = t_mm
            free["MT"] = t_mm
            free["aux"] = t_Gs
            ab_free[("B", 0)] = [ab_free[("B", 0)], free[("S", 2)]]
            ab_free[("B", 1)] = [ab_free[("B", 1)], free[("S", 3)]]
            t_rs, t_rs0 = make_rstd(rs[:], SS[:, :], t_pe_last, guard=[fr("rs"), t_a])
            free["SS"] = t_rs0
            for nb in range(KC):
                s, t_x = load_x(xr3[nb], extra=st_tok[(id(xr3), nb)])
                dve.wait(t_x, t_rs)
                t_o = dve.mark(nc.vector.scalar_tensor_tensor(out=xc[s][:], in0=xc[s][:], scalar=g_sb[:, 4, nb:nb + 1], in1=rs[:],
                                                              op0=ALU.mult, op1=ALU.mult))
                sp.wait(t_o)
                t_st = k.dma(sp, outT[p, nb], xc[s][:], s_m)
                x_free[s] = [t_st]
            free["rs"] = t_o
        sp.wait((s_m, s_m.count))
    return nc


def tile_w(W):
    K_, N_ = W.shape
    return np.ascontiguousarray(W.reshape(K_ // 128, 128, N_ // 128, 128).transpose(2, 1, 0, 3))


def vec_pk(v, KC):
    o = np.zeros((128, KC), np.float32)
    n = v.shape[0] // 128
    o[:, :n] = v.reshape(n, 128).T
    return o


def make_cst():
    s_idx = np.arange(128)
    cst = np.zeros((128, 384), np.float32)
    cst[:, 0:128] = (s_idx[:, None] < s_idx[None, :])
    cst[:, 128:256] = -1.0 * (s_idx[:, None] >= s_idx[None, :])
    cst[:, 256:384] = np.eye(128)
    return cst


def host_phase2_shared(inp, D):
    KC = D // 128
    SBW = 2048
    PW = D // 2
    PC = PW // 128
    w_in = inp["w_in"][0]
    o = 3 * SBW
    sh = {}
    sh["w_pool"] = tile_w(w_in[:, o:o + PW])
    sh["w_gsb"] = tile_w(w_in[:, o + PW:o + PW + D])
    sh["w_gpool"] = tile_w(w_in[:, o + PW + D:o + PW + 2 * D])
    sh["w_grp"] = np.concatenate([tile_w(inp["pool_group_w"][0, g]) for g in range(4)], axis=0)
    sh["w_bsb"] = tile_w(inp["w_branch_sb"][0])
    sh["w_bpool"] = tile_w(inp["w_branch_pool"][0])
    sh["w_out"] = tile_w(inp["w_out"][0])
    sh["w_xq"] = tile_w(inp["xa_w_q"][0])
    sh["w_xkv"] = tile_w(inp["xa_w_kv"][0])
    sh["w_xo"] = tile_w(inp["xa_w_o"][0])
    sh["w_pq"] = tile_w(inp["peer_w_query"][0])
    sh["w_down"] = tile_w(inp["peer_down"][0].T)
    sh["w_up"] = np.stack([tile_w(inp["peer_up"][0][q * 4096:(q + 1) * 4096]) for q in range(4)], axis=0)
    sh["keysT"] = np.ascontiguousarray(inp["peer_sub_keys"][0].transpose(3, 0, 1, 2).reshape(128, 16, 128))
    g = np.zeros((128, 6, KC), np.float32)
    g[:, 0] = vec_pk(inp["norm_mix"][0], KC)
    g[:, 1] = vec_pk(inp["norm_mem_q"][0], KC)
    g[:, 2] = vec_pk(inp["norm_mem_kv"][0], KC)
    g[:, 3] = vec_pk(inp["norm_ffn"][0], KC)
    g[:, 4] = vec_pk(inp["norm_final"], KC)
    g[:, 5] = vec_pk(inp["pool_scale"][0], KC)
    sh["gains"] = g
    sh["cst"] = make_cst()
    mem = inp["mem"][0]
    sh["memT"] = np.ascontiguousarray(mem.reshape(256, KC, 128).transpose(1, 2, 0))
    return sh


def host_phase2_core(x, attn_full, tok0, TOK, D):
    KC = D // 128
    NP = TOK // 512
    xs = x[tok0:tok0 + TOK]
    m = {}
    m["xin"] = np.ascontiguousarray(xs.reshape(NP, 512, KC, 128).transpose(0, 2, 3, 1))
    xh = np.zeros((NP, 128, KC, 16), np.float32)
    ic = np.zeros((NP, 128, 4, 16), np.float32)
    for p in range(NP):
        st = tok0 + p * 512
        if st >= 16:
            xh[p] = x[st - 16:st].reshape(16, KC, 128).transpose(2, 1, 0)
        pos = st + np.arange(16)
        for gi, w in enumerate((2, 4, 8, 16)):
            ic[p, :, gi, :] = 1.0 / np.minimum(pos + 1, w)
    m["xhalo"] = xh
    m["invc"] = ic
    m["attn_in"] = np.ascontiguousarray(attn_full[:, tok0:tok0 + TOK])
    return m


def unpack_out(outT, TOK, D):
    KC = D // 128
    NP = TOK // 512
    return np.ascontiguousarray(outT.reshape(NP, KC, 128, 512).transpose(0, 3, 1, 2)).reshape(TOK, D)


def kernel(**inp):
    S, D = 8192, 4096
    inp = {k_: np.asarray(v) for k_, v in inp.items()}
    nc = bass.Bass("TRN2", target_bir_lowering=False)
    build_fused(nc, S, D)
    maps, owners = host_fused_inputs(inp, S, D)
    r = run_bass_kernel_spmd(nc, maps, core_ids=list(range(NCORES)))
    out = np.zeros((S, D), np.float32)
    for c in range(NCORES):
        o = unpack_out(r.results[c]["outT"], 1024, D)
        for p, g in enumerate(owners[c]):
            out[g * 512:(g + 1) * 512] = o[p * 512:(p + 1) * 512]
    return out.reshape(1, S, D)


def declare_tensors(nc, S, TOK, D):
    KC = D // 128
    NT = S // 128
    NP = TOK // 512
    PC = (D // 2) // 128
    CPG = PC // 4
    T = 512

    def din(name, shape, dt=F32):
        return nc.dram_tensor(name, shape, dt, kind="ExternalInput").ap()
    t = {}
    t["xT"] = din("xT", [NT, 128, KC, 128])
    t["wqkv"] = din("wqkv", [8, 128, KC, 768])
    t["bias_tab"] = din("bias_tab", [2, NT, 128, 512], BF16)
    t["xin"] = din("xin", [NP, KC, 128, T])
    t["xhalo"] = din("xhalo", [NP, 128, KC, 16])
    t["invc"] = din("invc", [NP, 128, 4, 16])
    t["memT"] = din("memT", [KC, 128, 256])
    t["gains"] = din("gains", [128, 6, KC])
    t["cst"] = din("cst", [128, 384])
    t["keysT"] = din("keysT", [128, 16, 128])
    t["w_pool"] = din("w_pool", [PC, 128, KC, 128])
    t["w_grp"] = din("w_grp", [PC, 128, CPG, 128])
    t["w_gsb"] = din("w_gsb", [KC, 128, KC, 128])
    t["w_gpool"] = din("w_gpool", [KC, 128, KC, 128])
    t["w_bsb"] = din("w_bsb", [KC, 128, 16, 128])
    t["w_bpool"] = din("w_bpool", [KC, 128, PC, 128])
    t["w_out"] = din("w_out", [KC, 128, KC, 128])
    t["w_xq"] = din("w_xq", [4, 128, KC, 128])
    t["w_xkv"] = din("w_xkv", [8, 128, KC, 128])
    t["w_xo"] = din("w_xo", [KC, 128, 4, 128])
    t["w_pq"] = din("w_pq", [16, 128, KC, 128])
    t["w_down"] = din("w_down", [128, 128, KC, 128])
    t["w_up"] = din("w_up", [4, KC, 128, 32, 128])
    t["outT"] = nc.dram_tensor("outT", [NP, KC, 128, T], F32, kind="ExternalOutput").ap()
    t["attn_scr"] = nc.dram_tensor("attn_scr", [16 * 128, TOK], BF16).ap()
    return t


def build_fused(nc, S, D):
    TOK = 1024
    t = declare_tensors(nc, S, TOK, D)
    with contextlib.ExitStack() as sem_stack:
        k = K(nc, None, sem_stack)
        build_phase1(nc, k, S, D, t)
        build_phase2(nc, k, TOK, D, t)
    return nc


def host_fused_inputs(inp, S, D):
    KC = D // 128
    NT = S // 128
    NG = S // 512
    x = inp["x"][0]
    sh = host_phase2_shared(inp, D)
    sh["xT"] = np.ascontiguousarray(x.reshape(NT, 128, KC, 128).transpose(0, 3, 2, 1))
    w_in = inp["w_in"][0]
    SBW = 2048
    wq = np.zeros((8, 128, KC, 768), np.float32)
    for hp in range(8):
        cols = []
        for part in range(3):
            for hh in range(2):
                h = hp * 2 + hh
                cols.append(w_in[:, part * SBW + h * 128: part * SBW + (h + 1) * 128])
        w = np.concatenate(cols, axis=1)
        wq[hp] = w.reshape(KC, 128, -1).transpose(1, 0, 2)
    sh["wqkv"] = wq
    maps = []
    owners = []
    s_idx = np.arange(128)
    t_idx = np.arange(512)
    for c in range(NCORES):
        groups = (c, NG - 1 - c)
        owners.append(groups)
        m = dict(sh)
        xs = np.concatenate([x[g * 512:(g + 1) * 512] for g in groups], axis=0)
        m["xin"] = np.ascontiguousarray(xs.reshape(2, 512, KC, 128).transpose(0, 2, 3, 1))
        xh = np.zeros((2, 128, KC, 16), np.float32)
        ic = np.zeros((2, 128, 4, 16), np.float32)
        bt = np.zeros((2, NT, 128, 512), np.float32)
        for p, g in enumerate(groups):
            st = g * 512
            if st >= 16:
                xh[p] = x[st - 16:st].reshape(16, KC, 128).transpose(2, 1, 0)
            pos = st + np.arange(16)
            for gi, w in enumerate((2, 4, 8, 16)):
                ic[p, :, gi, :] = 1.0 / np.minimum(pos + 1, w)
            qpos = st + t_idx
            for kb in range(NT):
                kpos = kb * 128 + s_idx
                bt[p, kb] = np.where(kpos[:, None] < qpos[None, :], 0.0, -30000.0)
        m["xhalo"] = xh
        m["invc"] = ic
        m["bias_tab"] = bt.astype(ml_dtypes.bfloat16)
        maps.append(m)
    return maps, owners
```

```python
import contextlib
import numpy as np
import ml_dtypes
import concourse.bass as bass
import concourse.mybir as mybir
from concourse.bass_utils import run_bass_kernel_spmd

F32, BF16 = mybir.dt.float32, mybir.dt.bfloat16
AF = mybir.ActivationFunctionType
ALU = mybir.AluOpType
AX = mybir.AxisListType

NCORES = 8
EPS = 1e-6
NEG = -1.0e30


class Sem:
    def __init__(self, handle):
        self.h = handle
        self.count = 0


class Eng:
    def __init__(self, k, eng, name):
        self.k, self.e, self.name = k, eng, name
        self.sem = k.new_sem(name)
        self.seen = {}

    def wait(self, *toks):
        for t in toks:
            if t is None:
                continue
            if isinstance(t, (list, tuple)) and len(t) and not isinstance(t[0], Sem):
                self.wait(*t)
                continue
            s, v = t
            if self.seen.get(id(s), 0) >= v:
                continue
            self.e.wait_ge(s.h, v)
            self.seen[id(s)] = v

    def mark(self, ins):
        ins.then_inc(self.sem.h, 1)
        self.sem.count += 1
        return (self.sem, self.sem.count)


class K:
    def __init__(self, nc, stack, sem_stack=None):
        self.nc, self.stack = nc, stack
        self.sem_stack = sem_stack if sem_stack is not None else stack
        self._sems = []
        self.pe = Eng(self, nc.tensor, "pe")
        self.act = Eng(self, nc.scalar, "act")
        self.dve = Eng(self, nc.vector, "dve")
        self.pool = Eng(self, nc.gpsimd, "pool")
        self.sp = Eng(self, nc.sync, "sp")

    def new_sem(self, name):
        s = Sem(self.sem_stack.enter_context(self.nc.semaphore(name + "_%d" % len(self._sems))))
        self._sems.append(s)
        return s

    def sbuf(self, name, shape, dt):
        return self.stack.enter_context(self.nc.sbuf_tensor(name, shape, dt))

    def psum(self, name, shape, dt=F32):
        return self.stack.enter_context(self.nc.psum_tensor(name, shape, dt))

    def dma(self, q, out, in_, sem):
        ins = q.e.dma_start(out=out, in_=in_)
        ins.then_inc(sem.h, 16)
        sem.count += 16
        return (sem, sem.count)


def bc(ap, axis, shape):
    return ap.unsqueeze(axis).broadcast_to(list(shape))


def build_phase1(nc, k, S, D, T_in):
    HPC = 2
    KC = D // 128
    NT = S // 128
    xT, xin, wqkv, gains, cst, bias_tab, attn_scr = (T_in[n] for n in
                                                     ("xT", "xin", "wqkv", "gains", "cst", "bias_tab", "attn_scr"))
    NKB = (NT // 2, NT)
    with contextlib.ExitStack() as st:
        k.stack = st
        nc_ = nc
        pe, act, dve, pool, sp = k.pe, k.act, k.dve, k.pool, k.sp
        W = k.sbuf("W", [128, KC, 3 * HPC * 128], BF16)
        KT = k.sbuf("KT", [128, HPC, S], BF16)
        V = k.sbuf("V", [128, NT, HPC * 128], BF16)
        QT = [k.sbuf("QT%d" % i, [128, HPC, 512], BF16) for i in range(2)]
        xs = [k.sbuf("xs%d" % i, [128, KC, 128], F32) for i in range(2)]
        sq = k.sbuf("sq", [128, KC, 128], BF16)
        hbl = [k.sbuf("hb%d" % i, [128, KC, 128], BF16) for i in range(2)]
        g_sb = k.sbuf("g1", [128, 6, KC], F32)
        c_sb = k.sbuf("c1", [128, 384], F32)
        ones_b = k.sbuf("ones1", [128, 128], BF16)
        nones_b = k.sbuf("nones", [128, 128], BF16)
        ntri_b = k.sbuf("ntri", [128, 128], BF16)
        ident_b = k.sbuf("ident1", [128, 128], BF16)
        rstdl = [k.sbuf("rstd%d" % i, [128, 132], F32) for i in range(2)]
        ebuf = [k.sbuf("eb%d" % i, [128, 512], F32) for i in range(3)]
        spb = [k.sbuf("sp%d" % i, [128, 512], BF16) for i in range(3)]
        atb = [k.sbuf("at%d" % i, [128, 512], BF16) for i in range(3)]
        bias = [k.sbuf("bias%d" % i, [128, 512], BF16) for i in range(3)]
        spacc = k.sbuf("spacc", [128, HPC, 512], BF16)
        ostage = [k.sbuf("os%d" % i, [128, 512], BF16) for i in range(2)]
        pz = [k.psum("pz%d" % i, [128, 512]) for i in range(3)]
        po = [k.psum("po%d" % i, [128, 512]) for i in range(HPC)]
        pqk = k.psum("pqk", [128, 512])
        pv = k.psum("pv", [128, 512])

        s_c = k.new_sem("ldc")
        s_w = k.new_sem("ldw")
        s_x = [k.new_sem("ldx") for _ in range(2)]
        s_o = [k.new_sem("sto") for _ in range(2)]
        s_b = [k.new_sem("ldb") for _ in range(3)]

        k.dma(sp, g_sb[:], gains, s_c)
        t_c = k.dma(sp, c_sb[:], cst, s_c)
        dve.wait(t_c)
        nc.vector.memset(ones_b[:], 1.0)
        nc.vector.memset(nones_b[:], -1.0)
        nc.vector.tensor_copy(out=ntri_b[:], in_=c_sb[:, 128:256])
        t_cst = dve.mark(nc.vector.tensor_copy(out=ident_b[:], in_=c_sb[:, 256:384]))
        pe.wait(t_cst)
        act.wait(t_cst)
        pool.wait(t_cst)

        scale = 128.0 ** -0.5
        st8 = {"x_free": [None, None], "sq_free": None, "hb_free": [None, None], "pqk_free": None, "pv_free": None,
               "rstd_free": [None, None], "xctr": 0, "scr_ready": None, "w_free": None, "kv_free": None, "last": None}
        qt_free = [None, None]

        hb_scr = nc.dram_tensor("hb_scr", [NT + 8, 128, KC, 128], BF16).ap()
        rs_scr = nc.dram_tensor("rs_scr", [NT + 8, 128, 132], F32).ap()
        s_hsl = [k.new_sem("sths") for _ in range(2)]
        s_r = [k.new_sem("ldr") for _ in range(2)]
        st8["rstd_free"] = [None, None]

        def project(src_ap, mode, idx, first, tt=None, slot=None, c0=None):
            d = st8
            sl = d["xctr"] % 2
            d["xctr"] += 1
            hb = hbl[sl]
            rstd = rstdl[sl]
            if first:
                sp.wait(d["x_free"][sl])
                t_x = k.dma(sp, xs[sl][:], src_ap, s_x[sl])
                act.wait(t_x, d["sq_free"])
                t_sq = act.mark(nc.scalar.activation(out=sq[:], in_=xs[sl][:], func=AF.Square))
                pool.wait(t_x, d["hb_free"][sl])
                t_hb = pool.mark(nc.gpsimd.tensor_tensor(out=hb[:], in0=xs[sl][:], in1=bc(g_sb[:, 0, :], 2, [128, KC, 128]),
                                                         op=ALU.mult))
                d["x_free"][sl] = [t_sq, t_hb]
                pool.wait(t_hb)
                t_hs = k.dma(pool, hb_scr[idx], hb[:], s_hsl[sl])
                pe.wait(t_sq, d["pv_free"])
                for kc in range(KC):
                    nc.tensor.matmul(pv[:, 256:384], lhsT=ones_b[:], rhs=sq[:, kc, :], start=(kc == 0), stop=(kc == KC - 1))
                for kc in range(KC):
                    ins = nc.tensor.matmul(pv[:, 384:385], lhsT=sq[:, kc, :], rhs=ones_b[:, 0:1], start=(kc == 0),
                                           stop=(kc == KC - 1))
                t_ss = pe.mark(ins)
                d["sq_free"] = t_ss
                dve.wait(t_ss, d["rstd_free"][sl])
                t_r00 = dve.mark(nc.vector.tensor_scalar(out=rstd[:, 0:129], in0=pv[:, 256:385], scalar1=1.0 / D, scalar2=EPS,
                                                         op0=ALU.mult, op1=ALU.add))
                act.wait(t_r00)
                t_r01 = act.mark(nc.scalar.activation(out=rstd[:, 0:129], in_=rstd[:, 0:129], func=AF.Ln))
                act.wait(t_r01)
                t_r0 = act.mark(nc.scalar.activation(out=rstd[:, 0:129], in_=rstd[:, 0:129], func=AF.Exp, scale=-0.5))
                act.wait(t_r0)
                t_rs = k.dma(act, rs_scr[idx][:, 0:129], rstd[:, 0:129], s_hsl[sl])
                pv_rel = [t_r00]
                extra_free = [t_hs, t_rs]
            else:
                sp.wait(d["hb_free"][sl], d["rstd_free"][sl], d["scr_ready"])
                t_hb = k.dma(sp, hb[:], hb_scr[idx], s_x[sl])
                t_r0 = k.dma(sp, rstd[:, 0:129], rs_scr[idx][:, 0:129], s_r[sl])
                pv_rel = []
                extra_free = []
            pe.wait(t_hb, d["pqk_free"], d["w_ready"])
            blks = range(HPC, 2 * HPC) if mode == "kv" else range(0, HPC)
            for blk in blks:
                for kc in range(KC):
                    ins = nc.tensor.matmul(pqk[:, blk * 128:(blk + 1) * 128], lhsT=W[:, kc, blk * 128:(blk + 1) * 128],
                                           rhs=hb[:, kc, :], start=(kc == 0), stop=(kc == KC - 1))
            t_qk = pe.mark(ins)
            t_v = None
            if mode == "kv":
                pe.wait(d["pv_free"])
                for kc in range(KC):
                    ins = nc.tensor.matmul(pv[:, 0:HPC * 128], lhsT=hb[:, kc, :], rhs=W[:, kc, 2 * HPC * 128:3 * HPC * 128],
                                           start=(kc == 0), stop=(kc == KC - 1))
                t_v = pe.mark(ins)
            d["hb_free"][sl] = [t_v if t_v is not None else t_qk] + extra_free
            d["w_last"] = t_v if t_v is not None else t_qk
            if mode == "kv":
                dve.wait(t_qk, t_r0, d["kv_free"])
                for hh in range(HPC):
                    ins = nc.vector.tensor_tensor(out=KT[:, hh, tt * 128:(tt + 1) * 128],
                                                  in0=pqk[:, (HPC + hh) * 128:(HPC + hh + 1) * 128], in1=rstd[:, 0:128],
                                                  op=ALU.mult)
                t_e1 = dve.mark(ins)
                act.wait(t_v, t_r0, d["kv_free"])
                t_e2 = act.mark(nc.scalar.activation(out=V[:, tt, :], in_=pv[:, 0:HPC * 128], func=AF.Copy,
                                                     scale=rstd[:, 128:129]))
                d["pv_free"] = [t_e2] + pv_rel
                d["rstd_free"][sl] = [t_e1, t_e2] + extra_free
                d["last"] = [t_e1, t_e2]
                d["last_kv"] = [t_e1, t_e2]
            else:
                dve.wait(t_qk, t_r0, qt_free[slot])
                for hh in range(HPC):
                    ins = nc.vector.scalar_tensor_tensor(out=QT[slot][:, hh, c0:c0 + 128], in0=pqk[:, hh * 128:(hh + 1) * 128],
                                                         scalar=scale, in1=rstd[:, 0:128], op0=ALU.mult, op1=ALU.mult)
                t_e1 = dve.mark(ins)
                if pv_rel:
                    d["pv_free"] = pv_rel
                d["rstd_free"][sl] = [t_e1] + extra_free
                d["last"] = [t_e1]
            d["pqk_free"] = t_e1

        it_ctr = [0]
        pz_free = [None] * 3
        sp_free = [None] * 3
        at_free = [None] * 3
        b_free = [None] * 3
        b_ctr = [0]
        po_free = [None] * HPC
        spacc_tok = [None] * HPC
        os_free = [None, None]
        o_ctr = [0]
        out_toks = []

        def attention(hp, slot):
            nkb = NKB[slot]
            qt = QT[slot]
            its = [(kb, hh) for kb in reversed(range(nkb)) for hh in range(HPC)]
            n = len(its)
            stt = [dict() for _ in range(n)]
            pe.wait(st8["last"], st8["last_kv"])
            pool.wait(st8["last"], st8["last_kv"])
            for hh in range(HPC):
                pool.wait(spacc_tok[hh])
            t_z = pool.mark(nc.gpsimd.memset(spacc[:], 0.0))
            for hh in range(HPC):
                spacc_tok[hh] = t_z
            last_av = [None] * HPC
            btile = {}

            def st0(i):
                kb, hh = its[i]
                b = (it_ctr[0] + i) % 3
                d = stt[i]
                d["b"] = b
                if hh == 0:
                    bs = b_ctr[0] % 3
                    b_ctr[0] += 1
                    sp.wait(b_free[bs])
                    btile[kb] = (bs, k.dma(sp, bias[bs][:], bias_tab[slot, kb], s_b[bs]))
                bs, t_b = btile[kb]
                pe.wait(pz_free[b], t_b)
                nc.tensor.matmul(pz[b][:, :], lhsT=KT[:, hh, kb * 128:(kb + 1) * 128], rhs=qt[:, hh, :], start=True, stop=False)
                d["z"] = pe.mark(nc.tensor.matmul(pz[b][:, :], lhsT=ident_b[:], rhs=bias[bs][:], start=False, stop=False))
                if hh == HPC - 1:
                    b_free[bs] = d["z"]
                act.wait(d["z"], sp_free[b])
                t_e = act.mark(nc.scalar.activation(out=ebuf[b][:], in_=pz[b][:, :], func=AF.Exp))
                act.wait(t_e)
                d["ln"] = act.mark(nc.scalar.activation(out=spb[b][:], in_=ebuf[b][:], func=AF.Ln, bias=1.0))

            def st1(i):
                kb, hh = its[i]
                d = stt[i]
                b = d["b"]
                first = (kb == nkb - 1)
                pe.wait(d["ln"], spacc_tok[hh])
                ins = nc.tensor.matmul(pz[b][:, :], lhsT=ntri_b[:], rhs=spb[b][:], start=False, stop=first)
                if not first:
                    ins = nc.tensor.matmul(pz[b][:, :], lhsT=nones_b[:], rhs=spacc[:, hh, :], start=False, stop=True)
                d["cs"] = pe.mark(ins)
                act.wait(d["cs"], at_free[b])
                d["ea"] = act.mark(nc.scalar.activation(out=atb[b][:], in_=pz[b][:, :], func=AF.Exp))
                pz_free[b] = d["ea"]
                pool.wait(d["cs"], d["ln"], spacc_tok[hh])
                spacc_tok[hh] = pool.mark(nc.gpsimd.tensor_tensor(out=spacc[:, hh, :], in0=spacc[:, hh, :], in1=spb[b][:],
                                                                  op=ALU.add))
                sp_free[b] = spacc_tok[hh]

            def st2(i):
                kb, hh = its[i]
                d = stt[i]
                b = d["b"]
                pe.wait(d["ea"], po_free[hh] if kb == nkb - 1 else None)
                d["av"] = pe.mark(nc.tensor.matmul(po[hh][:, :], lhsT=V[:, kb, hh * 128:(hh + 1) * 128], rhs=atb[b][:],
                                                   start=(kb == nkb - 1), stop=(kb == 0)))
                at_free[b] = d["av"]
                last_av[hh] = d["av"]

            for step in range(n + 2):
                if step < n:
                    st0(step)
                if 0 <= step - 1 < n:
                    st1(step - 1)
                if 0 <= step - 2 < n:
                    st2(step - 2)
            it_ctr[0] += n
            qt_free[slot] = [last_av[hh] for hh in range(HPC)]
            st8["kv_free"] = [last_av[hh] for hh in range(HPC)]
            for hh in range(HPC):
                o = o_ctr[0] % 2
                o_ctr[0] += 1
                act.wait(last_av[hh], os_free[o])
                t_cp = act.mark(nc.scalar.copy(out=ostage[o][:], in_=po[hh][:, :]))
                po_free[hh] = t_cp
                sp.wait(t_cp)
                h = hp * HPC + hh
                os_free[o] = k.dma(sp, attn_scr[h * 128:(h + 1) * 128, slot * 512:(slot + 1) * 512], ostage[o][:], s_o[o])
                out_toks.append(os_free[o])

        st8["w_ready"] = None
        st8["w_last"] = None
        for hp in range(8):
            pool.wait(st8["w_last"], st8["kv_free"])
            for i in range(3 * HPC):
                t_w = k.dma(pool, W[:, :, i * 128:(i + 1) * 128], wqkv[hp][:, :, i * 128:(i + 1) * 128], s_w)
            st8["w_ready"] = t_w
            for tt in range(NT):
                project(xT[tt], "kv", tt, hp == 0, tt=tt)
            for j in range(8):
                slot, c0 = j // 4, (j % 4) * 128
                project(xin[slot][:, :, c0:c0 + 128].rearrange("kc q t -> q kc t"), "q", NT + j, hp == 0, slot=slot, c0=c0)
            if hp == 0:
                st8["scr_ready"] = [(s_, s_.count) for s_ in s_hsl]
            for slot in range(2):
                attention(hp, slot)
        fin = [(e.sem, e.sem.count) for e in (pe, act, dve, pool)] + [(s_, s_.count) for s_ in s_o]
        for e in (pe, act, dve, pool, sp):
            e.wait(fin)
    return nc


def host_phase1_inputs(x, w_in, norm_mix, S, D, HPC, ncores):
    KC = D // 128
    NT = S // 128
    SBW = ncores * HPC * 128
    xT = np.ascontiguousarray(x.reshape(NT, 128, KC, 128).transpose(0, 3, 2, 1))
    g = np.ascontiguousarray(norm_mix.reshape(KC, 128).T)
    s_idx = np.arange(128)
    cst = np.zeros((128, 384), np.float32)
    cst[:, 0:128] = (s_idx[:, None] < s_idx[None, :])
    cst[:, 128:256] = -1.0 * (s_idx[:, None] >= s_idx[None, :])
    cst[:, 256:384] = np.eye(128)
    maps = []
    for c in range(ncores):
        cols = []
        for part in range(3):
            for hh in range(HPC):
                h = c * HPC + hh
                cols.append(w_in[:, part * SBW + h * 128: part * SBW + (h + 1) * 128])
        w = np.concatenate(cols, axis=1)
        w = np.ascontiguousarray(w.reshape(KC, 128, -1).transpose(1, 0, 2))
        maps.append({"xT": xT, "wqkv": w, "gmix": g, "cst": cst})
    return maps


def build_phase2(nc, k, TOK, D, T_in):
    KC = D // 128
    NP = TOK // 512
    PC = (D // 2) // 128
    CPG = PC // 4
    NE = 128
    T = 512
    (xin, xhalo, invc, attn_in, memT, gains, cst, keysT, w_pool, w_grp, w_gsb, w_gpool, w_bsb, w_bpool, w_out, w_xq, w_xkv,
     w_xo, w_pq, w_down, w_up, outT) = (T_in[n] for n in (
        "xin", "xhalo", "invc", "attn_scr", "memT", "gains", "cst", "keysT", "w_pool", "w_grp", "w_gsb", "w_gpool", "w_bsb",
        "w_bpool", "w_out", "w_xq", "w_xkv", "w_xo", "w_pq", "w_down", "w_up", "outT"))
    xr1 = nc.dram_tensor("xr1", [KC, 128, T], F32).ap()
    xr2 = nc.dram_tensor("xr2", [KC, 128, T], F32).ap()
    xr3 = nc.dram_tensor("xr3", [KC, 128, T], F32).ap()

    with contextlib.ExitStack() as st:
        k.stack = st
        pe, act, dve, pool, sp = k.pe, k.act, k.dve, k.pool, k.sp
        KW = max(KC, 32)
        hT = k.sbuf("hT", [128, KC, T], BF16)
        MT = k.sbuf("MT", [128, 32, T], BF16)
        aux = k.sbuf("aux", [128, 16, T], BF16)
        NWS = 6
        wsl = [k.sbuf("ws%d" % i, [128, KW, 128], BF16) for i in range(NWS)]
        xc = [k.sbuf("xc%d" % i, [128, T], F32) for i in range(3)]
        sqc = [k.sbuf("sqc%d" % i, [128, T], BF16) for i in range(2)]
        ft = [k.sbuf("ft%d" % i, [128, T], F32) for i in range(6)]
        bt = [k.sbuf("bt%d" % i, [128, T], BF16) for i in range(6)]
        rs = k.sbuf("rs", [128, T], F32)
        rsh = k.sbuf("rsh", [128, 16], F32)
        rsm = k.sbuf("rsm", [128, 260], F32)
        g_sb = k.sbuf("g", [128, 6, KC], F32)
        c_sb = k.sbuf("c", [128, 384], F32)
        ones_b = k.sbuf("ones", [128, 128], BF16)
        ident_b = k.sbuf("identb", [128, 128], BF16)
        keys_b = k.sbuf("keysb", [128, 16, 128], BF16)
        xh = k.sbuf("xh", [128, KC, 16], F32)
        sqh = k.sbuf("sqh", [128, KC, 16], BF16)
        hh = k.sbuf("hh", [128, KC, 16], BF16)
        ubuf = k.sbuf("ubuf", [128, 528], F32)
        ra = k.sbuf("ra", [128, 528], F32)
        rb = k.sbuf("rb", [128, 528], F32)
        icv = k.sbuf("icv", [128, 4, 16], F32)
        t16 = k.sbuf("t16", [128, 16], F32)
        kmT = k.sbuf("kmT", [128, 4, 256], BF16)
        vm = k.sbuf("vm", [128, 2, 512], BF16)
        qx = k.sbuf("qx", [128, 4, T], BF16)
        ox = k.sbuf("ox", [128, 4, T], BF16)
        pT = k.sbuf("pT", [128, 2, T], BF16)
        top = k.sbuf("top", [128, 16, 16], F32)
        best = k.sbuf("best", [128, 8, 16], F32)
        zz = k.sbuf("zz", [128, 8, 4], F32)
        tau = k.sbuf("tau", [128, 4, 8], F32)
        negb = k.sbuf("negb", [128, 4, 8], F32)
        ps = [k.psum("ps%d" % i, [128, 512]) for i in range(8)]
        A, B, SS, X = ps[0:2], ps[2:4], ps[4], ps[5:8]

        s_c = k.new_sem("ldc")
        s_w = [k.new_sem("ldw") for _ in range(6)]
        s_x = [k.new_sem("ldx") for _ in range(3)]
        s_m = k.new_sem("misc")

        k.dma(sp, g_sb[:], gains, s_c)
        t_kb = k.dma(pool, keys_b[:], keysT, s_w[0])
        t_c = k.dma(sp, c_sb[:], cst, s_c)
        dve.wait(t_c)
        nc.vector.memset(ones_b[:], 1.0)
        dve.wait(t_kb)
        t_cst = dve.mark(nc.vector.tensor_copy(out=ident_b[:], in_=c_sb[:, 256:384]))
        pe.wait(t_cst)
        act.wait(t_cst)

        wctr = [0]
        w_free = [None] * NWS
        free = {}

        def fr(name):
            return free.get(name)

        def load_w(tile_ap, KCw):
            s = wctr[0] % NWS
            wctr[0] += 1
            pool.wait(w_free[s])
            t = k.dma(pool, wsl[s][:, 0:KCw, :], tile_ap, s_w[s])
            return s, t

        def lin(tile_ap, KCw, outs, extra_wait=None):
            s, t = load_w(tile_ap, KCw)
            pe.wait(t, extra_wait)
            ins = None
            for (pap, rf) in outs:
                for kc in range(KCw):
                    ins = nc.tensor.matmul(pap, lhsT=wsl[s][:, kc, :], rhs=rf(kc), start=(kc == 0), stop=(kc == KCw - 1))
            tok = pe.mark(ins)
            w_free[s] = tok
            return tok

        xctr = [0]
        x_free = [None] * 3

        def load_x(src_ap, extra=None, n=T):
            s = xctr[0] % 3
            xctr[0] += 1
            sp.wait(x_free[s], extra)
            t = k.dma(sp, xc[s][:, 0:n], src_ap, s_x[s])
            return s, t

        sqctr = [0]
        sq_free = [None] * 2

        def stats_sq(src_ap, src_tok, n=T):
            s = sqctr[0] % 2
            sqctr[0] += 1
            act.wait(src_tok, sq_free[s])
            t = act.mark(nc.scalar.activation(out=sqc[s][:, 0:n], in_=src_ap, func=AF.Square))
            return (s, t, n)

        def stats_mm(pend, kc):
            s, t, n = pend
            pe.wait(t, fr("SS") if kc == 0 else None)
            tp = pe.mark(nc.tensor.matmul(SS[:, 0:n], lhsT=ones_b[:], rhs=sqc[s][:, 0:n], start=(kc == 0),
                                          stop=(kc == KC - 1)))
            sq_free[s] = tp
            return tp

        def stats_acc(src_ap, src_tok, kc, n=T):
            pend = stats_sq(src_ap, src_tok, n)
            return pend[1], stats_mm(pend, kc)

        def make_rstd(dst_ap, src_ps_ap, tok, guard=None):
            dve.wait(tok, guard)
            t0 = dve.mark(nc.vector.tensor_scalar(out=dst_ap, in0=src_ps_ap, scalar1=1.0 / D, scalar2=EPS, op0=ALU.mult,
                                                  op1=ALU.add))
            act.wait(t0)
            t1 = act.mark(nc.scalar.activation(out=dst_ap, in_=dst_ap, func=AF.Ln))
            act.wait(t1)
            t2 = act.mark(nc.scalar.activation(out=dst_ap, in_=dst_ap, func=AF.Exp, scale=-0.5))
            return t2, t0

        abctr = [0]

        def nextAB():
            i = abctr[0] % 2
            abctr[0] += 1
            return i

        ab_free = {("A", 0): None, ("A", 1): None, ("B", 0): None, ("B", 1): None}

        hm = MT
        t_hm = None
        for kc in range(KC):
            s, t = load_x(memT[kc], n=256)
            t_sq, t_pe = stats_acc(xc[s][:, 0:256], t, kc, n=256)
            sqs = sqc[(sqctr[0] - 1) % 2]
            for mc in range(2):
                t_pe = pe.mark(nc.tensor.matmul(X[mc][:, 0:1], lhsT=sqs[:, mc * 128:(mc + 1) * 128],
                                                rhs=ones_b[:, 0:1], start=(kc == 0), stop=(kc == KC - 1)))
            sq_free[(sqctr[0] - 1) % 2] = t_pe
            dve.wait(t)
            t_hm = dve.mark(nc.vector.tensor_scalar(out=hm[:, kc, 0:256], in0=xc[s][:, 0:256], scalar1=g_sb[:, 2, kc:kc + 1],
                                                    scalar2=None, op0=ALU.mult))
            x_free[s] = [t_sq, t_hm]
        t_rsm, t_rsm0 = make_rstd(rsm[:, 0:256], SS[:, 0:256], t_pe)
        free["SS"] = t_rsm0
        for mc in range(2):
            t_rsm, t_x0 = make_rstd(rsm[:, 256 + mc:257 + mc], X[mc][:, 0:1], t_pe)
            free["X%d" % mc] = t_x0
        pe.wait(t_hm)
        for hd in range(4):
            a = nextAB()
            t_mm = lin(w_xkv[hd], KC, [(A[a][:, 0:256], lambda kc: hm[:, kc, 0:256])], extra_wait=ab_free[("A", a)])
            dve.wait(t_mm, t_rsm)
            ab_free[("A", a)] = dve.mark(nc.vector.tensor_tensor(out=kmT[:, hd, :], in0=A[a][:, 0:256], in1=rsm[:, 0:256],
                                                                 op=ALU.mult))
        for blk in range(4):
            s, t = load_w(w_xkv[4 + blk], KC)
            a = nextAB()
            pe.wait(t, ab_free[("A", a)])
            for mc in range(2):
                for kc in range(KC):
                    ins = nc.tensor.matmul(A[a][:, mc * 128:(mc + 1) * 128], lhsT=hm[:, kc, mc * 128:(mc + 1) * 128],
                                           rhs=wsl[s][:, kc, :], start=(kc == 0), stop=(kc == KC - 1))
            t_mm = pe.mark(ins)
            w_free[s] = t_mm
            act.wait(t_mm, t_rsm)
            for mc in range(2):
                t_e = act.mark(nc.scalar.activation(out=vm[:, mc, blk * 128:(blk + 1) * 128],
                                                    in_=A[a][:, mc * 128:(mc + 1) * 128], func=AF.Copy,
                                                    scale=rsm[:, 256 + mc:257 + mc]))
            ab_free[("A", a)] = t_e
        t_mem_done = [ab_free[("A", 0)], ab_free[("A", 1)]]
        free["MT"] = t_mm

        xa_scale = 128.0 ** -0.5
        st_tok = {}

        def residual_stage(p, n_blocks, w_tiles, KCw, rhs_fn, src_fn, dst, gain_idx, pre_wait=None, final=False):
            t_pe_last = None
            t_h = None
            pend = None
            for nb in range(n_blocks):
                a = nextAB()
                t_mm = lin(w_tiles[nb], KCw, [(A[a][:, :], rhs_fn)], extra_wait=[ab_free[("A", a)], pre_wait])
                if pend is not None:
                    stats_mm(pend, nb - 1)
                s, t_x = load_x(src_fn(nb), extra=st_tok.get(("src", id(src_fn), nb)))
                dve.wait(t_mm, t_x)
                t_add = dve.mark(nc.vector.tensor_tensor(out=xc[s][:], in0=A[a][:, :], in1=xc[s][:], op=ALU.add))
                ab_free[("A", a)] = t_add
                sp.wait(t_add)
                t_st = k.dma(sp, dst[nb], xc[s][:], s_m)
                st_tok[(id(dst), nb)] = t_st
                pend = stats_sq(xc[s][:], t_add)
                t_sq = pend[1]
                if nb == n_blocks - 1:
                    t_pe_last = stats_mm(pend, nb)
                if gain_idx is not None:
                    dve.wait(fr("hT"))
                    t_h = dve.mark(nc.vector.tensor_scalar(out=hT[:, nb, :], in0=xc[s][:], scalar1=g_sb[:, gain_idx, nb:nb + 1],
                                                           scalar2=None, op0=ALU.mult))
                    x_free[s] = [t_st, t_sq, t_h]
                else:
                    x_free[s] = [t_st, t_sq]
            t_rs, t_rs0 = make_rstd(rs[:], SS[:, :], t_pe_last, guard=fr("rs"))
            free["SS"] = t_rs0
            return t_rs, t_h

        for p in range(NP):
            tc0 = p * T
            dve.wait(fr("hT"))
            for kc in range(KC):
                s, t = load_x(xin[p, kc])
                t_sq, t_pe = stats_acc(xc[s][:], t, kc)
                dve.wait(t)
                t_h = dve.mark(nc.vector.tensor_scalar(out=hT[:, kc, :], in0=xc[s][:], scalar1=g_sb[:, 0, kc:kc + 1],
                                                       scalar2=None, op0=ALU.mult))
                x_free[s] = [t_sq, t_h]
            t_rs, t_rs0 = make_rstd(rs[:], SS[:, :], t_pe, guard=fr("rs"))
            free["SS"] = t_rs0
            sp.wait(fr("xh"))
            k.dma(sp, icv[:], invc[p], s_m)
            t_xh = k.dma(sp, xh[:], xhalo[p], s_m)
            act.wait(t_xh, fr("sqh"))
            t_sqh = act.mark(nc.scalar.activation(out=sqh[:], in_=xh[:], func=AF.Square))
            dve.wait(t_xh, fr("hh"))
            t_hh = dve.mark(nc.vector.tensor_tensor(out=hh[:], in0=xh[:], in1=bc(g_sb[:, 0, :], 2, [128, KC, 16]), op=ALU.mult))
            free["xh"] = [t_sqh, t_hh]
            pe.wait(t_sqh, fr("SS"))
            for kc in range(KC):
                ins = nc.tensor.matmul(SS[:, 0:16], lhsT=ones_b[:], rhs=sqh[:, kc, :], start=(kc == 0), stop=(kc == KC - 1))
            t_ssh = pe.mark(ins)
            free["sqh"] = t_ssh
            t_rsh, t_rsh0 = make_rstd(rsh[:], SS[:, 0:16], t_ssh, guard=fr("rsh"))
            free["SS"] = t_rsh0
            pe.wait(t_h, t_hh)

            pooledT = MT
            dve.wait(fr("MT"))
            t_pool = None
            for nb in range(PC):
                g = nb // CPG
                a = nextAB()
                t_mm = lin(w_pool[nb], KC, [(A[a][:, :], lambda kc: hT[:, kc, :]), (B[a][:, 0:16], lambda kc: hh[:, kc, :])],
                           extra_wait=[ab_free[("A", a)], ab_free[("B", a)]])
                dve.wait(t_mm, t_rs, t_rsh, t_pool)
                nc.vector.tensor_tensor(out=ubuf[:, 16:528], in0=A[a][:, :], in1=rs[:], op=ALU.mult)
                t_u = dve.mark(nc.vector.tensor_tensor(out=ubuf[:, 0:16], in0=B[a][:, 0:16], in1=rsh[:], op=ALU.mult))
                ab_free[("A", a)] = t_u
                ab_free[("B", a)] = t_u
                dve.wait(t_u)
                t_r = dve.mark(nc.vector.tensor_tensor(out=ra[:, 1:528], in0=ubuf[:, 1:528], in1=ubuf[:, 0:527], op=ALU.add))
                r = ra
                if g >= 1:
                    dve.wait(t_r)
                    t_r = dve.mark(nc.vector.tensor_tensor(out=rb[:, 3:528], in0=ra[:, 3:528], in1=ra[:, 1:526], op=ALU.add))
                    r = rb
                if g >= 2:
                    dve.wait(t_r)
                    t_r = dve.mark(nc.vector.tensor_tensor(out=ra[:, 7:528], in0=rb[:, 7:528], in1=rb[:, 3:524], op=ALU.add))
                    r = ra
                if g >= 3:
                    dve.wait(t_r)
                    t_r = dve.mark(nc.vector.tensor_tensor(out=rb[:, 15:528], in0=ra[:, 15:528], in1=ra[:, 7:520], op=ALU.add))
                    r = rb
                w = (2, 4, 8, 16)[g]
                dve.wait(t_r)
                nc.vector.scalar_tensor_tensor(out=pooledT[:, nb, 16:512], in0=r[:, 32:528], scalar=1.0 / w,
                                               in1=ubuf[:, 32:528], op0=ALU.mult, op1=ALU.subtract)
                t_a = dve.mark(nc.vector.tensor_tensor(out=t16[:], in0=r[:, 16:32], in1=icv[:, g, :], op=ALU.mult))
                dve.wait(t_a)
                t_pool = dve.mark(nc.vector.tensor_tensor(out=pooledT[:, nb, 0:16], in0=t16[:], in1=ubuf[:, 16:32],
                                                          op=ALU.subtract))
            mixedT = aux
            pe.wait(t_pool)
            act.wait(fr("aux"))
            for nb in range(PC):
                g = nb // CPG
                a = nextAB()
                t_mm = lin(w_grp[nb], CPG, [(A[a][:, :], lambda kc, g=g: pooledT[:, g * CPG + kc, :])],
                           extra_wait=ab_free[("A", a)])
                act.wait(t_mm)
                t_mx = act.mark(nc.scalar.activation(out=mixedT[:, nb, :], in_=A[a][:, :], func=AF.Copy,
                                                     scale=g_sb[:, 5, nb:nb + 1]))
                ab_free[("A", a)] = t_mx
            pe.wait(t_mx)
            dve.wait(t_mm)
            for nb in range(KC):
                a = nextAB()
                t_a = lin(w_bpool[nb], PC, [(A[a][:, :], lambda kc: mixedT[:, kc, :])], extra_wait=ab_free[("A", a)])
                t_b = lin(w_gpool[nb], KC, [(B[a][:, :], lambda kc: hT[:, kc, :])], extra_wait=ab_free[("B", a)])
                f = ft[nb % 2]
                dve.wait(t_b, fr(("ft", nb % 2)))
                t_g = dve.mark(nc.vector.tensor_tensor(out=f[:], in0=B[a][:, :], in1=rs[:], op=ALU.mult))
                ab_free[("B", a)] = t_g
                act.wait(t_g)
                t_s = act.mark(nc.scalar.activation(out=f[:], in_=f[:], func=AF.Sigmoid))
                dve.wait(t_s, t_a)
                t_m = dve.mark(nc.vector.tensor_tensor(out=MT[:, nb, :], in0=f[:], in1=A[a][:, :], op=ALU.mult))
                ab_free[("A", a)] = t_m
                free[("ft", nb % 2)] = t_m
            sp.wait(t_a)
            t_at = k.dma(sp, aux[:], attn_in.rearrange("(h q) t -> q h t", q=128)[:, :, tc0:tc0 + T], s_m)
            pe.wait(t_at)
            for nb in range(KC):
                a = nextAB()
                t_a = lin(w_bsb[nb], 16, [(A[a][:, :], lambda kc: aux[:, kc, :])], extra_wait=ab_free[("A", a)])
                t_b = lin(w_gsb[nb], KC, [(B[a][:, :], lambda kc: hT[:, kc, :])], extra_wait=ab_free[("B", a)])
                f = ft[nb % 2]
                dve.wait(t_b, fr(("ft", nb % 2)))
                t_g = dve.mark(nc.vector.tensor_tensor(out=f[:], in0=B[a][:, :], in1=rs[:], op=ALU.mult))
                ab_free[("B", a)] = t_g
                act.wait(t_g)
                t_s = act.mark(nc.scalar.activation(out=f[:], in_=f[:], func=AF.Sigmoid))
                dve.wait(t_s, t_a)
                t_m0 = dve.mark(nc.vector.tensor_tensor(out=f[:], in0=f[:], in1=A[a][:, :], op=ALU.mult))
                ab_free[("A", a)] = t_m0
                dve.wait(t_m0)
                t_m = dve.mark(nc.vector.tensor_tensor(out=MT[:, nb, :], in0=MT[:, nb, :], in1=f[:], op=ALU.add))
                free[("ft", nb % 2)] = t_m
            free["aux"] = t_a
            free["hT"] = t_b
            free["rs"] = t_g
            pe.wait(t_m)
            t_rs, t_h = residual_stage(p, KC, w_out, KC, lambda kc: MT[:, kc, :], lambda nb: xin[p, nb], xr1, 1)
            pe.wait(t_h)
            for hd in range(4):
                a = nextAB()
                t_mm = lin(w_xq[hd], KC, [(A[a][:, :], lambda kc: hT[:, kc, :])], extra_wait=ab_free[("A", a)])
                dve.wait(t_mm, t_rs, fr("qx"))
                t_q = dve.mark(nc.vector.scalar_tensor_tensor(out=qx[:, hd, :], in0=A[a][:, :], scalar=xa_scale, in1=rs[:],
                                                              op0=ALU.mult, op1=ALU.mult))
                ab_free[("A", a)] = t_q
            free["hT"] = t_mm
            free["rs"] = t_q
            pe.wait(t_q, t_mem_done)
            for hd in range(4):
                pe.wait(fr("X0"), fr("X1"))
                for mc in range(2):
                    ins = nc.tensor.matmul(X[mc][:, :], lhsT=kmT[:, hd, mc * 128:(mc + 1) * 128], rhs=qx[:, hd, :], start=True,
                                           stop=True)
                t_sc = pe.mark(ins)
                act.wait(t_sc, fr("pT"))
                for mc in range(2):
                    t_p = act.mark(nc.scalar.activation(out=pT[:, mc, :], in_=X[mc][:, :], func=AF.Exp))
                free["X0"] = t_p
                free["X1"] = t_p
                a = nextAB()
                pe.wait(t_p, ab_free[("A", a)], ab_free[("B", a)])
                for mc in range(2):
                    nc.tensor.matmul(B[a][:, :], lhsT=ones_b[:], rhs=pT[:, mc, :], start=(mc == 0), stop=(mc == 1))
                for mc in range(2):
                    ins = nc.tensor.matmul(A[a][:, :], lhsT=vm[:, mc, hd * 128:(hd + 1) * 128], rhs=pT[:, mc, :],
                                           start=(mc == 0), stop=(mc == 1))
                t_o = pe.mark(ins)
                free["pT"] = t_o
                f = ft[hd % 2]
                dve.wait(t_o, fr(("ft", hd % 2)), fr("ox"))
                t_rd = dve.mark(nc.vector.reciprocal(out=f[:], in_=B[a][:, :]))
                dve.wait(t_rd)
                t_ox = dve.mark(nc.vector.tensor_tensor(out=ox[:, hd, :], in0=A[a][:, :], in1=f[:], op=ALU.mult))
                ab_free[("A", a)] = t_ox
                ab_free[("B", a)] = t_ox
                free[("ft", hd % 2)] = t_ox
            free["qx"] = t_sc
            pe.wait(t_ox)
            src1 = lambda nb: xr1[nb]
            for nb in range(KC):
                st_tok[("src", id(src1), nb)] = st_tok[(id(xr1), nb)]
            t_rs, t_h = residual_stage(p, KC, w_xo, 4, lambda kc: ox[:, kc, :], src1, xr2, 3)
            free["ox"] = w_free[(wctr[0] - 1) % NWS]
            qp = aux
            pe.wait(t_h)
            dve.wait(fr("aux"))
            for nb in range(16):
                a = nextAB()
                t_mm = lin(w_pq[nb], KC, [(A[a][:, :], lambda kc: hT[:, kc, :])], extra_wait=ab_free[("A", a)])
                dve.wait(t_mm, t_rs)
                t_q = dve.mark(nc.vector.tensor_tensor(out=qp[:, nb, :], in0=A[a][:, :], in1=rs[:], op=ALU.mult))
                ab_free[("A", a)] = t_q
            MTf = MT[:].rearrange("q a b -> q (a b)").bitcast(F32)
            sc = MTf[:, 0:2048]
            sc2 = MTf[:, 2048:4096]
            cand = MTf[:, 4096:6144]
            pe.wait(t_q)
            dve.wait(fr("MT"), t_m)
            act.wait(t_m)
            t_tk = None
            for tt in range(4):
                tsl = slice(tt * 128, (tt + 1) * 128)
                pe.wait(fr("X0"), fr("X1"), fr("X2"), fr("SS"))
                banks = [X[0], X[1], X[2], SS]
                for l in range(16):
                    ins = nc.tensor.matmul(banks[l // 4][:, (l % 4) * 128:(l % 4 + 1) * 128], lhsT=qp[:, l, tsl],
                                           rhs=keys_b[:, l, :], start=True, stop=True)
                t_s = pe.mark(ins)
                act.wait(t_s, t_tk)
                for bi in range(4):
                    t_cp = act.mark(nc.scalar.copy(out=sc[:, bi * 512:(bi + 1) * 512], in_=banks[bi][:, :]))
                for nm in ("X0", "X1", "X2", "SS"):
                    free[nm] = t_cp
                dve.wait(t_cp)
                for l in range(16):
                    row = sc[:, l * 128:(l + 1) * 128]
                    row2 = sc2[:, l * 128:(l + 1) * 128]
                    t1 = dve.mark(nc.vector.max(out=top[:, l, 0:8], in_=row))
                    dve.wait(t1)
                    t2 = dve.mark(nc.vector.match_replace(out=row2, in_to_replace=top[:, l, 0:8], in_values=row, imm_value=NEG))
                    dve.wait(t2)
                    t3 = dve.mark(nc.vector.max(out=top[:, l, 8:16], in_=row2))
                dve.wait(t3)
                top4 = top[:].rearrange("q (h two) a -> q h two a", two=2)
                cand4 = cand[:, 0:2048].rearrange("q (h a b) -> q h a b", h=8, a=16)
                t_cd = dve.mark(nc.vector.tensor_tensor(out=cand4, in0=bc(top4[:, :, 0, :], 3, [128, 8, 16, 16]),
                                                        in1=bc(top4[:, :, 1, :], 2, [128, 8, 16, 16]), op=ALU.add))
                dve.wait(t_cd)
                for h in range(8):
                    row = cand[:, h * 256:(h + 1) * 256]
                    row2 = sc2[:, h * 256:(h + 1) * 256]
                    t1 = dve.mark(nc.vector.max(out=best[:, h, 0:8], in_=row))
                    dve.wait(t1)
                    t2 = dve.mark(nc.vector.match_replace(out=row2, in_to_replace=best[:, h, 0:8], in_values=row, imm_value=NEG))
                    dve.wait(t2)
                    t3 = dve.mark(nc.vector.max(out=best[:, h, 8:16], in_=row2))
                dve.wait(t3)
                dd = sc2[:, 0:128].rearrange("q (h a) -> q h a", h=8)
                t_d = dve.mark(nc.vector.tensor_tensor(out=dd, in0=best[:], in1=bc(best[:, :, 0], 2, [128, 8, 16]),
                                                       op=ALU.subtract))
                act.wait(t_d)
                t_e = act.mark(nc.scalar.activation(out=dd, in_=dd, func=AF.Exp))
                dve.wait(t_e)
                t_z = dve.mark(nc.vector.reduce_sum(out=zz[:, :, 0], in_=dd, axis=AX.X))
                act.wait(t_z)
                t_lz = act.mark(nc.scalar.activation(out=zz[:, :, 1], in_=zz[:, :, 0], func=AF.Ln))
                dve.wait(t_lz)
                nc.vector.scalar_tensor_tensor(out=negb[:, tt, :], in0=best[:, :, 0], scalar=-1.0, in1=zz[:, :, 1],
                                               op0=ALU.mult, op1=ALU.subtract)
                t_tk = dve.mark(nc.vector.tensor_scalar(out=tau[:, tt, :], in0=best[:, :, 15], scalar1=-2e-5, scalar2=None,
                                                        op0=ALU.add))
            act.wait(t_tk)
            WT = MT
            gctr = 0
            free[("S", 0)] = fr("X0")
            free[("S", 1)] = fr("SS")
            free[("S", 2)] = ab_free[("B", 0)]
            free[("S", 3)] = ab_free[("B", 1)]
            for i_ in range(4):
                free[("et", i_)] = [fr(("ft", 0)), fr(("ft", 1))]
            free["Gs"] = [fr("X1"), fr("X2")]
            for qtr in range(4):
                for grp in range(16):
                    i0 = qtr * 32 + grp * 2
                    Gs = [X[1], X[2]]
                    gits = [(tt, h) for tt in range(4) for h in range(8)]
                    grec = [dict() for _ in gits]

                    def gA(ii):
                        tt, h = gits[ii]
                        tsl = slice(tt * 128, (tt + 1) * 128)
                        it = gbase + ii
                        sb_ = it % 4
                        sbank = [X[0], SS, B[0], B[1]][sb_]
                        pe.wait(fr(("S", sb_)))
                        nc.tensor.matmul(sbank[:, 0:256], lhsT=qp[:, 2 * h, tsl],
                                         rhs=bc(keys_b[:, 2 * h, i0:i0 + 2], 2, [128, 2, 128]), start=True, stop=False)
                        t_S = pe.mark(nc.tensor.matmul(sbank[:, 0:256], lhsT=qp[:, 2 * h + 1, tsl],
                                                       rhs=bc(keys_b[:, 2 * h + 1, :], 1, [128, 2, 128]), start=False, stop=True))
                        e_t = ft[2 + sb_]
                        act.wait(t_S, fr(("et", sb_)))
                        t_E = act.mark(nc.scalar.activation(out=e_t[:, 0:256], in_=sbank[:, 0:256], func=AF.Exp,
                                                            bias=negb[:, tt, h:h + 1]))
                        g_t = bt[it % 6]
                        dve.wait(t_E, fr(("gt", it % 6)))
                        t_G = dve.mark(nc.vector.scalar_tensor_tensor(out=g_t[:, 0:256], in0=sbank[:, 0:256],
                                                                      scalar=tau[:, tt, h:h + 1], in1=e_t[:, 0:256],
                                                                      op0=ALU.is_ge, op1=ALU.mult))
                        free[("S", sb_)] = t_G
                        free[("et", sb_)] = t_G
                        grec[ii]["G"] = t_G

                    def gB(ii):
                        tt, h = gits[ii]
                        tsl = slice(tt * 128, (tt + 1) * 128)
                        it = gbase + ii
                        g_t = bt[it % 6]
                        pe.wait(grec[ii]["G"], fr("Gs") if ii == 0 else None)
                        for ib in range(2):
                            ins = nc.tensor.matmul(Gs[ib][:, tsl], lhsT=g_t[:, ib * 128:(ib + 1) * 128], rhs=ident_b[:],
                                                   start=(h == 0), stop=(h == 7))
                        free[("gt", it % 6)] = pe.mark(ins)

                    gbase = gctr
                    SK = 3
                    for ii in range(len(gits) + SK):
                        if ii < len(gits):
                            gA(ii)
                        if ii >= SK:
                            gB(ii - SK)
                    gctr += len(gits)
                    t_Gs = free[("gt", (gctr - 1) % 6)]
                    for ib in range(2):
                        eb = i0 + ib
                        ebl = eb - qtr * 32
                        a = nextAB()
                        t_mm = lin(w_down[eb], KC, [(A[a][:, :], lambda kc: hT[:, kc, :])], extra_wait=ab_free[("A", a)])
                        f = ft[ib]
                        dve.wait(t_mm, t_rs, fr(("ft", ib)))
                        t_a = dve.mark(nc.vector.tensor_tensor(out=f[:], in0=A[a][:, :], in1=rs[:], op=ALU.mult))
                        ab_free[("A", a)] = t_a
                        act.wait(t_a)
                        t_ge = act.mark(nc.scalar.activation(out=f[:], in_=f[:], func=AF.Gelu))
                        dve.wait(t_ge, t_Gs, fr("WT"))
                        t_w = dve.mark(nc.vector.tensor_tensor(out=WT[:, ebl, :], in0=f[:], in1=Gs[ib][:, :], op=ALU.mult))
                        free[("ft", ib)] = t_w
                    free["Gs"] = t_w
                pe.wait(t_w)
                free["SS"] = free[("S", 1)]
                free["X0"] = free[("S", 0)]
                free["X1"] = t_w
                free["X2"] = t_w
                last = (qtr == 3)
                if qtr == 0:
                    srcq = lambda nb: xr2[nb]
                    for nb in range(KC):
                        st_tok[("src", id(srcq), nb)] = st_tok[(id(xr2), nb)]
                else:
                    srcq = lambda nb: xr3[nb]
                    for nb in range(KC):
                        st_tok[("src", id(srcq), nb)] = st_tok[(id(xr3), nb)]
                t_pe_last = None
                pend = None
                for nb in range(KC):
                    a = nextAB()
                    t_mm = lin(w_up[qtr, nb], 32, [(A[a][:, :], lambda kc: WT[:, kc, :])], extra_wait=ab_free[("A", a)])
                    if pend is not None:
                        stats_mm(pend, nb - 1)
                        pend = None
                    s, t_x = load_x(srcq(nb), extra=st_tok.get(("src", id(srcq), nb)))
                    dve.wait(t_mm, t_x)
                    t_add = dve.mark(nc.vector.tensor_tensor(out=xc[s][:], in0=A[a][:, :], in1=xc[s][:], op=ALU.add))
                    ab_free[("A", a)] = t_add
                    sp.wait(t_add)
                    t_st = k.dma(sp, xr3[nb], xc[s][:], s_m)
                    st_tok[(id(xr3), nb)] = t_st
                    if last:
                        pend = stats_sq(xc[s][:], t_add)
                        x_free[s] = [t_st, pend[1]]
                        if nb == KC - 1:
                            t_pe_last = stats_mm(pend, nb)
                    else:
                        x_free[s] = [t_st]
                free["WT"] = t_mm
            free["hT"] = t_mm
            free["MT"] = t_mm
            free["aux"] = t_Gs
            ab_free[("B", 0)] = [ab_free[("B", 0)], free[("S", 2)]]
            ab_free[("B", 1)] = [ab_free[("B", 1)], free[("S", 3)]]
            t_rs, t_rs0 = make_rstd(rs[:], SS[:, :], t_pe_last, guard=[fr("rs"), t_a])
            free["SS"] = t_rs0
            for nb in range(KC):
                s, t_x = load_x(xr3[nb], extra=st_tok[(id(xr3), nb)])
                dve.wait(t_x, t_rs)
                t_o = dve.mark(nc.vector.scalar_tensor_tensor(out=xc[s][:], in0=xc[s][:], scalar=g_sb[:, 4, nb:nb + 1], in1=rs[:],
                                                              op0=ALU.mult, op1=ALU.mult))
                sp.wait(t_o)
                t_st = k.dma(sp, outT[p, nb], xc[s][:], s_m)
                x_free[s] = [t_st]
            free["rs"] = t_o
        sp.wait((s_m, s_m.count))
    return nc


def tile_w(W):
    K_, N_ = W.shape
    return np.ascontiguousarray(W.reshape(K_ // 128, 128, N_ // 128, 128).transpose(2, 1, 0, 3))


def vec_pk(v, KC):
    o = np.zeros((128, KC), np.float32)
    n = v.shape[0] // 128
    o[:, :n] = v.reshape(n, 128).T
    return o


def make_cst():
    s_idx = np.arange(128)
    cst = np.zeros((128, 384), np.float32)
    cst[:, 0:128] = (s_idx[:, None] < s_idx[None, :])
    cst[:, 128:256] = -1.0 * (s_idx[:, None] >= s_idx[None, :])
    cst[:, 256:384] = np.eye(128)
    return cst


def host_phase2_shared(inp, D):
    KC = D // 128
    SBW = 2048
    PW = D // 2
    PC = PW // 128
    w_in = inp["w_in"][0]
    o = 3 * SBW
    sh = {}
    sh["w_pool"] = tile_w(w_in[:, o:o + PW])
    sh["w_gsb"] = tile_w(w_in[:, o + PW:o + PW + D])
    sh["w_gpool"] = tile_w(w_in[:, o + PW + D:o + PW + 2 * D])
    sh["w_grp"] = np.concatenate([tile_w(inp["pool_group_w"][0, g]) for g in range(4)], axis=0)
    sh["w_bsb"] = tile_w(inp["w_branch_sb"][0])
    sh["w_bpool"] = tile_w(inp["w_branch_pool"][0])
    sh["w_out"] = tile_w(inp["w_out"][0])
    sh["w_xq"] = tile_w(inp["xa_w_q"][0])
    sh["w_xkv"] = tile_w(inp["xa_w_kv"][0])
    sh["w_xo"] = tile_w(inp["xa_w_o"][0])
    sh["w_pq"] = tile_w(inp["peer_w_query"][0])
    sh["w_down"] = tile_w(inp["peer_down"][0].T)
    sh["w_up"] = np.stack([tile_w(inp["peer_up"][0][q * 4096:(q + 1) * 4096]) for q in range(4)], axis=0)
    sh["keysT"] = np.ascontiguousarray(inp["peer_sub_keys"][0].transpose(3, 0, 1, 2).reshape(128, 16, 128))
    g = np.zeros((128, 6, KC), np.float32)
    g[:, 0] = vec_pk(inp["norm_mix"][0], KC)
    g[:, 1] = vec_pk(inp["norm_mem_q"][0], KC)
    g[:, 2] = vec_pk(inp["norm_mem_kv"][0], KC)
    g[:, 3] = vec_pk(inp["norm_ffn"][0], KC)
    g[:, 4] = vec_pk(inp["norm_final"], KC)
    g[:, 5] = vec_pk(inp["pool_scale"][0], KC)
    sh["gains"] = g
    sh["cst"] = make_cst()
    mem = inp["mem"][0]
    sh["memT"] = np.ascontiguousarray(mem.reshape(256, KC, 128).transpose(1, 2, 0))
    return sh


def host_phase2_core(x, attn_full, tok0, TOK, D):
    KC = D // 128
    NP = TOK // 512
    xs = x[tok0:tok0 + TOK]
    m = {}
    m["xin"] = np.ascontiguousarray(xs.reshape(NP, 512, KC, 128).transpose(0, 2, 3, 1))
    xh = np.zeros((NP, 128, KC, 16), np.float32)
    ic = np.zeros((NP, 128, 4, 16), np.float32)
    for p in range(NP):
        st = tok0 + p * 512
        if st >= 16:
            xh[p] = x[st - 16:st].reshape(16, KC, 128).transpose(2, 1, 0)
        pos = st + np.arange(16)
        for gi, w in enumerate((2, 4, 8, 16)):
            ic[p, :, gi, :] = 1.0 / np.minimum(pos + 1, w)
    m["xhalo"] = xh
    m["invc"] = ic
    m["attn_in"] = np.ascontiguousarray(attn_full[:, tok0:tok0 + TOK])
    return m


def unpack_out(outT, TOK, D):
    KC = D // 128
    NP = TOK // 512
    return np.ascontiguousarray(outT.reshape(NP, KC, 128, 512).transpose(0, 3, 1, 2)).reshape(TOK, D)


def kernel(**inp):
    S, D = 8192, 4096
    inp = {k_: np.asarray(v) for k_, v in inp.items()}
    nc = bass.Bass("TRN2", target_bir_lowering=False)
    build_fused(nc, S, D)
    maps, owners = host_fused_inputs(inp, S, D)
    r = run_bass_kernel_spmd(nc, maps, core_ids=list(range(NCORES)))
    out = np.zeros((S, D), np.float32)
    for c in range(NCORES):
        o = unpack_out(r.results[c]["outT"], 1024, D)
        for p, g in enumerate(owners[c]):
            out[g * 512:(g + 1) * 512] = o[p * 512:(p + 1) * 512]
    return out.reshape(1, S, D)


def declare_tensors(nc, S, TOK, D):
    KC = D // 128
    NT = S // 128
    NP = TOK // 512
    PC = (D // 2) // 128
    CPG = PC // 4
    T = 512

    def din(name, shape, dt=F32):
        return nc.dram_tensor(name, shape, dt, kind="ExternalInput").ap()
    t = {}
    t["xT"] = din("xT", [NT, 128, KC, 128])
    t["wqkv"] = din("wqkv", [8, 128, KC, 768])
    t["bias_tab"] = din("bias_tab", [2, NT, 128, 512], BF16)
    t["xin"] = din("xin", [NP, KC, 128, T])
    t["xhalo"] = din("xhalo", [NP, 128, KC, 16])
    t["invc"] = din("invc", [NP, 128, 4, 16])
    t["memT"] = din("memT", [KC, 128, 256])
    t["gains"] = din("gains", [128, 6, KC])
    t["cst"] = din("cst", [128, 384])
    t["keysT"] = din("keysT", [128, 16, 128])
    t["w_pool"] = din("w_pool", [PC, 128, KC, 128])
    t["w_grp"] = din("w_grp", [PC, 128, CPG, 128])
    t["w_gsb"] = din("w_gsb", [KC, 128, KC, 128])
    t["w_gpool"] = din("w_gpool", [KC, 128, KC, 128])
    t["w_bsb"] = din("w_bsb", [KC, 128, 16, 128])
    t["w_bpool"] = din("w_bpool", [KC, 128, PC, 128])
    t["w_out"] = din("w_out", [KC, 128, KC, 128])
    t["w_xq"] = din("w_xq", [4, 128, KC, 128])
    t["w_xkv"] = din("w_xkv", [8, 128, KC, 128])
    t["w_xo"] = din("w_xo", [KC, 128, 4, 128])
    t["w_pq"] = din("w_pq", [16, 128, KC, 128])
    t["w_down"] = din("w_down", [128, 128, KC, 128])
    t["w_up"] = din("w_up", [4, KC, 128, 32, 128])
    t["outT"] = nc.dram_tensor("outT", [NP, KC, 128, T], F32, kind="ExternalOutput").ap()
    t["attn_scr"] = nc.dram_tensor("attn_scr", [16 * 128, TOK], BF16).ap()
    return t


def build_fused(nc, S, D):
    TOK = 1024
    t = declare_tensors(nc, S, TOK, D)
    with contextlib.ExitStack() as sem_stack:
        k = K(nc, None, sem_stack)
        build_phase1(nc, k, S, D, t)
        build_phase2(nc, k, TOK, D, t)
    return nc


def host_fused_inputs(inp, S, D):
    KC = D // 128
    NT = S // 128
    NG = S // 512
    x = inp["x"][0]
    sh = host_phase2_shared(inp, D)
    sh["xT"] = np.ascontiguousarray(x.reshape(NT, 128, KC, 128).transpose(0, 3, 2, 1))
    w_in = inp["w_in"][0]
    SBW = 2048
    wq = np.zeros((8, 128, KC, 768), np.float32)
    for hp in range(8):
        cols = []
        for part in range(3):
            for hh in range(2):
                h = hp * 2 + hh
                cols.append(w_in[:, part * SBW + h * 128: part * SBW + (h + 1) * 128])
        w = np.concatenate(cols, axis=1)
        wq[hp] = w.reshape(KC, 128, -1).transpose(1, 0, 2)
    sh["wqkv"] = wq
    maps = []
    owners = []
    s_idx = np.arange(128)
    t_idx = np.arange(512)
    for c in range(NCORES):
        groups = (c, NG - 1 - c)
        owners.append(groups)
        m = dict(sh)
        xs = np.concatenate([x[g * 512:(g + 1) * 512] for g in groups], axis=0)
        m["xin"] = np.ascontiguousarray(xs.reshape(2, 512, KC, 128).transpose(0, 2, 3, 1))
        xh = np.zeros((2, 128, KC, 16), np.float32)
        ic = np.zeros((2, 128, 4, 16), np.float32)
        bt = np.zeros((2, NT, 128, 512), np.float32)
        for p, g in enumerate(groups):
            st = g * 512
            if st >= 16:
                xh[p] = x[st - 16:st].reshape(16, KC, 128).transpose(2, 1, 0)
            pos = st + np.arange(16)
            for gi, w in enumerate((2, 4, 8, 16)):
                ic[p, :, gi, :] = 1.0 / np.minimum(pos + 1, w)
            qpos = st + t_idx
            for kb in range(NT):
                kpos = kb * 128 + s_idx
                bt[p, kb] = np.where(kpos[:, None] < qpos[None, :], 0.0, -30000.0)
        m["xhalo"] = xh
        m["invc"] = ic
        m["bias_tab"] = bt.astype(ml_dtypes.bfloat16)
        maps.append(m)
    return maps, owners
```

```python
import contextlib
import numpy as np
import ml_dtypes
import concourse.bass as bass
import concourse.mybir as mybir
from concourse.bass_utils import run_bass_kernel_spmd

F32, BF16 = mybir.dt.float32, mybir.dt.bfloat16
AF = mybir.ActivationFunctionType
ALU = mybir.AluOpType
AX = mybir.AxisListType

NCORES = 8
EPS = 1e-6
NEG = -1.0e30


class Sem:
    def __init__(self, handle):
        self.h = handle
        self.count = 0


class Eng:
    def __init__(self, k, eng, name):
        self.k, self.e, self.name = k, eng, name
        self.sem = k.new_sem(name)
        self.seen = {}

    def wait(self, *toks):
        for t in toks:
            if t is None:
                continue
            if isinstance(t, (list, tuple)) and len(t) and not isinstance(t[0], Sem):
                self.wait(*t)
                continue
            s, v = t
            if self.seen.get(id(s), 0) >= v:
                continue
            self.e.wait_ge(s.h, v)
            self.seen[id(s)] = v

    def mark(self, ins):
        ins.then_inc(self.sem.h, 1)
        self.sem.count += 1
        return (self.sem, self.sem.count)


class K:
    def __init__(self, nc, stack, sem_stack=None):
        self.nc, self.stack = nc, stack
        self.sem_stack = sem_stack if sem_stack is not None else stack
        self._sems = []
        self.pe = Eng(self, nc.tensor, "pe")
        self.act = Eng(self, nc.scalar, "act")
        self.dve = Eng(self, nc.vector, "dve")
        self.pool = Eng(self, nc.gpsimd, "pool")
        self.sp = Eng(self, nc.sync, "sp")

    def new_sem(self, name):
        s = Sem(self.sem_stack.enter_context(self.nc.semaphore(name + "_%d" % len(self._sems))))
        self._sems.append(s)
        return s

    def sbuf(self, name, shape, dt):
        return self.stack.enter_context(self.nc.sbuf_tensor(name, shape, dt))

    def psum(self, name, shape, dt=F32):
        return self.stack.enter_context(self.nc.psum_tensor(name, shape, dt))

    def dma(self, q, out, in_, sem):
        ins = q.e.dma_start(out=out, in_=in_)
        ins.then_inc(sem.h, 16)
        sem.count += 16
        return (sem, sem.count)


def bc(ap, axis, shape):
    return ap.unsqueeze(axis).broadcast_to(list(shape))


def build_phase1(nc, k, S, D, T_in):
    HPC = 2
    KC = D // 128
    NT = S // 128
    xT, xin, wqkv, gains, cst, bias_tab, attn_scr = (T_in[n] for n in
                                                     ("xT", "xin", "wqkv", "gains", "cst", "bias_tab", "attn_scr"))
    NKB = (NT // 2, NT)
    with contextlib.ExitStack() as st:
        k.stack = st
        nc_ = nc
        pe, act, dve, pool, sp = k.pe, k.act, k.dve, k.pool, k.sp
        W = k.sbuf("W", [128, KC, 3 * HPC * 128], BF16)
        KT = k.sbuf("KT", [128, HPC, S], BF16)
        V = k.sbuf("V", [128, NT, HPC * 128], BF16)
        QT = [k.sbuf("QT%d" % i, [128, HPC, 512], BF16) for i in range(2)]
        xs = [k.sbuf("xs%d" % i, [128, KC, 128], F32) for i in range(2)]
        sq = k.sbuf("sq", [128, KC, 128], BF16)
        hbl = [k.sbuf("hb%d" % i, [128, KC, 128], BF16) for i in range(2)]
        g_sb = k.sbuf("g1", [128, 6, KC], F32)
        c_sb = k.sbuf("c1", [128, 384], F32)
        ones_b = k.sbuf("ones1", [128, 128], BF16)
        nones_b = k.sbuf("nones", [128, 128], BF16)
        ntri_b = k.sbuf("ntri", [128, 128], BF16)
        ident_b = k.sbuf("ident1", [128, 128], BF16)
        rstdl = [k.sbuf("rstd%d" % i, [128, 132], F32) for i in range(2)]
        ebuf = [k.sbuf("eb%d" % i, [128, 512], F32) for i in range(3)]
        spb = [k.sbuf("sp%d" % i, [128, 512], BF16) for i in range(3)]
        atb = [k.sbuf("at%d" % i, [128, 512], BF16) for i in range(3)]
        bias = [k.sbuf("bias%d" % i, [128, 512], BF16) for i in range(3)]
        spacc = k.sbuf("spacc", [128, HPC, 512], BF16)
        ostage = [k.sbuf("os%d" % i, [128, 512], BF16) for i in range(2)]
        pz = [k.psum("pz%d" % i, [128, 512]) for i in range(3)]
        po = [k.psum("po%d" % i, [128, 512]) for i in range(HPC)]
        pqk = k.psum("pqk", [128, 512])
        pv = k.psum("pv", [128, 512])

        s_c = k.new_sem("ldc")
        s_w = k.new_sem("ldw")
        s_x = [k.new_sem("ldx") for _ in range(2)]
        s_o = [k.new_sem("sto") for _ in range(2)]
        s_b = [k.new_sem("ldb") for _ in range(3)]

        k.dma(sp, g_sb[:], gains, s_c)
        t_c = k.dma(sp, c_sb[:], cst, s_c)
        dve.wait(t_c)
        nc.vector.memset(ones_b[:], 1.0)
        nc.vector.memset(nones_b[:], -1.0)
        nc.vector.tensor_copy(out=ntri_b[:], in_=c_sb[:, 128:256])
        t_cst = dve.mark(nc.vector.tensor_copy(out=ident_b[:], in_=c_sb[:, 256:384]))
        pe.wait(t_cst)
        act.wait(t_cst)
        pool.wait(t_cst)

        scale = 128.0 ** -0.5
        st8 = {"x_free": [None, None], "sq_free": None, "hb_free": [None, None], "pqk_free": None, "pv_free": None,
               "rstd_free": [None, None], "xctr": 0, "scr_ready": None, "w_free": None, "kv_free": None, "last": None}
        qt_free = [None, None]

        hb_scr = nc.dram_tensor("hb_scr", [NT + 8, 128, KC, 128], BF16).ap()
        rs_scr = nc.dram_tensor("rs_scr", [NT + 8, 128, 132], F32).ap()
        s_hsl = [k.new_sem("sths") for _ in range(2)]
        s_r = [k.new_sem("ldr") for _ in range(2)]
        st8["rstd_free"] = [None, None]

        def project(src_ap, mode, idx, first, tt=None, slot=None, c0=None):
            d = st8
            sl = d["xctr"] % 2
            d["xctr"] += 1
            hb = hbl[sl]
            rstd = rstdl[sl]
            if first:
                sp.wait(d["x_free"][sl])
                t_x = k.dma(sp, xs[sl][:], src_ap, s_x[sl])
                act.wait(t_x, d["sq_free"])
                t_sq = act.mark(nc.scalar.activation(out=sq[:], in_=xs[sl][:], func=AF.Square))
                pool.wait(t_x, d["hb_free"][sl])
                t_hb = pool.mark(nc.gpsimd.tensor_tensor(out=hb[:], in0=xs[sl][:], in1=bc(g_sb[:, 0, :], 2, [128, KC, 128]),
                                                         op=ALU.mult))
                d["x_free"][sl] = [t_sq, t_hb]
                pool.wait(t_hb)
                t_hs = k.dma(pool, hb_scr[idx], hb[:], s_hsl[sl])
                pe.wait(t_sq, d["pv_free"])
                for kc in range(KC):
                    nc.tensor.matmul(pv[:, 256:384], lhsT=ones_b[:], rhs=sq[:, kc, :], start=(kc == 0), stop=(kc == KC - 1))
                for kc in range(KC):
                    ins = nc.tensor.matmul(pv[:, 384:385], lhsT=sq[:, kc, :], rhs=ones_b[:, 0:1], start=(kc == 0),
                                           stop=(kc == KC - 1))
                t_ss = pe.mark(ins)
                d["sq_free"] = t_ss
                dve.wait(t_ss, d["rstd_free"][sl])
                t_r00 = dve.mark(nc.vector.tensor_scalar(out=rstd[:, 0:129], in0=pv[:, 256:385], scalar1=1.0 / D, scalar2=EPS,
                                                         op0=ALU.mult, op1=ALU.add))
                act.wait(t_r00)
                t_r01 = act.mark(nc.scalar.activation(out=rstd[:, 0:129], in_=rstd[:, 0:129], func=AF.Ln))
                act.wait(t_r01)
                t_r0 = act.mark(nc.scalar.activation(out=rstd[:, 0:129], in_=rstd[:, 0:129], func=AF.Exp, scale=-0.5))
                act.wait(t_r0)
                t_rs = k.dma(act, rs_scr[idx][:, 0:129], rstd[:, 0:129], s_hsl[sl])
                pv_rel = [t_r00]
                extra_free = [t_hs, t_rs]
            else:
                sp.wait(d["hb_free"][sl], d["rstd_free"][sl], d["scr_ready"])
                t_hb = k.dma(sp, hb[:], hb_scr[idx], s_x[sl])
                t_r0 = k.dma(sp, rstd[:, 0:129], rs_scr[idx][:, 0:129], s_r[sl])
                pv_rel = []
                extra_free = []
            pe.wait(t_hb, d["pqk_free"], d["w_ready"])
            blks = range(HPC, 2 * HPC) if mode == "kv" else range(0, HPC)
            for blk in blks:
                for kc in range(KC):
                    ins = nc.tensor.matmul(pqk[:, blk * 128:(blk + 1) * 128], lhsT=W[:, kc, blk * 128:(blk + 1) * 128],
                                           rhs=hb[:, kc, :], start=(kc == 0), stop=(kc == KC - 1))
            t_qk = pe.mark(ins)
            t_v = None
            if mode == "kv":
                pe.wait(d["pv_free"])
                for kc in range(KC):
                    ins = nc.tensor.matmul(pv[:, 0:HPC * 128], lhsT=hb[:, kc, :], rhs=W[:, kc, 2 * HPC * 128:3 * HPC * 128],
                                           start=(kc == 0), stop=(kc == KC - 1))
                t_v = pe.mark(ins)
            d["hb_free"][sl] = [t_v if t_v is not None else t_qk] + extra_free
            d["w_last"] = t_v if t_v is not None else t_qk
            if mode == "kv":
                dve.wait(t_qk, t_r0, d["kv_free"])
                for hh in range(HPC):
                    ins = nc.vector.tensor_tensor(out=KT[:, hh, tt * 128:(tt + 1) * 128],
                                                  in0=pqk[:, (HPC + hh) * 128:(HPC + hh + 1) * 128], in1=rstd[:, 0:128],
                                                  op=ALU.mult)
                t_e1 = dve.mark(ins)
                act.wait(t_v, t_r0, d["kv_free"])
                t_e2 = act.mark(nc.scalar.activation(out=V[:, tt, :], in_=pv[:, 0:HPC * 128], func=AF.Copy,
                                                     scale=rstd[:, 128:129]))
                d["pv_free"] = [t_e2] + pv_rel
                d["rstd_free"][sl] = [t_e1, t_e2] + extra_free
                d["last"] = [t_e1, t_e2]
                d["last_kv"] = [t_e1, t_e2]
            else:
                dve.wait(t_qk, t_r0, qt_free[slot])
                for hh in range(HPC):
                    ins = nc.vector.scalar_tensor_tensor(out=QT[slot][:, hh, c0:c0 + 128], in0=pqk[:, hh * 128:(hh + 1) * 128],
                                                         scalar=scale, in1=rstd[:, 0:128], op0=ALU.mult, op1=ALU.mult)
                t_e1 = dve.mark(ins)
                if pv_rel:
                    d["pv_free"] = pv_rel
                d["rstd_free"][sl] = [t_e1] + extra_free
                d["last"] = [t_e1]
            d["pqk_free"] = t_e1

        it_ctr = [0]
        pz_free = [None] * 3
        sp_free = [None] * 3
        at_free = [None] * 3
        b_free = [None] * 3
        b_ctr = [0]
        po_free = [None] * HPC
        spacc_tok = [None] * HPC
        os_free = [None, None]
        o_ctr = [0]
        out_toks = []

        def attention(hp, slot, filler=None):
            nkb = NKB[slot]
            qt = QT[slot]
            its = [(kb, hh) for kb in reversed(range(nkb)) for hh in range(HPC)]
            n = len(its)
            stt = [dict() for _ in range(n)]
            pe.wait(st8["last"], st8["last_kv"])
            pool.wait(st8["last"], st8["last_kv"])
            for hh in range(HPC):
                pool.wait(spacc_tok[hh])
            t_z = pool.mark(nc.gpsimd.memset(spacc[:], 0.0))
            for hh in range(HPC):
                spacc_tok[hh] = t_z
            last_av = [None] * HPC
            btile = {}

            def st0(i):
                kb, hh = its[i]
                b = (it_ctr[0] + i) % 3
                d = stt[i]
                d["b"] = b
                if hh == 0:
                    bs = b_ctr[0] % 3
                    b_ctr[0] += 1
                    sp.wait(b_free[bs])
                    btile[kb] = (bs, k.dma(sp, bias[bs][:], bias_tab[slot, kb], s_b[bs]))
                bs, t_b = btile[kb]
                pe.wait(pz_free[b], t_b)
                nc.tensor.matmul(pz[b][:, :], lhsT=KT[:, hh, kb * 128:(kb + 1) * 128], rhs=qt[:, hh, :], start=True, stop=False)
                d["z"] = pe.mark(nc.tensor.matmul(pz[b][:, :], lhsT=ident_b[:], rhs=bias[bs][:], start=False, stop=False))
                if hh == HPC - 1:
                    b_free[bs] = d["z"]
                act.wait(d["z"], sp_free[b])
                t_e = act.mark(nc.scalar.activation(out=ebuf[b][:], in_=pz[b][:, :], func=AF.Exp))
                act.wait(t_e)
                d["ln"] = act.mark(nc.scalar.activation(out=spb[b][:], in_=ebuf[b][:], func=AF.Ln, bias=1.0))

            def st1(i):
                kb, hh = its[i]
                d = stt[i]
                b = d["b"]
                first = (kb == nkb - 1)
                pe.wait(d["ln"], spacc_tok[hh])
                ins = nc.tensor.matmul(pz[b][:, :], lhsT=ntri_b[:], rhs=spb[b][:], start=False, stop=first)
                if not first:
                    ins = nc.tensor.matmul(pz[b][:, :], lhsT=nones_b[:], rhs=spacc[:, hh, :], start=False, stop=True)
                d["cs"] = pe.mark(ins)
                act.wait(d["cs"], at_free[b])
                d["ea"] = act.mark(nc.scalar.activation(out=atb[b][:], in_=pz[b][:, :], func=AF.Exp))
                pz_free[b] = d["ea"]
                pool.wait(d["cs"], d["ln"], spacc_tok[hh])
                spacc_tok[hh] = pool.mark(nc.gpsimd.tensor_tensor(out=spacc[:, hh, :], in0=spacc[:, hh, :], in1=spb[b][:],
                                                                  op=ALU.add))
                sp_free[b] = spacc_tok[hh]

            def st2(i):
                kb, hh = its[i]
                d = stt[i]
                b = d["b"]
                pe.wait(d["ea"], po_free[hh] if kb == nkb - 1 else None)
                d["av"] = pe.mark(nc.tensor.matmul(po[hh][:, :], lhsT=V[:, kb, hh * 128:(hh + 1) * 128], rhs=atb[b][:],
                                                   start=(kb == nkb - 1), stop=(kb == 0)))
                at_free[b] = d["av"]
                last_av[hh] = d["av"]

            for step in range(n + 2):
                if step < n:
                    st0(step)
                if 0 <= step - 1 < n:
                    st1(step - 1)
                if 0 <= step - 2 < n:
                    st2(step - 2)
                if filler is not None and step % 2 == 1:
                    filler()
            it_ctr[0] += n
            qt_free[slot] = [last_av[hh] for hh in range(HPC)]
            st8["kv_free"] = [last_av[hh] for hh in range(HPC)]
            for hh in range(HPC):
                o = o_ctr[0] % 2
                o_ctr[0] += 1
                act.wait(last_av[hh], os_free[o])
                t_cp = act.mark(nc.scalar.copy(out=ostage[o][:], in_=po[hh][:, :]))
                po_free[hh] = t_cp
                sp.wait(t_cp)
                h = hp * HPC + hh
                os_free[o] = k.dma(sp, attn_scr[h * 128:(h + 1) * 128, slot * 512:(slot + 1) * 512], ostage[o][:], s_o[o])
                out_toks.append(os_free[o])

        st8["w_ready"] = None
        st8["w_last"] = None
        for hp in range(8):
            pool.wait(st8["w_last"], st8["kv_free"])
            for i in range(3 * HPC):
                t_w = k.dma(pool, W[:, :, i * 128:(i + 1) * 128], wqkv[hp][:, :, i * 128:(i + 1) * 128], s_w)
            st8["w_ready"] = t_w
            def proj_q(j):
                slot, c0 = j // 4, (j % 4) * 128
                project(xin[slot][:, :, c0:c0 + 128].rearrange("kc q t -> q kc t"), "q", NT + j, hp == 0, slot=slot, c0=c0)

            half = NT // 2
            for tt in range(half):
                project(xT[tt], "kv", tt, hp == 0, tt=tt)
            for j in range(4):
                proj_q(j)
            pending = list(range(half, NT))

            def filler():
                if pending:
                    tt = pending.pop(0)
                    project(xT[tt], "kv", tt, hp == 0, tt=tt)
            attention(hp, 0, filler=filler)
            while pending:
                filler()
            for j in range(4, 8):
                proj_q(j)
            if hp == 0:
                st8["scr_ready"] = [(s_, s_.count) for s_ in s_hsl]
            attention(hp, 1)
        fin = [(e.sem, e.sem.count) for e in (pe, act, dve, pool)] + [(s_, s_.count) for s_ in s_o]
        for e in (pe, act, dve, pool, sp):
            e.wait(fin)
    return nc


def host_phase1_inputs(x, w_in, norm_mix, S, D, HPC, ncores):
    KC = D // 128
    NT = S // 128
    SBW = ncores * HPC * 128
    xT = np.ascontiguousarray(x.reshape(NT, 128, KC, 128).transpose(0, 3, 2, 1))
    g = np.ascontiguousarray(norm_mix.reshape(KC, 128).T)
    s_idx = np.arange(128)
    cst = np.zeros((128, 384), np.float32)
    cst[:, 0:128] = (s_idx[:, None] < s_idx[None, :])
    cst[:, 128:256] = -1.0 * (s_idx[:, None] >= s_idx[None, :])
    cst[:, 256:384] = np.eye(128)
    maps = []
    for c in range(ncores):
        cols = []
        for part in range(3):
            for hh in range(HPC):
                h = c * HPC + hh
                cols.append(w_in[:, part * SBW + h * 128: part * SBW + (h + 1) * 128])
        w = np.concatenate(cols, axis=1)
        w = np.ascontiguousarray(w.reshape(KC, 128, -1).transpose(1, 0, 2))
        maps.append({"xT": xT, "wqkv": w, "gmix": g, "cst": cst})
    return maps


def build_phase2(nc, k, TOK, D, T_in):
    KC = D // 128
    NP = TOK // 512
    PC = (D // 2) // 128
    CPG = PC // 4
    NE = 128
    T = 512
    (xin, xhalo, invc, attn_in, memT, gains, cst, keysT, w_pool, w_grp, w_gsb, w_gpool, w_bsb, w_bpool, w_out, w_xq, w_xkv,
     w_xo, w_pq, w_down, w_up, outT) = (T_in[n] for n in (
        "xin", "xhalo", "invc", "attn_scr", "memT", "gains", "cst", "keysT", "w_pool", "w_grp", "w_gsb", "w_gpool", "w_bsb",
        "w_bpool", "w_out", "w_xq", "w_xkv", "w_xo", "w_pq", "w_down", "w_up", "outT"))
    xr1 = nc.dram_tensor("xr1", [KC, 128, T], F32).ap()
    xr2 = nc.dram_tensor("xr2", [KC, 128, T], F32).ap()
    xr3 = nc.dram_tensor("xr3", [KC, 128, T], F32).ap()

    with contextlib.ExitStack() as st:
        k.stack = st
        pe, act, dve, pool, sp = k.pe, k.act, k.dve, k.pool, k.sp
        KW = max(KC, 32)
        hT = k.sbuf("hT", [128, KC, T], BF16)
        MT = k.sbuf("MT", [128, 32, T], BF16)
        aux = k.sbuf("aux", [128, 16, T], BF16)
        NWS = 6
        wsl = [k.sbuf("ws%d" % i, [128, KW, 128], BF16) for i in range(NWS)]
        xc = [k.sbuf("xc%d" % i, [128, T], F32) for i in range(3)]
        sqc = [k.sbuf("sqc%d" % i, [128, T], BF16) for i in range(2)]
        ft = [k.sbuf("ft%d" % i, [128, T], F32) for i in range(6)]
        bt = [k.sbuf("bt%d" % i, [128, T], BF16) for i in range(6)]
        rs = k.sbuf("rs", [128, T], F32)
        rsh = k.sbuf("rsh", [128, 16], F32)
        rsm = k.sbuf("rsm", [128, 260], F32)
        g_sb = k.sbuf("g", [128, 6, KC], F32)
        c_sb = k.sbuf("c", [128, 384], F32)
        ones_b = k.sbuf("ones", [128, 128], BF16)
        ident_b = k.sbuf("identb", [128, 128], BF16)
        keys_b = k.sbuf("keysb", [128, 16, 128], BF16)
        xh = k.sbuf("xh", [128, KC, 16], F32)
        sqh = k.sbuf("sqh", [128, KC, 16], BF16)
        hh = k.sbuf("hh", [128, KC, 16], BF16)
        ubuf = k.sbuf("ubuf", [128, 528], F32)
        ra = k.sbuf("ra", [128, 528], F32)
        rb = k.sbuf("rb", [128, 528], F32)
        icv = k.sbuf("icv", [128, 4, 16], F32)
        t16 = k.sbuf("t16", [128, 16], F32)
        kmT = k.sbuf("kmT", [128, 4, 256], BF16)
        vm = k.sbuf("vm", [128, 2, 512], BF16)
        qx = k.sbuf("qx", [128, 4, T], BF16)
        ox = k.sbuf("ox", [128, 4, T], BF16)
        pT = k.sbuf("pT", [128, 2, T], BF16)
        top = k.sbuf("top", [128, 16, 16], F32)
        best = k.sbuf("best", [128, 8, 16], F32)
        zz = k.sbuf("zz", [128, 8, 4], F32)
        tau = k.sbuf("tau", [128, 4, 8], F32)
        negb = k.sbuf("negb", [128, 4, 8], F32)
        ps = [k.psum("ps%d" % i, [128, 512]) for i in range(8)]
        A, B, SS, X = ps[0:2], ps[2:4], ps[4], ps[5:8]

        s_c = k.new_sem("ldc")
        s_w = [k.new_sem("ldw") for _ in range(6)]
        s_x = [k.new_sem("ldx") for _ in range(3)]
        s_m = k.new_sem("misc")

        k.dma(sp, g_sb[:], gains, s_c)
        t_kb = k.dma(pool, keys_b[:], keysT, s_w[0])
        t_c = k.dma(sp, c_sb[:], cst, s_c)
        dve.wait(t_c)
        nc.vector.memset(ones_b[:], 1.0)
        dve.wait(t_kb)
        t_cst = dve.mark(nc.vector.tensor_copy(out=ident_b[:], in_=c_sb[:, 256:384]))
        pe.wait(t_cst)
        act.wait(t_cst)

        wctr = [0]
        w_free = [None] * NWS
        free = {}

        def fr(name):
            return free.get(name)

        def load_w(tile_ap, KCw):
            s = wctr[0] % NWS
            wctr[0] += 1
            pool.wait(w_free[s])
            t = k.dma(pool, wsl[s][:, 0:KCw, :], tile_ap, s_w[s])
            return s, t

        def lin(tile_ap, KCw, outs, extra_wait=None):
            s, t = load_w(tile_ap, KCw)
            pe.wait(t, extra_wait)
            ins = None
            for (pap, rf) in outs:
                for kc in range(KCw):
                    ins = nc.tensor.matmul(pap, lhsT=wsl[s][:, kc, :], rhs=rf(kc), start=(kc == 0), stop=(kc == KCw - 1))
            tok = pe.mark(ins)
            w_free[s] = tok
            return tok

        xctr = [0]
        x_free = [None] * 3

        def load_x(src_ap, extra=None, n=T):
            s = xctr[0] % 3
            xctr[0] += 1
            sp.wait(x_free[s], extra)
            t = k.dma(sp, xc[s][:, 0:n], src_ap, s_x[s])
            return s, t

        sqctr = [0]
        sq_free = [None] * 2

        def stats_sq(src_ap, src_tok, n=T):
            s = sqctr[0] % 2
            sqctr[0] += 1
            act.wait(src_tok, sq_free[s])
            t = act.mark(nc.scalar.activation(out=sqc[s][:, 0:n], in_=src_ap, func=AF.Square))
            return (s, t, n)

        def stats_mm(pend, kc):
            s, t, n = pend
            pe.wait(t, fr("SS") if kc == 0 else None)
            tp = pe.mark(nc.tensor.matmul(SS[:, 0:n], lhsT=ones_b[:], rhs=sqc[s][:, 0:n], start=(kc == 0),
                                          stop=(kc == KC - 1)))
            sq_free[s] = tp
            return tp

        def stats_acc(src_ap, src_tok, kc, n=T):
            pend = stats_sq(src_ap, src_tok, n)
            return pend[1], stats_mm(pend, kc)

        def make_rstd(dst_ap, src_ps_ap, tok, guard=None):
            dve.wait(tok, guard)
            t0 = dve.mark(nc.vector.tensor_scalar(out=dst_ap, in0=src_ps_ap, scalar1=1.0 / D, scalar2=EPS, op0=ALU.mult,
                                                  op1=ALU.add))
            act.wait(t0)
            t1 = act.mark(nc.scalar.activation(out=dst_ap, in_=dst_ap, func=AF.Ln))
            act.wait(t1)
            t2 = act.mark(nc.scalar.activation(out=dst_ap, in_=dst_ap, func=AF.Exp, scale=-0.5))
            return t2, t0

        abctr = [0]

        def nextAB():
            i = abctr[0] % 2
            abctr[0] += 1
            return i

        ab_free = {("A", 0): None, ("A", 1): None, ("B", 0): None, ("B", 1): None}

        hm = MT
        t_hm = None
        for kc in range(KC):
            s, t = load_x(memT[kc], n=256)
            t_sq, t_pe = stats_acc(xc[s][:, 0:256], t, kc, n=256)
            sqs = sqc[(sqctr[0] - 1) % 2]
            for mc in range(2):
                t_pe = pe.mark(nc.tensor.matmul(X[mc][:, 0:1], lhsT=sqs[:, mc * 128:(mc + 1) * 128],
                                                rhs=ones_b[:, 0:1], start=(kc == 0), stop=(kc == KC - 1)))
            sq_free[(sqctr[0] - 1) % 2] = t_pe
            dve.wait(t)
            t_hm = dve.mark(nc.vector.tensor_scalar(out=hm[:, kc, 0:256], in0=xc[s][:, 0:256], scalar1=g_sb[:, 2, kc:kc + 1],
                                                    scalar2=None, op0=ALU.mult))
            x_free[s] = [t_sq, t_hm]
        t_rsm, t_rsm0 = make_rstd(rsm[:, 0:256], SS[:, 0:256], t_pe)
        free["SS"] = t_rsm0
        for mc in range(2):
            t_rsm, t_x0 = make_rstd(rsm[:, 256 + mc:257 + mc], X[mc][:, 0:1], t_pe)
            free["X%d" % mc] = t_x0
        pe.wait(t_hm)
        for hd in range(4):
            a = nextAB()
            t_mm = lin(w_xkv[hd], KC, [(A[a][:, 0:256], lambda kc: hm[:, kc, 0:256])], extra_wait=ab_free[("A", a)])
            dve.wait(t_mm, t_rsm)
            ab_free[("A", a)] = dve.mark(nc.vector.tensor_tensor(out=kmT[:, hd, :], in0=A[a][:, 0:256], in1=rsm[:, 0:256],
                                                                 op=ALU.mult))
        for blk in range(4):
            s, t = load_w(w_xkv[4 + blk], KC)
            a = nextAB()
            pe.wait(t, ab_free[("A", a)])
            for mc in range(2):
                for kc in range(KC):
                    ins = nc.tensor.matmul(A[a][:, mc * 128:(mc + 1) * 128], lhsT=hm[:, kc, mc * 128:(mc + 1) * 128],
                                           rhs=wsl[s][:, kc, :], start=(kc == 0), stop=(kc == KC - 1))
            t_mm = pe.mark(ins)
            w_free[s] = t_mm
            act.wait(t_mm, t_rsm)
            for mc in range(2):
                t_e = act.mark(nc.scalar.activation(out=vm[:, mc, blk * 128:(blk + 1) * 128],
                                                    in_=A[a][:, mc * 128:(mc + 1) * 128], func=AF.Copy,
                                                    scale=rsm[:, 256 + mc:257 + mc]))
            ab_free[("A", a)] = t_e
        t_mem_done = [ab_free[("A", 0)], ab_free[("A", 1)]]
        free["MT"] = t_mm

        xa_scale = 128.0 ** -0.5
        st_tok = {}

        def residual_stage(p, n_blocks, w_tiles, KCw, rhs_fn, src_fn, dst, gain_idx, pre_wait=None, final=False):
            t_pe_last = None
            t_h = None
            pend = None
            for nb in range(n_blocks):
                a = nextAB()
                t_mm = lin(w_tiles[nb], KCw, [(A[a][:, :], rhs_fn)], extra_wait=[ab_free[("A", a)], pre_wait])
                if pend is not None:
                    stats_mm(pend, nb - 1)
                s, t_x = load_x(src_fn(nb), extra=st_tok.get(("src", id(src_fn), nb)))
                dve.wait(t_mm, t_x)
                t_add = dve.mark(nc.vector.tensor_tensor(out=xc[s][:], in0=A[a][:, :], in1=xc[s][:], op=ALU.add))
                ab_free[("A", a)] = t_add
                sp.wait(t_add)
                t_st = k.dma(sp, dst[nb], xc[s][:], s_m)
                st_tok[(id(dst), nb)] = t_st
                pend = stats_sq(xc[s][:], t_add)
                t_sq = pend[1]
                if nb == n_blocks - 1:
                    t_pe_last = stats_mm(pend, nb)
                if gain_idx is not None:
                    dve.wait(fr("hT"))
                    t_h = dve.mark(nc.vector.tensor_scalar(out=hT[:, nb, :], in0=xc[s][:], scalar1=g_sb[:, gain_idx, nb:nb + 1],
                                                           scalar2=None, op0=ALU.mult))
                    x_free[s] = [t_st, t_sq, t_h]
                else:
                    x_free[s] = [t_st, t_sq]
            t_rs, t_rs0 = make_rstd(rs[:], SS[:, :], t_pe_last, guard=fr("rs"))
            free["SS"] = t_rs0
            return t_rs, t_h

        for p in range(NP):
            tc0 = p * T
            dve.wait(fr("hT"))
            for kc in range(KC):
                s, t = load_x(xin[p, kc])
                t_sq, t_pe = stats_acc(xc[s][:], t, kc)
                dve.wait(t)
                t_h = dve.mark(nc.vector.tensor_scalar(out=hT[:, kc, :], in0=xc[s][:], scalar1=g_sb[:, 0, kc:kc + 1],
                                                       scalar2=None, op0=ALU.mult))
                x_free[s] = [t_sq, t_h]
            t_rs, t_rs0 = make_rstd(rs[:], SS[:, :], t_pe, guard=fr("rs"))
            free["SS"] = t_rs0
            sp.wait(fr("xh"))
            k.dma(sp, icv[:], invc[p], s_m)
            t_xh = k.dma(sp, xh[:], xhalo[p], s_m)
            act.wait(t_xh, fr("sqh"))
            t_sqh = act.mark(nc.scalar.activation(out=sqh[:], in_=xh[:], func=AF.Square))
            dve.wait(t_xh, fr("hh"))
            t_hh = dve.mark(nc.vector.tensor_tensor(out=hh[:], in0=xh[:], in1=bc(g_sb[:, 0, :], 2, [128, KC, 16]), op=ALU.mult))
            free["xh"] = [t_sqh, t_hh]
            pe.wait(t_sqh, fr("SS"))
            for kc in range(KC):
                ins = nc.tensor.matmul(SS[:, 0:16], lhsT=ones_b[:], rhs=sqh[:, kc, :], start=(kc == 0), stop=(kc == KC - 1))
            t_ssh = pe.mark(ins)
            free["sqh"] = t_ssh
            t_rsh, t_rsh0 = make_rstd(rsh[:], SS[:, 0:16], t_ssh, guard=fr("rsh"))
            free["SS"] = t_rsh0
            pe.wait(t_h, t_hh)

            pooledT = MT
            dve.wait(fr("MT"))
            t_pool = None
            for nb in range(PC):
                g = nb // CPG
                a = nextAB()
                t_mm = lin(w_pool[nb], KC, [(A[a][:, :], lambda kc: hT[:, kc, :]), (B[a][:, 0:16], lambda kc: hh[:, kc, :])],
                           extra_wait=[ab_free[("A", a)], ab_free[("B", a)]])
                dve.wait(t_mm, t_rs, t_rsh, t_pool)
                nc.vector.tensor_tensor(out=ubuf[:, 16:528], in0=A[a][:, :], in1=rs[:], op=ALU.mult)
                t_u = dve.mark(nc.vector.tensor_tensor(out=ubuf[:, 0:16], in0=B[a][:, 0:16], in1=rsh[:], op=ALU.mult))
                ab_free[("A", a)] = t_u
                ab_free[("B", a)] = t_u
                dve.wait(t_u)
                t_r = dve.mark(nc.vector.tensor_tensor(out=ra[:, 1:528], in0=ubuf[:, 1:528], in1=ubuf[:, 0:527], op=ALU.add))
                r = ra
                if g >= 1:
                    dve.wait(t_r)
                    t_r = dve.mark(nc.vector.tensor_tensor(out=rb[:, 3:528], in0=ra[:, 3:528], in1=ra[:, 1:526], op=ALU.add))
                    r = rb
                if g >= 2:
                    dve.wait(t_r)
                    t_r = dve.mark(nc.vector.tensor_tensor(out=ra[:, 7:528], in0=rb[:, 7:528], in1=rb[:, 3:524], op=ALU.add))
                    r = ra
                if g >= 3:
                    dve.wait(t_r)
                    t_r = dve.mark(nc.vector.tensor_tensor(out=rb[:, 15:528], in0=ra[:, 15:528], in1=ra[:, 7:520], op=ALU.add))
                    r = rb
                w = (2, 4, 8, 16)[g]
                dve.wait(t_r)
                nc.vector.scalar_tensor_tensor(out=pooledT[:, nb, 16:512], in0=r[:, 32:528], scalar=1.0 / w,
                                               in1=ubuf[:, 32:528], op0=ALU.mult, op1=ALU.subtract)
                t_a = dve.mark(nc.vector.tensor_tensor(out=t16[:], in0=r[:, 16:32], in1=icv[:, g, :], op=ALU.mult))
                dve.wait(t_a)
                t_pool = dve.mark(nc.vector.tensor_tensor(out=pooledT[:, nb, 0:16], in0=t16[:], in1=ubuf[:, 16:32],
                                                          op=ALU.subtract))
            mixedT = aux
            pe.wait(t_pool)
            act.wait(fr("aux"))
            for nb in range(PC):
                g = nb // CPG
                a = nextAB()
                t_mm = lin(w_grp[nb], CPG, [(A[a][:, :], lambda kc, g=g: pooledT[:, g * CPG + kc, :])],
                           extra_wait=ab_free[("A", a)])
                act.wait(t_mm)
                t_mx = act.mark(nc.scalar.activation(out=mixedT[:, nb, :], in_=A[a][:, :], func=AF.Copy,
                                                     scale=g_sb[:, 5, nb:nb + 1]))
                ab_free[("A", a)] = t_mx
            pe.wait(t_mx)
            dve.wait(t_mm)
            for nb in range(KC):
                a = nextAB()
                t_a = lin(w_bpool[nb], PC, [(A[a][:, :], lambda kc: mixedT[:, kc, :])], extra_wait=ab_free[("A", a)])
                t_b = lin(w_gpool[nb], KC, [(B[a][:, :], lambda kc: hT[:, kc, :])], extra_wait=ab_free[("B", a)])
                f = ft[nb % 2]
                dve.wait(t_b, fr(("ft", nb % 2)))
                t_g = dve.mark(nc.vector.tensor_tensor(out=f[:], in0=B[a][:, :], in1=rs[:], op=ALU.mult))
                ab_free[("B", a)] = t_g
                act.wait(t_g)
                t_s = act.mark(nc.scalar.activation(out=f[:], in_=f[:], func=AF.Sigmoid))
                dve.wait(t_s, t_a)
                t_m = dve.mark(nc.vector.tensor_tensor(out=MT[:, nb, :], in0=f[:], in1=A[a][:, :], op=ALU.mult))
                ab_free[("A", a)] = t_m
                free[("ft", nb % 2)] = t_m
            sp.wait(t_a)
            t_at = k.dma(sp, aux[:], attn_in.rearrange("(h q) t -> q h t", q=128)[:, :, tc0:tc0 + T], s_m)
            pe.wait(t_at)
            for nb in range(KC):
                a = nextAB()
                t_a = lin(w_bsb[nb], 16, [(A[a][:, :], lambda kc: aux[:, kc, :])], extra_wait=ab_free[("A", a)])
                t_b = lin(w_gsb[nb], KC, [(B[a][:, :], lambda kc: hT[:, kc, :])], extra_wait=ab_free[("B", a)])
                f = ft[nb % 2]
                dve.wait(t_b, fr(("ft", nb % 2)))
                t_g = dve.mark(nc.vector.tensor_tensor(out=f[:], in0=B[a][:, :], in1=rs[:], op=ALU.mult))
                ab_free[("B", a)] = t_g
                act.wait(t_g)
                t_s = act.mark(nc.scalar.activation(out=f[:], in_=f[:], func=AF.Sigmoid))
                dve.wait(t_s, t_a)
                t_m0 = dve.mark(nc.vector.tensor_tensor(out=f[:], in0=f[:], in1=A[a][:, :], op=ALU.mult))
                ab_free[("A", a)] = t_m0
                dve.wait(t_m0)
                t_m = dve.mark(nc.vector.tensor_tensor(out=MT[:, nb, :], in0=MT[:, nb, :], in1=f[:], op=ALU.add))
                free[("ft", nb % 2)] = t_m
            free["aux"] = t_a
            free["hT"] = t_b
            free["rs"] = t_g
            pe.wait(t_m)
            t_rs, t_h = residual_stage(p, KC, w_out, KC, lambda kc: MT[:, kc, :], lambda nb: xin[p, nb], xr1, 1)
            pe.wait(t_h)
            for hd in range(4):
                a = nextAB()
                t_mm = lin(w_xq[hd], KC, [(A[a][:, :], lambda kc: hT[:, kc, :])], extra_wait=ab_free[("A", a)])
                dve.wait(t_mm, t_rs, fr("qx"))
                t_q = dve.mark(nc.vector.scalar_tensor_tensor(out=qx[:, hd, :], in0=A[a][:, :], scalar=xa_scale, in1=rs[:],
                                                              op0=ALU.mult, op1=ALU.mult))
                ab_free[("A", a)] = t_q
            free["hT"] = t_mm
            free["rs"] = t_q
            pe.wait(t_q, t_mem_done)
            for hd in range(4):
                pe.wait(fr("X0"), fr("X1"))
                for mc in range(2):
                    ins = nc.tensor.matmul(X[mc][:, :], lhsT=kmT[:, hd, mc * 128:(mc + 1) * 128], rhs=qx[:, hd, :], start=True,
                                           stop=True)
                t_sc = pe.mark(ins)
                act.wait(t_sc, fr("pT"))
                for mc in range(2):
                    t_p = act.mark(nc.scalar.activation(out=pT[:, mc, :], in_=X[mc][:, :], func=AF.Exp))
                free["X0"] = t_p
                free["X1"] = t_p
                a = nextAB()
                pe.wait(t_p, ab_free[("A", a)], ab_free[("B", a)])
                for mc in range(2):
                    nc.tensor.matmul(B[a][:, :], lhsT=ones_b[:], rhs=pT[:, mc, :], start=(mc == 0), stop=(mc == 1))
                for mc in range(2):
                    ins = nc.tensor.matmul(A[a][:, :], lhsT=vm[:, mc, hd * 128:(hd + 1) * 128], rhs=pT[:, mc, :],
                                           start=(mc == 0), stop=(mc == 1))
                t_o = pe.mark(ins)
                free["pT"] = t_o
                f = ft[hd % 2]
                dve.wait(t_o, fr(("ft", hd % 2)), fr("ox"))
                t_rd = dve.mark(nc.vector.reciprocal(out=f[:], in_=B[a][:, :]))
                dve.wait(t_rd)
                t_ox = dve.mark(nc.vector.tensor_tensor(out=ox[:, hd, :], in0=A[a][:, :], in1=f[:], op=ALU.mult))
                ab_free[("A", a)] = t_ox
                ab_free[("B", a)] = t_ox
                free[("ft", hd % 2)] = t_ox
            free["qx"] = t_sc
            pe.wait(t_ox)
            src1 = lambda nb: xr1[nb]
            for nb in range(KC):
                st_tok[("src", id(src1), nb)] = st_tok[(id(xr1), nb)]
            t_rs, t_h = residual_stage(p, KC, w_xo, 4, lambda kc: ox[:, kc, :], src1, xr2, 3)
            free["ox"] = w_free[(wctr[0] - 1) % NWS]
            qp = aux
            pe.wait(t_h)
            dve.wait(fr("aux"))
            for nb in range(16):
                a = nextAB()
                t_mm = lin(w_pq[nb], KC, [(A[a][:, :], lambda kc: hT[:, kc, :])], extra_wait=ab_free[("A", a)])
                dve.wait(t_mm, t_rs)
                t_q = dve.mark(nc.vector.tensor_tensor(out=qp[:, nb, :], in0=A[a][:, :], in1=rs[:], op=ALU.mult))
                ab_free[("A", a)] = t_q
            MTf = MT[:].rearrange("q a b -> q (a b)").bitcast(F32)
            sc = MTf[:, 0:2048]
            sc2 = MTf[:, 2048:4096]
            cand = MTf[:, 4096:6144]
            pe.wait(t_q)
            dve.wait(fr("MT"), t_m)
            act.wait(t_m)
            t_tk = None
            for tt in range(4):
                tsl = slice(tt * 128, (tt + 1) * 128)
                pe.wait(fr("X0"), fr("X1"), fr("X2"), fr("SS"))
                banks = [X[0], X[1], X[2], SS]
                for l in range(16):
                    ins = nc.tensor.matmul(banks[l // 4][:, (l % 4) * 128:(l % 4 + 1) * 128], lhsT=qp[:, l, tsl],
                                           rhs=keys_b[:, l, :], start=True, stop=True)
                t_s = pe.mark(ins)
                act.wait(t_s, t_tk)
                for bi in range(4):
                    t_cp = act.mark(nc.scalar.copy(out=sc[:, bi * 512:(bi + 1) * 512], in_=banks[bi][:, :]))
                for nm in ("X0", "X1", "X2", "SS"):
                    free[nm] = t_cp
                dve.wait(t_cp)
                for l in range(16):
                    row = sc[:, l * 128:(l + 1) * 128]
                    row2 = sc2[:, l * 128:(l + 1) * 128]
                    t1 = dve.mark(nc.vector.max(out=top[:, l, 0:8], in_=row))
                    dve.wait(t1)
                    t2 = dve.mark(nc.vector.match_replace(out=row2, in_to_replace=top[:, l, 0:8], in_values=row, imm_value=NEG))
                    dve.wait(t2)
                    t3 = dve.mark(nc.vector.max(out=top[:, l, 8:16], in_=row2))
                dve.wait(t3)
                top4 = top[:].rearrange("q (h two) a -> q h two a", two=2)
                cand4 = cand[:, 0:2048].rearrange("q (h a b) -> q h a b", h=8, a=16)
                t_cd = dve.mark(nc.vector.tensor_tensor(out=cand4, in0=bc(top4[:, :, 0, :], 3, [128, 8, 16, 16]),
                                                        in1=bc(top4[:, :, 1, :], 2, [128, 8, 16, 16]), op=ALU.add))
                dve.wait(t_cd)
                for h in range(8):
                    row = cand[:, h * 256:(h + 1) * 256]
                    row2 = sc2[:, h * 256:(h + 1) * 256]
                    t1 = dve.mark(nc.vector.max(out=best[:, h, 0:8], in_=row))
                    dve.wait(t1)
                    t2 = dve.mark(nc.vector.match_replace(out=row2, in_to_replace=best[:, h, 0:8], in_values=row, imm_value=NEG))
                    dve.wait(t2)
                    t3 = dve.mark(nc.vector.max(out=best[:, h, 8:16], in_=row2))
                dve.wait(t3)
                dd = sc2[:, 0:128].rearrange("q (h a) -> q h a", h=8)
                t_d = dve.mark(nc.vector.tensor_tensor(out=dd, in0=best[:], in1=bc(best[:, :, 0], 2, [128, 8, 16]),
                                                       op=ALU.subtract))
                act.wait(t_d)
                t_e = act.mark(nc.scalar.activation(out=dd, in_=dd, func=AF.Exp))
                dve.wait(t_e)
                t_z = dve.mark(nc.vector.reduce_sum(out=zz[:, :, 0], in_=dd, axis=AX.X))
                act.wait(t_z)
                t_lz = act.mark(nc.scalar.activation(out=zz[:, :, 1], in_=zz[:, :, 0], func=AF.Ln))
                dve.wait(t_lz)
                nc.vector.scalar_tensor_tensor(out=negb[:, tt, :], in0=best[:, :, 0], scalar=-1.0, in1=zz[:, :, 1],
                                               op0=ALU.mult, op1=ALU.subtract)
                t_tk = dve.mark(nc.vector.tensor_scalar(out=tau[:, tt, :], in0=best[:, :, 15], scalar1=-2e-5, scalar2=None,
                                                        op0=ALU.add))
            act.wait(t_tk)
            WT = MT
            gctr = 0
            free[("S", 0)] = fr("X0")
            free[("S", 1)] = fr("SS")
            free[("S", 2)] = ab_free[("B", 0)]
            free[("S", 3)] = ab_free[("B", 1)]
            for i_ in range(4):
                free[("et", i_)] = [fr(("ft", 0)), fr(("ft", 1))]
            free["Gs"] = [fr("X1"), fr("X2")]
            for qtr in range(4):
                for grp in range(16):
                    i0 = qtr * 32 + grp * 2
                    Gs = [X[1], X[2]]
                    gits = [(tt, h) for tt in range(4) for h in range(8)]
                    grec = [dict() for _ in gits]

                    def gA(ii):
                        tt, h = gits[ii]
                        tsl = slice(tt * 128, (tt + 1) * 128)
                        it = gbase + ii
                        sb_ = it % 4
                        sbank = [X[0], SS, B[0], B[1]][sb_]
                        pe.wait(fr(("S", sb_)))
                        nc.tensor.matmul(sbank[:, 0:256], lhsT=qp[:, 2 * h, tsl],
                                         rhs=bc(keys_b[:, 2 * h, i0:i0 + 2], 2, [128, 2, 128]), start=True, stop=False)
                        t_S = pe.mark(nc.tensor.matmul(sbank[:, 0:256], lhsT=qp[:, 2 * h + 1, tsl],
                                                       rhs=bc(keys_b[:, 2 * h + 1, :], 1, [128, 2, 128]), start=False, stop=True))
                        e_t = ft[2 + sb_]
                        act.wait(t_S, fr(("et", sb_)))
                        t_E = act.mark(nc.scalar.activation(out=e_t[:, 0:256], in_=sbank[:, 0:256], func=AF.Exp,
                                                            bias=negb[:, tt, h:h + 1]))
                        g_t = bt[it % 6]
                        dve.wait(t_E, fr(("gt", it % 6)))
                        t_G = dve.mark(nc.vector.scalar_tensor_tensor(out=g_t[:, 0:256], in0=sbank[:, 0:256],
                                                                      scalar=tau[:, tt, h:h + 1], in1=e_t[:, 0:256],
                                                                      op0=ALU.is_ge, op1=ALU.mult))
                        free[("S", sb_)] = t_G
                        free[("et", sb_)] = t_G
                        grec[ii]["G"] = t_G

                    def gB(ii):
                        tt, h = gits[ii]
                        tsl = slice(tt * 128, (tt + 1) * 128)
                        it = gbase + ii
                        g_t = bt[it % 6]
                        pe.wait(grec[ii]["G"], fr("Gs") if ii == 0 else None)
                        for ib in range(2):
                            ins = nc.tensor.matmul(Gs[ib][:, tsl], lhsT=g_t[:, ib * 128:(ib + 1) * 128], rhs=ident_b[:],
                                                   start=(h == 0), stop=(h == 7))
                        free[("gt", it % 6)] = pe.mark(ins)

                    gbase = gctr
                    SK = 3
                    for ii in range(len(gits) + SK):
                        if ii < len(gits):
                            gA(ii)
                        if ii >= SK:
                            gB(ii - SK)
                    gctr += len(gits)
                    t_Gs = free[("gt", (gctr - 1) % 6)]
                    for ib in range(2):
                        eb = i0 + ib
                        ebl = eb - qtr * 32
                        a = nextAB()
                        t_mm = lin(w_down[eb], KC, [(A[a][:, :], lambda kc: hT[:, kc, :])], extra_wait=ab_free[("A", a)])
                        f = ft[ib]
                        dve.wait(t_mm, t_rs, fr(("ft", ib)))
                        t_a = dve.mark(nc.vector.tensor_tensor(out=f[:], in0=A[a][:, :], in1=rs[:], op=ALU.mult))
                        ab_free[("A", a)] = t_a
                        act.wait(t_a)
                        t_ge = act.mark(nc.scalar.activation(out=f[:], in_=f[:], func=AF.Gelu))
                        dve.wait(t_ge, t_Gs, fr("WT"))
                        t_w = dve.mark(nc.vector.tensor_tensor(out=WT[:, ebl, :], in0=f[:], in1=Gs[ib][:, :], op=ALU.mult))
                        free[("ft", ib)] = t_w
                    free["Gs"] = t_w
                pe.wait(t_w)
                free["SS"] = free[("S", 1)]
                free["X0"] = free[("S", 0)]
                free["X1"] = t_w
                free["X2"] = t_w
                last = (qtr == 3)
                if qtr == 0:
                    srcq = lambda nb: xr2[nb]
                    for nb in range(KC):
                        st_tok[("src", id(srcq), nb)] = st_tok[(id(xr2), nb)]
                else:
                    srcq = lambda nb: xr3[nb]
                    for nb in range(KC):
                        st_tok[("src", id(srcq), nb)] = st_tok[(id(xr3), nb)]
                t_pe_last = None
                pend = None
                for nb in range(KC):
                    a = nextAB()
                    t_mm = lin(w_up[qtr, nb], 32, [(A[a][:, :], lambda kc: WT[:, kc, :])], extra_wait=ab_free[("A", a)])
                    if pend is not None:
                        stats_mm(pend, nb - 1)
                        pend = None
                    s, t_x = load_x(srcq(nb), extra=st_tok.get(("src", id(srcq), nb)))
                    dve.wait(t_mm, t_x)
                    t_add = dve.mark(nc.vector.tensor_tensor(out=xc[s][:], in0=A[a][:, :], in1=xc[s][:], op=ALU.add))
                    ab_free[("A", a)] = t_add
                    sp.wait(t_add)
                    t_st = k.dma(sp, xr3[nb], xc[s][:], s_m)
                    st_tok[(id(xr3), nb)] = t_st
                    if last:
                        pend = stats_sq(xc[s][:], t_add)
                        x_free[s] = [t_st, pend[1]]
                        if nb == KC - 1:
                            t_pe_last = stats_mm(pend, nb)
                    else:
                        x_free[s] = [t_st]
                free["WT"] = t_mm
            free["hT"] = t_mm
            free["MT"] = t_mm
            free["aux"] = t_Gs
            ab_free[("B", 0)] = [ab_free[("B", 0)], free[("S", 2)]]
            ab_free[("B", 1)] = [ab_free[("B", 1)], free[("S", 3)]]
            t_rs, t_rs0 = make_rstd(rs[:], SS[:, :], t_pe_last, guard=[fr("rs"), t_a])
            free["SS"] = t_rs0
            for nb in range(KC):
                s, t_x = load_x(xr3[nb], extra=st_tok[(id(xr3), nb)])
                dve.wait(t_x, t_rs)
                t_o = dve.mark(nc.vector.scalar_tensor_tensor(out=xc[s][:], in0=xc[s][:], scalar=g_sb[:, 4, nb:nb + 1], in1=rs[:],
                                                              op0=ALU.mult, op1=ALU.mult))
                sp.wait(t_o)
                t_st = k.dma(sp, outT[p, nb], xc[s][:], s_m)
                x_free[s] = [t_st]
            free["rs"] = t_o
        sp.wait((s_m, s_m.count))
    return nc


def tile_w(W):
    K_, N_ = W.shape
    return np.ascontiguousarray(W.reshape(K_ // 128, 128, N_ // 128, 128).transpose(2, 1, 0, 3))


def vec_pk(v, KC):
    o = np.zeros((128, KC), np.float32)
    n = v.shape[0] // 128
    o[:, :n] = v.reshape(n, 128).T
    return o


def make_cst():
    s_idx = np.arange(128)
    cst = np.zeros((128, 384), np.float32)
    cst[:, 0:128] = (s_idx[:, None] < s_idx[None, :])
    cst[:, 128:256] = -1.0 * (s_idx[:, None] >= s_idx[None, :])
    cst[:, 256:384] = np.eye(128)
    return cst


def host_phase2_shared(inp, D):
    KC = D // 128
    SBW = 2048
    PW = D // 2
    PC = PW // 128
    w_in = inp["w_in"][0]
    o = 3 * SBW
    sh = {}
    sh["w_pool"] = tile_w(w_in[:, o:o + PW])
    sh["w_gsb"] = tile_w(w_in[:, o + PW:o + PW + D])
    sh["w_gpool"] = tile_w(w_in[:, o + PW + D:o + PW + 2 * D])
    sh["w_grp"] = np.concatenate([tile_w(inp["pool_group_w"][0, g]) for g in range(4)], axis=0)
    sh["w_bsb"] = tile_w(inp["w_branch_sb"][0])
    sh["w_bpool"] = tile_w(inp["w_branch_pool"][0])
    sh["w_out"] = tile_w(inp["w_out"][0])
    sh["w_xq"] = tile_w(inp["xa_w_q"][0])
    sh["w_xkv"] = tile_w(inp["xa_w_kv"][0])
    sh["w_xo"] = tile_w(inp["xa_w_o"][0])
    sh["w_pq"] = tile_w(inp["peer_w_query"][0])
    sh["w_down"] = tile_w(inp["peer_down"][0].T)
    sh["w_up"] = np.stack([tile_w(inp["peer_up"][0][q * 4096:(q + 1) * 4096]) for q in range(4)], axis=0)
    sh["keysT"] = np.ascontiguousarray(inp["peer_sub_keys"][0].transpose(3, 0, 1, 2).reshape(128, 16, 128))
    g = np.zeros((128, 6, KC), np.float32)
    g[:, 0] = vec_pk(inp["norm_mix"][0], KC)
    g[:, 1] = vec_pk(inp["norm_mem_q"][0], KC)
    g[:, 2] = vec_pk(inp["norm_mem_kv"][0], KC)
    g[:, 3] = vec_pk(inp["norm_ffn"][0], KC)
    g[:, 4] = vec_pk(inp["norm_final"], KC)
    g[:, 5] = vec_pk(inp["pool_scale"][0], KC)
    sh["gains"] = g
    sh["cst"] = make_cst()
    mem = inp["mem"][0]
    sh["memT"] = np.ascontiguousarray(mem.reshape(256, KC, 128).transpose(1, 2, 0))
    return sh


def host_phase2_core(x, attn_full, tok0, TOK, D):
    KC = D // 128
    NP = TOK // 512
    xs = x[tok0:tok0 + TOK]
    m = {}
    m["xin"] = np.ascontiguousarray(xs.reshape(NP, 512, KC, 128).transpose(0, 2, 3, 1))
    xh = np.zeros((NP, 128, KC, 16), np.float32)
    ic = np.zeros((NP, 128, 4, 16), np.float32)
    for p in range(NP):
        st = tok0 + p * 512
        if st >= 16:
            xh[p] = x[st - 16:st].reshape(16, KC, 128).transpose(2, 1, 0)
        pos = st + np.arange(16)
        for gi, w in enumerate((2, 4, 8, 16)):
            ic[p, :, gi, :] = 1.0 / np.minimum(pos + 1, w)
    m["xhalo"] = xh
    m["invc"] = ic
    m["attn_in"] = np.ascontiguousarray(attn_full[:, tok0:tok0 + TOK])
    return m


def unpack_out(outT, TOK, D):
    KC = D // 128
    NP = TOK // 512
    return np.ascontiguousarray(outT.reshape(NP, KC, 128, 512).transpose(0, 3, 1, 2)).reshape(TOK, D)


def kernel(**inp):
    S, D = 8192, 4096
    inp = {k_: np.asarray(v) for k_, v in inp.items()}
    nc = bass.Bass("TRN2", target_bir_lowering=False)
    build_fused(nc, S, D)
    maps, owners = host_fused_inputs(inp, S, D)
    r = run_bass_kernel_spmd(nc, maps, core_ids=list(range(NCORES)))
    out = np.zeros((S, D), np.float32)
    for c in range(NCORES):
        o = unpack_out(r.results[c]["outT"], 1024, D)
        for p, g in enumerate(owners[c]):
            out[g * 512:(g + 1) * 512] = o[p * 512:(p + 1) * 512]
    return out.reshape(1, S, D)


def declare_tensors(nc, S, TOK, D):
    KC = D // 128
    NT = S // 128
    NP = TOK // 512
    PC = (D // 2) // 128
    CPG = PC // 4
    T = 512

    def din(name, shape, dt=F32):
        return nc.dram_tensor(name, shape, dt, kind="ExternalInput").ap()
    t = {}
    t["xT"] = din("xT", [NT, 128, KC, 128])
    t["wqkv"] = din("wqkv", [8, 128, KC, 768])
    t["bias_tab"] = din("bias_tab", [2, NT, 128, 512], BF16)
    t["xin"] = din("xin", [NP, KC, 128, T])
    t["xhalo"] = din("xhalo", [NP, 128, KC, 16])
    t["invc"] = din("invc", [NP, 128, 4, 16])
    t["memT"] = din("memT", [KC, 128, 256])
    t["gains"] = din("gains", [128, 6, KC])
    t["cst"] = din("cst", [128, 384])
    t["keysT"] = din("keysT", [128, 16, 128])
    t["w_pool"] = din("w_pool", [PC, 128, KC, 128])
    t["w_grp"] = din("w_grp", [PC, 128, CPG, 128])
    t["w_gsb"] = din("w_gsb", [KC, 128, KC, 128])
    t["w_gpool"] = din("w_gpool", [KC, 128, KC, 128])
    t["w_bsb"] = din("w_bsb", [KC, 128, 16, 128])
    t["w_bpool"] = din("w_bpool", [KC, 128, PC, 128])
    t["w_out"] = din("w_out", [KC, 128, KC, 128])
    t["w_xq"] = din("w_xq", [4, 128, KC, 128])
    t["w_xkv"] = din("w_xkv", [8, 128, KC, 128])
    t["w_xo"] = din("w_xo", [KC, 128, 4, 128])
    t["w_pq"] = din("w_pq", [16, 128, KC, 128])
    t["w_down"] = din("w_down", [128, 128, KC, 128])
    t["w_up"] = din("w_up", [4, KC, 128, 32, 128])
    t["outT"] = nc.dram_tensor("outT", [NP, KC, 128, T], F32, kind="ExternalOutput").ap()
    t["attn_scr"] = nc.dram_tensor("attn_scr", [16 * 128, TOK], BF16).ap()
    return t


def build_fused(nc, S, D):
    TOK = 1024
    t = declare_tensors(nc, S, TOK, D)
    with contextlib.ExitStack() as sem_stack:
        k = K(nc, None, sem_stack)
        build_phase1(nc, k, S, D, t)
        build_phase2(nc, k, TOK, D, t)
    return nc


def host_fused_inputs(inp, S, D):
    KC = D // 128
    NT = S // 128
    NG = S // 512
    x = inp["x"][0]
    sh = host_phase2_shared(inp, D)
    sh["xT"] = np.ascontiguousarray(x.reshape(NT, 128, KC, 128).transpose(0, 3, 2, 1))
    w_in = inp["w_in"][0]
    SBW = 2048
    wq = np.zeros((8, 128, KC, 768), np.float32)
    for hp in range(8):
        cols = []
        for part in range(3):
            for hh in range(2):
                h = hp * 2 + hh
                cols.append(w_in[:, part * SBW + h * 128: part * SBW + (h + 1) * 128])
        w = np.concatenate(cols, axis=1)
        wq[hp] = w.reshape(KC, 128, -1).transpose(1, 0, 2)
    sh["wqkv"] = wq
    maps = []
    owners = []
    s_idx = np.arange(128)
    t_idx = np.arange(512)
    for c in range(NCORES):
        groups = (c, NG - 1 - c)
        owners.append(groups)
        m = dict(sh)
        xs = np.concatenate([x[g * 512:(g + 1) * 512] for g in groups], axis=0)
        m["xin"] = np.ascontiguousarray(xs.reshape(2, 512, KC, 128).transpose(0, 2, 3, 1))
        xh = np.zeros((2, 128, KC, 16), np.float32)
        ic = np.zeros((2, 128, 4, 16), np.float32)
        bt = np.zeros((2, NT, 128, 512), np.float32)
        for p, g in enumerate(groups):
            st = g * 512
            if st >= 16:
                xh[p] = x[st - 16:st].reshape(16, KC, 128).transpose(2, 1, 0)
            pos = st + np.arange(16)
            for gi, w in enumerate((2, 4, 8, 16)):
                ic[p, :, gi, :] = 1.0 / np.minimum(pos + 1, w)
            qpos = st + t_idx
            for kb in range(NT):
                kpos = kb * 128 + s_idx
                bt[p, kb] = np.where(kpos[:, None] < qpos[None, :], 0.0, -30000.0)
        m["xhalo"] = xh
        m["invc"] = ic
        m["bias_tab"] = bt.astype(ml_dtypes.bfloat16)
        maps.append(m)
    return maps, owners
```
